# Optimizing a Trainium2 kernel written in Bass

```python
import jax, jax.numpy as jnp
from jax import lax
import numpy as np

D_MODEL = 2048
BATCH = 4
SEQ = 2048
DEPTH = 2

GRID_W = 64
CTX_LEN = 256

NA_HEADS = 16
NA_HEAD_DIM = 64
NA_WIN_ROWS = 8
NA_WIN_COLS = 16
GLA_HEADS = 4
GLA_DK = 128
GLA_DV = 256
GLA_GATE_RANK = 16
GLA_TAU = 16.0
GLA_CHUNK = 64
GQA_HEADS = 8
GQA_KV_HEADS = 2
GQA_HEAD_DIM = 128
ROPE_THETA = 10000.0
Q_BLOCK = 128
D_FF = 5632
N_EXPERTS = 8
TOP_K = 2
MOE_BLOCK = 256
NORM_EPS = 1e-6

NA_W = NA_HEADS * NA_HEAD_DIM
GLA_KW = GLA_HEADS * GLA_DK
GLA_VW = GLA_HEADS * GLA_DV
GQA_QW = GQA_HEADS * GQA_HEAD_DIM
GQA_KVW = GQA_KV_HEADS * GQA_HEAD_DIM
BRANCH_W = 1024

IN_SPLITS = (
    ("na_q", NA_W), ("na_k", NA_W), ("na_v", NA_W),
    ("gla_q", GLA_KW), ("gla_k", GLA_KW), ("gla_v", GLA_VW), ("gla_og", GLA_VW),
    ("gla_af", GLA_GATE_RANK), ("gla_ab", GLA_GATE_RANK),
    ("gqa_q", GQA_QW), ("gqa_k", GQA_KVW), ("gqa_v", GQA_KVW),
    ("gate_a", D_MODEL), ("gate_b", D_MODEL), ("gate_c", D_MODEL),
)
D_IN = 3 * NA_W + 2 * GLA_KW + 2 * GLA_VW + 2 * GLA_GATE_RANK + GQA_QW + 2 * GQA_KVW + 3 * D_MODEL
CTX_KV_SPLITS = ("na_k", "na_v", "gla_k", "gla_v", "gla_af", "gla_ab", "gqa_k", "gqa_v")

kernel_name = "hybrid_diffusion_parallel_mixer_block"


def rmsnorm(x, g):
    xf = x.astype(jnp.float32)
    y = xf * lax.rsqrt(jnp.mean(xf * xf, axis=-1, keepdims=True) + NORM_EPS)
    return (y * g.astype(jnp.float32)).astype(x.dtype)


def swiglu(h, w_gate, w_up, w_down):
    return (jax.nn.silu(h @ w_gate) * (h @ w_up)) @ w_down


def in_proj(h, w_in, names=None):
    out, o = {}, 0
    p = h @ w_in if names is None else None
    for name, n in IN_SPLITS:
        if names is None:
            out[name] = p[..., o:o + n]
        elif name in names:
            out[name] = h @ w_in[:, o:o + n]
        o += n
    return out


def axial_rope_tables(n_tok):
    half = GQA_HEAD_DIM // 2
    freqs = ROPE_THETA ** (-jnp.arange(0, half, 2, dtype=jnp.float32) / half)
    t = jnp.arange(n_tok)
    row = (t // GRID_W).astype(jnp.float32)
    col = (t % GRID_W).astype(jnp.float32)
    ang = jnp.concatenate([row[:, None] * freqs, col[:, None] * freqs], axis=-1)
    return jnp.cos(ang), jnp.sin(ang)


def apply_rope(x, cos, sin):
    xf = x.astype(jnp.float32)
    x1, x2 = xf[..., 0::2], xf[..., 1::2]
    c, s = cos[None, :, None, :], sin[None, :, None, :]
    out = jnp.stack([x1 * c - x2 * s, x1 * s + x2 * c], axis=-1).reshape(x.shape)
    return out.astype(x.dtype)


def _attend(q5, k, v):
    s = jnp.einsum('bqkgd,bnkd->bkgqn', q5, k).astype(jnp.float32) * (q5.shape[-1] ** -0.5)
    p = jax.nn.softmax(s, axis=-1).astype(v.dtype)
    return jnp.einsum('bkgqn,bnkd->bqkgd', p, v)


def gqa_dense(q, k, v):
    b, t, hq, dh = q.shape
    hkv = k.shape[2]
    return _attend(q.reshape(b, t, hkv, hq // hkv, dh), k, v).reshape(b, t, hq * dh)


def gqa_blocked(q, k, v, kc, vc):
    b, s, hq, dh = q.shape
    hkv = k.shape[2]
    k_all = jnp.concatenate([k, kc], axis=1)
    v_all = jnp.concatenate([v, vc], axis=1)
    qb = q.reshape(b, s // Q_BLOCK, Q_BLOCK, hkv, hq // hkv, dh).swapaxes(0, 1)
    o = lax.map(lambda qi: _attend(qi, k_all, v_all), qb)
    return o.swapaxes(0, 1).reshape(b, s, hq * dh)


def na_latent(q, k, v, kc, vc, rpb):
    b, s, h, dh = q.shape
    rows_n = s // GRID_W
    kr = min(NA_WIN_ROWS, rows_n)
    rows = jnp.arange(rows_n)
    row_idx = jnp.clip(rows - kr // 2, 0, rows_n - kr)[:, None] + jnp.arange(kr)[None, :]
    cols = jnp.arange(GRID_W)
    col_start = jnp.clip(cols - NA_WIN_COLS // 2, 0, GRID_W - NA_WIN_COLS)
    in_win = (cols[None, :] >= col_start[:, None]) & (cols[None, :] < col_start[:, None] + NA_WIN_COLS)
    dr = row_idx - rows[:, None]
    dc = jnp.clip(cols[None, :] - cols[:, None] + NA_WIN_COLS - 1, 0, 2 * NA_WIN_COLS - 2)
    bias = rpb[:, (dr + NA_WIN_ROWS - 1)[:, :, None, None], dc[None, None]]
    bias = bias.astype(jnp.float32).transpose(0, 1, 3, 2, 4)
    bias = jnp.where(in_win[:, None, :], bias, -jnp.inf)
    qg = q.reshape(b, rows_n, GRID_W, h, dh) * (dh ** -0.5)
    kg = k.reshape(b, rows_n, GRID_W, h, dh)[:, row_idx]
    vg = v.reshape(b, rows_n, GRID_W, h, dh)[:, row_idx]
    s_loc = jnp.einsum('brqhd,brkwhd->bhrqkw', qg, kg).astype(jnp.float32) + bias[None]
    s_ctx = jnp.einsum('brqhd,blhd->bhrql', qg, kc).astype(jnp.float32)
    n_loc = kr * GRID_W
    p = jax.nn.softmax(jnp.concatenate([s_loc.reshape(b, h, rows_n, GRID_W, n_loc), s_ctx], axis=-1), axis=-1)
    p = p.astype(v.dtype)
    p_loc = p[..., :n_loc].reshape(b, h, rows_n, GRID_W, kr, GRID_W)
    o = jnp.einsum('bhrqkw,brkwhd->brqhd', p_loc, vg) + jnp.einsum('bhrql,blhd->brqhd', p[..., n_loc:], vc)
    return o.reshape(b, s, h * dh)


def gla_scan(q, k, v, log_a, s0):
    b, t, h, dk = k.shape
    n = t // GLA_CHUNK

    def chunks(a):
        return a.astype(jnp.float32).reshape(b, n, GLA_CHUNK, h, a.shape[-1]).transpose(1, 0, 3, 2, 4)

    mask = jnp.tril(jnp.ones((GLA_CHUNK, GLA_CHUNK), bool))[..., None]
    with_out = q is not None
    xs = (chunks(k), chunks(v), chunks(log_a)) + ((chunks(q),) if with_out else ())

    def step(state, inp):
        kc, vc, gc = inp[0], inp[1], inp[2]
        bcum = jnp.cumsum(gc, axis=-2)
        b_last = bcum[..., -1:, :]
        new_state = jnp.exp(b_last)[..., 0, :, None] * state + jnp.einsum(
            'bhjd,bhje->bhde', kc * jnp.exp(b_last - bcum), vc)
        if not with_out:
            return new_state, None
        qc = inp[3]
        diff = bcum[..., :, None, :] - bcum[..., None, :, :]
        decay = jnp.exp(jnp.where(mask, diff, -jnp.inf))
        attn = jnp.einsum('bhid,bhjd,bhijd->bhij', qc, kc, decay)
        o = attn @ vc + jnp.einsum('bhid,bhde->bhie', qc * jnp.exp(bcum), state)
        return new_state, o

    s_fin, o = lax.scan(step, s0, xs)
    if with_out:
        o = o.transpose(1, 0, 3, 2, 4).reshape(b, t, h, v.shape[-1])
    return o, s_fin


def na_branch(pl, pc, rpb, with_ctx):
    def heads(a):
        return a.reshape(a.shape[0], a.shape[1], NA_HEADS, NA_HEAD_DIM)
    kc, vc = heads(pc['na_k']), heads(pc['na_v'])
    yl = na_latent(heads(pl['na_q']), heads(pl['na_k']), heads(pl['na_v']), kc, vc, rpb)
    yc = gqa_dense(heads(pc['na_q']), kc, vc) if with_ctx else None
    return yl, yc


def gla_branch(pl, pc, w_a2, b_a, norm_g, with_ctx):
    def heads(a, d):
        return a.reshape(a.shape[0], a.shape[1], GLA_HEADS, d)

    def log_decay(low, dirn):
        z = (low @ w_a2[dirn]).astype(jnp.float32) + b_a[dirn].astype(jnp.float32)
        return heads(jax.nn.log_sigmoid(z) / GLA_TAU, GLA_DK)

    def flip(a):
        return None if a is None else a[:, ::-1]

    def finish(o, og):
        o = rmsnorm(o.astype(og.dtype), norm_g)
        return o.reshape(og.shape) * jax.nn.silu(og)

    scale = GLA_DK ** -0.5
    bsz = pl['gla_k'].shape[0]
    s0 = jnp.zeros((bsz, GLA_HEADS, GLA_DK, GLA_DV), jnp.float32)
    qc = heads(pc['gla_q'], GLA_DK) * scale if with_ctx else None
    kc, vc = heads(pc['gla_k'], GLA_DK), heads(pc['gla_v'], GLA_DV)
    oc_f, s_f = gla_scan(qc, kc, vc, log_decay(pc['gla_af'], 0), s0)
    oc_b, s_b = gla_scan(flip(qc), flip(kc), flip(vc), flip(log_decay(pc['gla_ab'], 1)), s0)
    ql = heads(pl['gla_q'], GLA_DK) * scale
    kl, vl = heads(pl['gla_k'], GLA_DK), heads(pl['gla_v'], GLA_DV)
    ol_f, _ = gla_scan(ql, kl, vl, log_decay(pl['gla_af'], 0), s_f)
    ol_b, _ = gla_scan(flip(ql), flip(kl), flip(vl), flip(log_decay(pl['gla_ab'], 1)), s_b)
    yl = finish(ol_f + flip(ol_b), pl['gla_og'])
    yc = finish(oc_f + flip(oc_b), pc['gla_og']) if with_ctx else None
    return yl, yc


def gqa_branch(pl, pc, qn_g, kn_g, cos, sin, with_ctx):
    def heads(a, n):
        return a.reshape(a.shape[0], a.shape[1], n, GQA_HEAD_DIM)
    kc = rmsnorm(heads(pc['gqa_k'], GQA_KV_HEADS), kn_g)
    vc = heads(pc['gqa_v'], GQA_KV_HEADS)
    ql = apply_rope(rmsnorm(heads(pl['gqa_q'], GQA_HEADS), qn_g), cos, sin)
    kl = apply_rope(rmsnorm(heads(pl['gqa_k'], GQA_KV_HEADS), kn_g), cos, sin)
    vl = heads(pl['gqa_v'], GQA_KV_HEADS)
    yl = gqa_blocked(ql, kl, vl, kc, vc)
    yc = gqa_dense(rmsnorm(heads(pc['gqa_q'], GQA_HEADS), qn_g), kc, vc) if with_ctx else None
    return yl, yc


def mixer(hl, hc, w_in, rpb, gla_w_a2, gla_b_a, gla_norm_g, qn_g, kn_g,
          w_pa, w_pb, w_pc, w_out, cos, sin, with_ctx):
    pl = in_proj(hl, w_in)
    pc = in_proj(hc, w_in, None if with_ctx else CTX_KV_SPLITS)
    a_l, a_c = na_branch(pl, pc, rpb, with_ctx)
    b_l, b_c = gla_branch(pl, pc, gla_w_a2, gla_b_a, gla_norm_g, with_ctx)
    c_l, c_c = gqa_branch(pl, pc, qn_g, kn_g, cos, sin, with_ctx)

    def merge(p, a, b, c):
        m = (jax.nn.sigmoid(p['gate_a']) * (a @ w_pa) + jax.nn.sigmoid(p['gate_b']) * (b @ w_pb)
             + jax.nn.sigmoid(p['gate_c']) * (c @ w_pc))
        return m @ w_out

    yl = merge(pl, a_l, b_l, c_l)
    yc = merge(pc, a_c, b_c, c_c) if with_ctx else None
    return yl, yc


def moe_ffn(h, router_w, router_b, w_gate, w_up, w_down):
    x = h.reshape(-1, D_MODEL)
    t = x.shape[0]
    n_assign = t * TOP_K
    logits = (x @ router_w).astype(jnp.float32) + router_b.astype(jnp.float32)
    top_v, top_i = lax.top_k(logits, TOP_K)
    wts = jax.nn.softmax(top_v, axis=-1)
    flat_e = top_i.reshape(-1)
    flat_t = jnp.repeat(jnp.arange(t, dtype=jnp.int32), TOP_K)
    flat_w = wts.reshape(-1)
    order = jnp.argsort(flat_e)
    se = flat_e[order]
    counts = jnp.bincount(flat_e, length=N_EXPERTS)
    starts = jnp.cumsum(counts) - counts
    padded = (counts + MOE_BLOCK - 1) // MOE_BLOCK * MOE_BLOCK
    pends = jnp.cumsum(padded)
    pstarts = pends - padded
    dest = pstarts[se] + jnp.arange(n_assign) - starts[se]
    n_blk = -(-n_assign // MOE_BLOCK) + N_EXPERTS
    slot_t = jnp.full((n_blk * MOE_BLOCK,), t, jnp.int32).at[dest].set(flat_t[order])
    slot_w = jnp.zeros((n_blk * MOE_BLOCK,), jnp.float32).at[dest].set(flat_w[order])
    block_e = jnp.minimum(jnp.searchsorted(pends, jnp.arange(n_blk) * MOE_BLOCK, side='right'), N_EXPERTS - 1)
    xp = jnp.concatenate([x, jnp.zeros((1, D_MODEL), x.dtype)], axis=0)
    xb = xp[slot_t].reshape(n_blk, MOE_BLOCK, D_MODEL)
    yb = lax.map(lambda a: swiglu(a[0], w_gate[a[1]], w_up[a[1]], w_down[a[1]]), (xb, block_e))
    y = jnp.zeros((t + 1, D_MODEL), x.dtype).at[slot_t].add(
        yb.reshape(-1, D_MODEL) * slot_w[:, None].astype(x.dtype))
    return y[:t].reshape(h.shape)


def setup_inputs(seed: int = 0) -> dict:
    key = jax.random.key(seed)
    k = jax.random.split(key, 28)
    n_dense = (DEPTH + 1) // 2
    n_moe = DEPTH // 2
    d = D_MODEL

    def nrm(i, shape, scale):
        return jax.random.normal(k[i], shape, jnp.float32) * scale

    return {
        "x": nrm(0, (BATCH, SEQ, d), 1.0),
        "c": nrm(1, (BATCH, d), 1.0),
        "ctx": nrm(2, (BATCH, CTX_LEN, d), 1.0),
        "c_ctx": nrm(3, (d,), 1.0),
        "w_ada": nrm(4, (DEPTH, d, 6 * d), 0.5 * d ** -0.5),
        "b_ada": nrm(5, (DEPTH, 6 * d), 0.01),
        "norm1_g": 1.0 + nrm(6, (DEPTH, d), 0.05),
        "norm2_g": 1.0 + nrm(7, (DEPTH, d), 0.05),
        "w_in": nrm(8, (DEPTH, d, D_IN), d ** -0.5),
        "na_rpb": nrm(9, (DEPTH, NA_HEADS, 2 * NA_WIN_ROWS - 1, 2 * NA_WIN_COLS - 1), 0.1),
        "gla_w_a2": nrm(10, (DEPTH, 2, GLA_GATE_RANK, GLA_KW), GLA_GATE_RANK ** -0.5),
        "gla_b_a": nrm(11, (DEPTH, 2, GLA_KW), 0.1),
        "gla_norm_g": 1.0 + nrm(12, (DEPTH, GLA_DV), 0.05),
        "gqa_qn_g": 1.0 + nrm(13, (DEPTH, GQA_HEAD_DIM), 0.05),
        "gqa_kn_g": 1.0 + nrm(14, (DEPTH, GQA_HEAD_DIM), 0.05),
        "w_pa": nrm(15, (DEPTH, BRANCH_W, d), BRANCH_W ** -0.5),
        "w_pb": nrm(16, (DEPTH, BRANCH_W, d), BRANCH_W ** -0.5),
        "w_pc": nrm(17, (DEPTH, BRANCH_W, d), BRANCH_W ** -0.5),
        "w_out": nrm(18, (DEPTH, d, d), d ** -0.5),
        "dense_w_gate": nrm(19, (n_dense, d, D_FF), d ** -0.5),
        "dense_w_up": nrm(20, (n_dense, d, D_FF), d ** -0.5),
        "dense_w_down": nrm(21, (n_dense, D_FF, d), D_FF ** -0.5),
        "router_w": nrm(22, (n_moe, d, N_EXPERTS), d ** -0.5),
        "router_b": nrm(23, (n_moe, N_EXPERTS), 0.01),
        "moe_w_gate": nrm(24, (n_moe, N_EXPERTS, d, D_FF), d ** -0.5),
        "moe_w_up": nrm(25, (n_moe, N_EXPERTS, d, D_FF), d ** -0.5),
        "moe_w_down": nrm(26, (n_moe, N_EXPERTS, D_FF, d), D_FF ** -0.5),
        "final_norm_g": 1.0 + nrm(27, (d,), 0.05),
    }


def reference(x, c, ctx, c_ctx, w_ada, b_ada, norm1_g, norm2_g, w_in, na_rpb, gla_w_a2, gla_b_a,
              gla_norm_g, gqa_qn_g, gqa_kn_g, w_pa, w_pb, w_pc, w_out, dense_w_gate, dense_w_up,
              dense_w_down, router_w, router_b, moe_w_gate, moe_w_up, moe_w_down, final_norm_g):
    cos, sin = axial_rope_tables(x.shape[1])
    xl, xc = x, ctx
    c_act = jax.nn.silu(c)
    cc_act = jax.nn.silu(c_ctx)
    for i in range(DEPTH):
        last = i == DEPTH - 1
        mod_l = (c_act @ w_ada[i] + b_ada[i])[:, None, :]
        mod_c = cc_act @ w_ada[i] + b_ada[i]
        sh1, sc1, g1, sh2, sc2, g2 = jnp.split(mod_l, 6, axis=-1)
        csh1, csc1, cg1, csh2, csc2, cg2 = jnp.split(mod_c, 6, axis=-1)
        hl = rmsnorm(xl, norm1_g[i]) * (1.0 + sc1) + sh1
        hc = rmsnorm(xc, norm1_g[i]) * (1.0 + csc1) + csh1
        yl, yc = mixer(hl, hc, w_in[i], na_rpb[i], gla_w_a2[i], gla_b_a[i], gla_norm_g[i],
                       gqa_qn_g[i], gqa_kn_g[i], w_pa[i], w_pb[i], w_pc[i], w_out[i], cos, sin,
                       not last)
        xl = xl + g1 * yl
        j = i // 2
        if i % 2 == 0:
            ffn = lambda h: swiglu(h, dense_w_gate[j], dense_w_up[j], dense_w_down[j])
        else:
            ffn = lambda h: moe_ffn(h, router_w[j], router_b[j], moe_w_gate[j], moe_w_up[j], moe_w_down[j])
        xl = xl + g2 * ffn(rmsnorm(xl, norm2_g[i]) * (1.0 + sc2) + sh2)
        if not last:
            xc = xc + cg1 * yc
            xc = xc + cg2 * ffn(rmsnorm(xc, norm2_g[i]) * (1.0 + csc2) + csh2)
    return rmsnorm(xl, final_norm_g)
```

```python
import numpy as np
import ml_dtypes
from contextlib import ExitStack
import concourse.bass as bass
import concourse.mybir as mybir
from concourse.bass_utils import run_bass_kernel_spmd

F32 = mybir.dt.float32
BF16 = mybir.dt.bfloat16
AF = mybir.ActivationFunctionType
ALU = mybir.AluOpType
AX = mybir.AxisListType

D = 2048
DC = 16
S_LAT = 2048
L_CTX = 256
T = S_LAT + L_CTX
TT = T // 128
D_IN = 13856
D_FF = 5632
FC = D_FF // 128
NE = 8
EPS = 1e-6
NEG = -30000.0
BLK5 = [(0, 512, 0), (512, 512, 0), (1024, 512, 0), (1536, 512, 0), (2048, 256, 1)]


class Sem:
    def __init__(self, h):
        self.h = h
        self.n = 0


class Buf:
    __slots__ = ("name", "w", "r", "dsem")

    def __init__(self, name=""):
        self.name = name
        self.w = None
        self.r = []
        self.dsem = None


class Prog:
    ENG = ("pe", "act", "dve", "pool", "sp")
    ENGN = {"pe": "tensor", "act": "scalar", "dve": "vector", "pool": "gpsimd", "sp": "sync"}

    def __init__(self, nc, stack, n_dma_sems=90):
        self.nc = nc
        self.ops = {e: [] for e in self.ENG}
        self.esem = {e: Sem(stack.enter_context(nc.semaphore("es_" + e))) for e in self.ENG}
        self.seen = {e: {} for e in self.ENG}
        self.free_dsems = [Sem(stack.enter_context(nc.semaphore("ds%d" % i))) for i in range(n_dma_sems)]
        self.stage_bufs = []
        self.bar = Sem(stack.enter_context(nc.semaphore("bar")))
        self.n_ops = 0

    def buf(self, name=""):
        b = Buf(name)
        self.stage_bufs.append(b)
        return b

    def bufs(self, n, name=""):
        return [self.buf("%s%d" % (name, i)) for i in range(n)]

    def _dsem(self, b):
        if b.dsem is None:
            b.dsem = self.free_dsems.pop()
        return b.dsem

    def _waits_for(self, eng, reads, writes):
        need = {}

        def add(m):
            if m is None:
                return
            s, v = m
            if need.get(id(s), (s, 0))[1] < v:
                need[id(s)] = (s, v)
        for b in reads:
            add(b.w)
        for b in writes:
            add(b.w)
            for m in b.r:
                add(m)
        out = []
        seen = self.seen[eng]
        for s, v in need.values():
            if s is self.esem[eng] and eng == "pe":
                continue
            if seen.get(id(s), 0) >= v:
                continue
            seen[id(s)] = v
            out.append((s.h, v))
        return out

    def op(self, eng, emit, reads=(), writes=()):
        waits = self._waits_for(eng, reads, writes)
        s = self.esem[eng]
        s.n += 1
        mark = (s, s.n)
        self.ops[eng].append((waits, emit, [(s.h, 1)]))
        for b in reads:
            b.r.append(mark)
            if len(b.r) > 24:
                b.r = b.r[-24:] if False else b.r
        for b in writes:
            b.w = mark
            b.r = []
        self.n_ops += 1

    def dma(self, q, emit, reads=(), writes=(), n=1):
        waits = self._waits_for(q, reads, writes)
        prim = writes[0] if len(writes) else reads[0]
        s = self._dsem(prim)
        s.n += 16 * n
        mark = (s, s.n)
        self.ops[q].append((waits, emit, [(s.h, 16)]))
        for b in reads:
            b.r.append(mark)
        for b in writes:
            b.w = mark
            b.r = []
        self.n_ops += 1

    def barrier(self):
        waits = []
        for e in self.ENG:
            if e != "sp" and self.esem[e].n > 0:
                waits.append((self.esem[e].h, self.esem[e].n))
        for b in self.stage_bufs:
            if b.dsem is not None:
                waits.append((b.dsem.h, b.dsem.n))
        self.bar.n += 1
        v = self.bar.n
        self.ops["sp"].append((waits, lambda e: e.nop(), [(self.bar.h, 1)]))
        for e in self.ENG:
            if e != "sp":
                self.ops[e].append(([(self.bar.h, v)], None, []))
        for b in self.stage_bufs:
            if b.dsem is not None:
                self.free_dsems.append(b.dsem)
                b.dsem = None
            b.w = None
            b.r = []
        self.stage_bufs = []
        for e in self.ENG:
            self.seen[e] = {}

    def flush(self):
        nc = self.nc
        with nc.Block() as block:
            for e in self.ENG:
                ops = self.ops[e]

                def body(eng, ops=ops):
                    for waits, emit, incs in ops:
                        for (sh, v) in waits:
                            eng.wait_ge(sh, v)
                        if emit is None:
                            continue
                        r = emit(eng)
                        rs = r if isinstance(r, (list, tuple)) else [r]
                        for ins in rs:
                            for (sh, amt) in incs:
                                ins.then_inc(sh, amt)
                getattr(block, self.ENGN[e])(body)
        self.ops = {e: [] for e in self.ENG}


class Ring:
    def __init__(self, tiles, bufs):
        self.t = tiles
        self.b = bufs
        self.i = 0

    def next(self):
        k = self.i % len(self.t)
        self.i += 1
        return self.t[k], self.b[k]


class Stage:
    def __init__(self, B, name):
        self.B = B
        self.name = name
        self.st = ExitStack()

    def __enter__(self):
        self.st.__enter__()
        return self

    def __exit__(self, *a):
        self.B.P.barrier()
        self.B.P.flush()
        return self.st.__exit__(*a)

    def sb(self, shape, dt, name=None):
        B = self.B
        B.uid += 1
        t = self.st.enter_context(B.nc.sbuf_tensor("%s_%s%d" % (self.name, name or "t", B.uid), list(shape), dt))
        return t, B.P.buf()

    def ps(self, shape, dt=F32, name=None):
        B = self.B
        B.uid += 1
        t = self.st.enter_context(B.nc.psum_tensor("%s_%s%d" % (self.name, name or "p", B.uid), list(shape), dt))
        return t, B.P.buf()

    def ring_sb(self, n, shape, dt, name=None):
        ts = [self.sb(shape, dt, name) for _ in range(n)]
        return Ring([t for t, _ in ts], [b for _, b in ts])

    def ring_ps(self, n, shape, dt=F32, name=None):
        ts = [self.ps(shape, dt, name) for _ in range(n)]
        return Ring([t for t, _ in ts], [b for _, b in ts])


class Builder:
    def __init__(self, cfg):
        self.cfg = cfg
        self.dbg = set(cfg.get("debug", ()))
        self.nc = bass.Bass("TRN2", target_bir_lowering=False)
        self.gst = ExitStack()
        self.gst.__enter__()
        self.P = Prog(self.nc, self.gst)
        self.uid = 0
        self.inputs = {}
        self.outputs = []

    def din(self, name, shape, dt=F32):
        ap = self.nc.dram_tensor(name, list(shape), dt, kind="ExternalInput").ap()
        self.inputs[name] = ap
        return ap

    def dscr(self, name, shape, dt):
        if name in self.dbg:
            self.outputs.append(name)
            return self.nc.dram_tensor(name, list(shape), dt, kind="ExternalOutput").ap()
        return self.nc.dram_tensor(name, list(shape), dt).ap()

    def gsb(self, name, shape, dt):
        return self.gst.enter_context(self.nc.sbuf_tensor(name, list(shape), dt))

    def stage(self, name):
        return Stage(self, name)

    def declare(self):
        c = self.cfg
        L = c["layers"]
        self.x_in = self.din("x", [S_LAT, D])
        self.ctx_in = self.din("ctx", [L_CTX, D])
        self.c_in = self.din("c", [1, D])
        self.cctx_in = self.din("c_ctx", [1, D])
        self.ident_in = self.din("k_ident", [128, 128])
        self.tri_in = self.din("k_tri", [4, 128, 128])
        self.cs_in = self.din("k_cs", [T, 512])
        self.mask_in = self.din("k_namask", [5, 128, 576])
        self.sel_in = self.din("k_sel", [8, 8 * 128])
        self.w_ada = self.din("w_ada", [2, D, 6 * D])
        self.b_ada = self.din("b_ada", [2, 6 * D])
        self.norm1_g = self.din("norm1_g", [2, D])
        self.norm2_g = self.din("norm2_g", [2, D])
        self.final_g = self.din("final_norm_g", [1, D])
        self.gqa_qn = self.din("gqa_qn_g", [2, 128])
        self.gqa_kn = self.din("gqa_kn_g", [2, 128])
        self.gla_ng = self.din("gla_norm_g", [2, 256])
        self.w_in = {}; self.rpb = {}; self.w_a2 = {}; self.b_a = {}
        self.w_pa = {}; self.w_pb = {}; self.w_pc = {}; self.w_out = {}
        for l in (L if c.get("mixer", True) else []):
            self.w_in[l] = self.din("w_in%d" % l, [D, D_IN])
            self.rpb[l] = self.din("na_rpb%d" % l, [16, 15 * 31])
            self.w_a2[l] = self.din("gla_w_a2_%d" % l, [2, 16, 512])
            self.b_a[l] = self.din("gla_b_a%d" % l, [2, 512])
            self.w_pa[l] = self.din("w_pa%d" % l, [1024, D])
            self.w_pb[l] = self.din("w_pb%d" % l, [1024, D])
            self.w_pc[l] = self.din("w_pc%d" % l, [1024, D])
            self.w_out[l] = self.din("w_out%d" % l, [D, D])
        if 0 in L and c.get("ffn", True):
            self.dw_gate = self.din("dense_w_gate", [D, D_FF])
            self.dw_up = self.din("dense_w_up", [D, D_FF])
            self.dw_down = self.din("dense_w_down", [D_FF, D])
        if 1 in L and c.get("ffn", True):
            self.router_w = self.din("router_w", [D, NE])
            self.router_b = self.din("router_b", [1, NE])
            self.mw_gate = self.din("moe_w_gate", [NE, D, D_FF])
            self.mw_up = self.din("moe_w_up", [NE, D, D_FF])
            self.mw_down = self.din("moe_w_down", [NE, D_FF, D])
        self.XT = self.dscr("XT", [DC, 128, T], F32)
        self.FM = self.dscr("FM", [72, 128, T], BF16)
        self.LOW = self.dscr("LOW", [32, T], F32)
        self.TM = self.dscr("TM", [T, 5120], BF16)
        self.AT = self.dscr("AT", [8, 128, T], BF16)
        self.BT = self.dscr("BT", [8, 128, T], BF16)
        self.CT = self.dscr("CT", [8, 128, T], BF16)
        self.MT = self.dscr("MT", [DC, 128, T], BF16)
        self.OF = self.dscr("OF", [T, 1024], F32)
        self.FB = self.dscr("FB", [16 * 18 * 64 * 96], F32)
        self.MODT = self.dscr("MODT", [2, 128, 96 * 2], F32)
        self.GATE = self.dscr("GATE", [8, S_LAT], F32)
        self.OUT = self.nc.dram_tensor("out", [S_LAT, D], F32, kind="ExternalOutput").ap()
        self.outputs.append("out")
        self.ident_f = self.gsb("ident_f", [128, 128], F32)
        self.ident_b = self.gsb("ident_b", [128, 128], BF16)
        self.ones_f = self.gsb("ones_f", [128, 128], F32)
        self.cactT = self.gsb("cactT", [128, 32], F32)
        self.modT = [self.gsb("modT%d" % l, [128, 96, 2], F32) for l in range(2)]
        self.A1 = [self.gsb("A1_%d" % l, [128, 16, 2], F32) for l in range(2)]
        self.A2 = [self.gsb("A2_%d" % l, [128, 16, 2], F32) for l in range(2)]
        self.gT = self.gsb("gT", [128, 80], F32)
        self.gbc = self.gsb("gbc", [128, 1024], F32)

    def stage_prep(self):
        P = self.P
        nc = self.nc
        with self.stage("prep") as S:
            b_id = P.buf()
            P.dma("sp", lambda e: e.dma_start(out=self.ident_f[:], in_=self.ident_in), writes=[b_id])
            P.op("dve", lambda e: e.tensor_copy(self.ident_b[:], self.ident_f[:]), reads=[b_id], writes=[P.buf()])
            b_ones = P.buf()
            P.op("pool", lambda e: e.memset(self.ones_f[:], 1.0), writes=[b_ones])
            xs_r = S.ring_sb(2, [128, D], F32, "xs")
            xo_r = S.ring_sb(2, [128, DC, 128], F32, "xo")
            pt_r = S.ring_ps(2, [128, 4, 128], F32, "pt")
            k = 0
            for tt in range(TT):
                src = self.x_in[tt * 128:(tt + 1) * 128, :] if tt < 16 else self.ctx_in[(tt - 16) * 128:(tt - 15) * 128, :]
                xs, bxs = xs_r.next()
                xo, bxo = xo_r.next()
                P.dma("sp", lambda e, xs=xs, src=src: e.dma_start(out=xs[:], in_=src), writes=[bxs])
                for g in range(4):
                    pt, bpt = pt_r.next()
                    for j in range(4):
                        cc = 4 * g + j
                        P.op("pe", lambda e, pt=pt, xs=xs, j=j, cc=cc: e.transpose(pt[:, j, :], xs[:, cc * 128:(cc + 1) * 128], self.ident_f[:]),
                             reads=[bxs, b_id], writes=[bpt])
                    if k % 2 == 0:
                        P.op("dve", lambda e, pt=pt, xo=xo, g=g: e.tensor_copy(xo[:, 4 * g:4 * g + 4, :], pt[:]), reads=[bpt], writes=[bxo])
                    else:
                        P.op("act", lambda e, pt=pt, xo=xo, g=g: e.copy(xo[:, 4 * g:4 * g + 4, :], pt[:]), reads=[bpt], writes=[bxo])
                    k += 1
                dst = self.XT[:, :, tt * 128:(tt + 1) * 128].rearrange("c p t -> p c t")
                P.dma("sp", lambda e, xo=xo, dst=dst: e.dma_start(out=dst, in_=xo[:]), reads=[bxo])
            cc_t, bcc = S.sb([32, 128], F32, "cc")
            P.dma("sp", lambda e: e.dma_start(out=cc_t[0:16, :], in_=self.c_in.rearrange("o (c p) -> (o c) p", p=128)), writes=[bcc])
            P.dma("sp", lambda e: e.dma_start(out=cc_t[16:32, :], in_=self.cctx_in.rearrange("o (c p) -> (o c) p", p=128)), writes=[bcc])
            cs_t, bcs = S.sb([32, 128], F32, "cs")
            P.op("act", lambda e: e.activation(cs_t[:], cc_t[:], AF.Silu), reads=[bcc], writes=[bcs])
            pm, bpm = S.ps([128, 128], F32, "pm")
            P.op("pe", lambda e: e.transpose(pm[:, 0:32], cs_t[:], self.ident_f[0:32, 0:32]), reads=[bcs, b_id], writes=[bpm])
            b_cact = P.buf()
            P.op("dve", lambda e: e.tensor_copy(self.cactT[:], pm[:, 0:32]), reads=[bpm], writes=[b_cact])
            gr, bgr = S.sb([80, 128], F32, "gr")
            srcs = [self.norm1_g[0:1, :], self.norm2_g[0:1, :], self.norm1_g[1:2, :], self.norm2_g[1:2, :], self.final_g[0:1, :]]
            for v, sap in enumerate(srcs):
                P.dma("sp", lambda e, v=v, sap=sap: e.dma_start(out=gr[v * 16:(v + 1) * 16, :], in_=sap.rearrange("o (c p) -> (o c) p", p=128)), writes=[bgr])
            P.op("pe", lambda e: e.transpose(pm[:, 0:80], gr[:], self.ident_f[0:80, 0:80]), reads=[bgr, b_id], writes=[bpm])
            b_gT = P.buf()
            P.op("dve", lambda e: e.tensor_copy(self.gT[:], pm[:, 0:80]), reads=[bpm], writes=[b_gT])
            grow, bgrow = S.sb([1, 1024], F32, "grow")
            rs = [(self.gqa_qn[0:1, :], 0, 128), (self.gqa_kn[0:1, :], 128, 128), (self.gqa_qn[1:2, :], 256, 128),
                  (self.gqa_kn[1:2, :], 384, 128), (self.gla_ng[0:1, :], 512, 256), (self.gla_ng[1:2, :], 768, 256)]
            for sap, o, n in rs:
                P.dma("sp", lambda e, sap=sap, o=o, n=n: e.dma_start(out=grow[0:1, o:o + n], in_=sap), writes=[bgrow])
            pb, bpb = S.ps([128, 512], F32, "pb")
            b_gbc = P.buf()
            for hh in range(2):
                P.op("pe", lambda e, hh=hh: e.matmul(pb[:], self.ones_f[0:1, :], grow[0:1, hh * 512:(hh + 1) * 512], start=True, stop=True),
                     reads=[bgrow, b_ones], writes=[bpb])
                P.op("dve", lambda e, hh=hh: e.tensor_copy(self.gbc[:, hh * 512:(hh + 1) * 512], pb[:]), reads=[bpb], writes=[b_gbc])
            wt_r = S.ring_sb(2, [128, DC, 512], F32, "wada")
            pmod_r = S.ring_ps(2, [128, 4, 2], F32, "pmod")
            bad, bbad = S.sb([96, 128], F32, "bad")
            badT, bbadT = S.sb([128, 96], F32, "badT")
            for l in range(2):
                P.dma("sp", lambda e, l=l: e.dma_start(out=bad[:], in_=self.b_ada[l:l + 1, :].rearrange("o (c p) -> (o c) p", p=128)), writes=[bbad])
                P.op("pe", lambda e: e.transpose(pm[:, 0:96], bad[:], self.ident_f[0:96, 0:96]), reads=[bbad, b_id], writes=[bpm])
                P.op("dve", lambda e: e.tensor_copy(badT[:], pm[:, 0:96]), reads=[bpm], writes=[bbadT])
                b_mod = P.buf()
                wv = self.w_ada[l].rearrange("(k p) n -> p k n", p=128)
                for g in range(24):
                    wt, bwt = wt_r.next()
                    P.dma("sp", lambda e, wt=wt, g=g, wv=wv: e.dma_start(out=wt[:], in_=wv[:, :, g * 512:(g + 1) * 512]), writes=[bwt])
                    pmod, bpmod = pmod_r.next()
                    for j in range(4):
                        for kk in range(DC):
                            P.op("pe", lambda e, pmod=pmod, wt=wt, j=j, kk=kk: e.matmul(pmod[:, j, :], wt[:, kk, j * 128:(j + 1) * 128], self.cactT[:, kk:32:16],
                                                                                       start=(kk == 0), stop=(kk == DC - 1)),
                                 reads=[bwt, b_cact], writes=[bpmod])
                    for w in range(2):
                        P.op("dve", lambda e, pmod=pmod, g=g, w=w, l=l: e.tensor_tensor(out=self.modT[l][:, 4 * g:4 * g + 4, w], in0=pmod[:, :, w],
                                                                                          in1=badT[:, 4 * g:4 * g + 4], op=ALU.add),
                             reads=[bpmod, bbadT], writes=[b_mod])
                for w in range(2):
                    P.op("dve", lambda e, l=l, w=w: e.scalar_tensor_tensor(out=self.A1[l][:, :, w], in0=self.modT[l][:, 16:32, w], scalar=1.0,
                                                                           in1=self.gT[:, (2 * l) * 16:(2 * l + 1) * 16], op0=ALU.add, op1=ALU.mult),
                         reads=[b_mod, b_gT], writes=[P.buf()])
                    P.op("dve", lambda e, l=l, w=w: e.scalar_tensor_tensor(out=self.A2[l][:, :, w], in0=self.modT[l][:, 64:80, w], scalar=1.0,
                                                                           in1=self.gT[:, (2 * l + 1) * 16:(2 * l + 2) * 16], op0=ALU.add, op1=ALU.mult),
                         reads=[b_mod, b_gT], writes=[P.buf()])
                if "MODT" in self.dbg:
                    P.dma("sp", lambda e, l=l: e.dma_start(out=self.MODT[l], in_=self.modT[l][:].rearrange("p a b -> p (a b)")), reads=[b_mod])

    def norm_blocks(self, S, hT, b_hT, Acol, shcol, tok0=0, ntok=T, nring=2, gain_only=None, cb=None, want32=False):
        P = self.P
        xb_r = S.ring_sb(nring, [128, DC, 256], F32, "xb")
        sq_r = S.ring_sb(2, [128, 256], F32, "sq")
        tmp_r = S.ring_sb(2, [128, 256], F32, "tmp")
        rstd_r = S.ring_sb(2, [128, 256], F32, "rstd")
        ss_r = S.ring_ps(1, [128, 256], F32, "ss")
        h32_r = S.ring_sb(2, [128, DC, 256], F32, "h32") if cb is not None else None
        for bi in range(ntok // 256):
            t0 = tok0 + bi * 256
            o0 = bi * 256
            w = 0 if t0 < S_LAT else 1
            xb, bxb = xb_r.next()
            P.dma("sp", lambda e, xb=xb, t0=t0: e.dma_start(out=xb[:], in_=self.XT[:, :, t0:t0 + 256].rearrange("c p t -> p c t")), writes=[bxb])
            ss, bss = ss_r.next()
            for c in range(DC):
                sq, bsq = sq_r.next()
                P.op("act", lambda e, sq=sq, xb=xb, c=c: e.activation(sq[:], xb[:, c, :], AF.Square), reads=[bxb], writes=[bsq])
                P.op("pe", lambda e, ss=ss, sq=sq, c=c: e.matmul(ss[:], self.ones_f[:], sq[:], start=(c == 0), stop=(c == DC - 1)), reads=[bsq], writes=[bss])
            rstd, brs = rstd_r.next()
            P.op("dve", lambda e, rstd=rstd, ss=ss: e.tensor_scalar(rstd[:], ss[:], 1.0 / D, EPS, ALU.mult, ALU.add), reads=[bss], writes=[brs])
            P.op("act", lambda e, rstd=rstd: e.sqrt(rstd[:], rstd[:]), reads=[brs], writes=[brs])
            P.op("dve", lambda e, rstd=rstd: e.reciprocal(rstd[:], rstd[:]), reads=[brs], writes=[brs])
            if cb is not None:
                h32, b_h32 = h32_r.next()
            for c in range(DC):
                tmp, btmp = tmp_r.next()
                P.op("pool", lambda e, tmp=tmp, xb=xb, c=c, rstd=rstd: e.tensor_tensor(out=tmp[:], in0=xb[:, c, :], in1=rstd[:], op=ALU.mult),
                     reads=[bxb, brs], writes=[btmp])
                if gain_only is not None:
                    P.op("dve", lambda e, tmp=tmp, c=c, h32=h32: e.tensor_scalar_mul(h32[:, c, :], tmp[:], gain_only[:, c:c + 1]), reads=[btmp], writes=[b_h32])
                    continue
                if hT is not None:
                    P.op("dve", lambda e, tmp=tmp, c=c, w=w, o0=o0: e.tensor_scalar(hT[:, c, o0:o0 + 256], tmp[:], Acol[:, c, w:w + 1], shcol[:, c, w:w + 1], ALU.mult, ALU.add),
                         reads=[btmp], writes=[b_hT])
                if cb is not None:
                    P.op("act", lambda e, tmp=tmp, c=c, w=w, h32=h32: e.activation(h32[:, c, :], tmp[:], AF.Identity, bias=shcol[:, c, w:w + 1], scale=Acol[:, c, w:w + 1]),
                         reads=[btmp], writes=[b_h32])
            if cb is not None:
                cb(bi, t0, h32, b_h32)

    def stage_inproj(self, l):
        P = self.P
        with self.stage("ip%d" % l) as S:
            hT, b_hT = S.sb([128, DC, T], BF16, "hT")
            self.norm_blocks(S, hT, b_hT, self.A1[l], self.modT[l][:, 0:16, :])
            wv = self.w_in[l].rearrange("(k p) n -> p k n", p=128)
            wt_r = S.ring_sb(2, [128, DC, 512], BF16, "wt")
            ps_r = S.ring_ps(4, [128, 512], F32, "ps")
            st_r = S.ring_sb(2, [128, T], BF16, "stf")
            fm_groups = [(0, 4, 0, False), (3072, 2, 16, False), (7712, 12, 24, True)]
            k = 0
            for (c0, ng, f0, sig) in fm_groups:
                for g in range(ng):
                    wt, bwt = wt_r.next()
                    P.dma("pool", lambda e, wt=wt, c0=c0, g=g: e.dma_start(out=wt[:], in_=wv[:, :, c0 + g * 512:c0 + (g + 1) * 512]), writes=[bwt])
                    for j in range(4):
                        st, bst = st_r.next()
                        for (t0, nb, w) in BLK5:
                            ps, bps = ps_r.next()
                            for kk in range(DC):
                                P.op("pe", lambda e, ps=ps, wt=wt, j=j, kk=kk, t0=t0, nb=nb: e.matmul(ps[:, 0:nb], wt[:, kk, j * 128:(j + 1) * 128], hT[:, kk, t0:t0 + nb],
                                                                                                    start=(kk == 0), stop=(kk == DC - 1)),
                                     reads=[bwt, b_hT], writes=[bps])
                            if sig:
                                P.op("act", lambda e, ps=ps, st=st, t0=t0, nb=nb: e.activation(st[:, t0:t0 + nb], ps[:, 0:nb], AF.Sigmoid), reads=[bps], writes=[bst])
                            elif k % 2 == 0:
                                P.op("dve", lambda e, ps=ps, st=st, t0=t0, nb=nb: e.tensor_copy(st[:, t0:t0 + nb], ps[:, 0:nb]), reads=[bps], writes=[bst])
                            else:
                                P.op("act", lambda e, ps=ps, st=st, t0=t0, nb=nb: e.copy(st[:, t0:t0 + nb], ps[:, 0:nb]), reads=[bps], writes=[bst])
                            k += 1
                        fi = f0 + g * 4 + j
                        P.dma("sp", lambda e, st=st, fi=fi: e.dma_start(out=self.FM[fi], in_=st[:]), reads=[bst])
            wl, bwl = S.sb([128, DC, 32], BF16, "wl")
            P.dma("pool", lambda e: e.dma_start(out=wl[:], in_=wv[:, :, 6144:6176]), writes=[bwl])
            lo, blo = S.sb([32, T], F32, "lo")
            for (t0, nb, w) in BLK5:
                ps, bps = ps_r.next()
                for kk in range(DC):
                    P.op("pe", lambda e, ps=ps, kk=kk, t0=t0, nb=nb: e.matmul(ps[0:32, 0:nb], wl[:, kk, :], hT[:, kk, t0:t0 + nb], start=(kk == 0), stop=(kk == DC - 1)),
                         reads=[bwl, b_hT], writes=[bps])
                P.op("dve", lambda e, ps=ps, t0=t0, nb=nb: e.tensor_copy(lo[:, t0:t0 + nb], ps[0:32, 0:nb]), reads=[bps], writes=[blo])
            P.dma("sp", lambda e: e.dma_start(out=self.LOW, in_=lo[:]), reads=[blo])
            tm_groups = [(2048, 0, "c"), (2560, 512, "c"), (3584, 1024, "c"), (4096, 1536, "c"), (4608, 2048, "c"),
                         (5120, 2560, "c"), (5632, 3072, "c"), (6176, 3584, "q"), (6688, 4096, "q"), (7200, 4608, "kv")]
            stt_r = S.ring_sb(3, [128, 512], BF16, "stt")
            cs_r = S.ring_sb(2, [128, 512], F32, "cs")
            sq_r = S.ring_sb(2, [128, 512], F32, "sq2")
            qn_r = S.ring_sb(2, [128, 512], F32, "qn")
            t1_r = S.ring_sb(2, [128, 256], F32, "t1")
            t2_r = S.ring_sb(2, [128, 256], F32, "t2")
            ss_r = S.ring_sb(2, [128, 4], F32, "ssq")
            qoff = 256 * l
            for (c0, off, kind) in tm_groups:
                wt, bwt = wt_r.next()
                P.dma("pool", lambda e, wt=wt, c0=c0: e.dma_start(out=wt[:], in_=wv[:, :, c0:c0 + 512]), writes=[bwt])
                for tt in range(TT):
                    ps, bps = ps_r.next()
                    for kk in range(DC):
                        P.op("pe", lambda e, ps=ps, wt=wt, kk=kk, tt=tt: e.matmul(ps[:], hT[:, kk, tt * 128:(tt + 1) * 128], wt[:, kk, :], start=(kk == 0), stop=(kk == DC - 1)),
                             reads=[bwt, b_hT], writes=[bps])
                    st, bst = stt_r.next()
                    if kind == "c":
                        if k % 2 == 0:
                            P.op("dve", lambda e, ps=ps, st=st: e.tensor_copy(st[:], ps[:]), reads=[bps], writes=[bst])
                        else:
                            P.op("act", lambda e, ps=ps, st=st: e.copy(st[:], ps[:]), reads=[bps], writes=[bst])
                        k += 1
                    else:
                        nh = 4 if kind == "q" else 2
                        gb = self.gbc[:, qoff:qoff + 128] if kind == "q" else self.gbc[:, qoff + 128:qoff + 256]
                        nw = nh * 128
                        cs, bcs = cs_r.next()
                        P.dma("sp", lambda e, cs=cs, tt=tt: e.dma_start(out=cs[:], in_=self.cs_in[tt * 128:(tt + 1) * 128, :]), writes=[bcs])
                        sq, bsq = sq_r.next()
                        P.op("act", lambda e, sq=sq, ps=ps, nw=nw: e.activation(sq[:, 0:nw], ps[:, 0:nw], AF.Square), reads=[bps], writes=[bsq])
                        ssq, bssq = ss_r.next()
                        P.op("dve", lambda e, ssq=ssq, sq=sq, nh=nh, nw=nw: e.tensor_reduce(out=ssq[:, 0:nh], in_=sq[:, 0:nw].rearrange("p (h d) -> p h d", h=nh), axis=AX.X, op=ALU.add),
                             reads=[bsq], writes=[bssq])
                        P.op("dve", lambda e, ssq=ssq, nh=nh: e.tensor_scalar(ssq[:, 0:nh], ssq[:, 0:nh], 1.0 / 128, EPS, ALU.mult, ALU.add), reads=[bssq], writes=[bssq])
                        P.op("act", lambda e, ssq=ssq, nh=nh: e.sqrt(ssq[:, 0:nh], ssq[:, 0:nh]), reads=[bssq], writes=[bssq])
                        P.op("dve", lambda e, ssq=ssq, nh=nh: e.reciprocal(ssq[:, 0:nh], ssq[:, 0:nh]), reads=[bssq], writes=[bssq])
                        qn, bqn = qn_r.next()
                        for h in range(nh):
                            P.op("dve", lambda e, qn=qn, ps=ps, ssq=ssq, h=h, gb=gb: e.scalar_tensor_tensor(out=qn[:, h * 128:(h + 1) * 128], in0=ps[:, h * 128:(h + 1) * 128],
                                                                                                           scalar=ssq[:, h:h + 1], in1=gb, op0=ALU.mult, op1=ALU.mult),
                                 reads=[bps, bssq], writes=[bqn])
                        qv = qn[:, 0:nw].rearrange("p (h i two) -> p h i two", h=nh, two=2)
                        ev, od = qv[:, :, :, 0], qv[:, :, :, 1]
                        cosv = cs[:, 0:nh * 64].rearrange("p (h i) -> p h i", h=nh)
                        sinv = cs[:, 256:256 + nh * 64].rearrange("p (h i) -> p h i", h=nh)
                        sv = st[:, 0:nw].rearrange("p (h i two) -> p h i two", h=nh, two=2)
                        t1, bt1 = t1_r.next()
                        t2, bt2 = t2_r.next()
                        t1v = t1[:, 0:nh * 64].rearrange("p (h i) -> p h i", h=nh)
                        t2v = t2[:, 0:nh * 64].rearrange("p (h i) -> p h i", h=nh)
                        P.op("pool", lambda e, t1v=t1v, ev=ev, cosv=cosv: e.tensor_tensor(out=t1v, in0=ev, in1=cosv, op=ALU.mult), reads=[bqn, bcs], writes=[bt1])
                        P.op("pool", lambda e, t2v=t2v, od=od, sinv=sinv: e.tensor_tensor(out=t2v, in0=od, in1=sinv, op=ALU.mult), reads=[bqn, bcs], writes=[bt2])
                        P.op("dve", lambda e, sv=sv, t1v=t1v, t2v=t2v: e.tensor_tensor(out=sv[:, :, :, 0], in0=t1v, in1=t2v, op=ALU.subtract), reads=[bt1, bt2], writes=[bst])
                        t1, bt1 = t1_r.next()
                        t2, bt2 = t2_r.next()
                        t1v = t1[:, 0:nh * 64].rearrange("p (h i) -> p h i", h=nh)
                        t2v = t2[:, 0:nh * 64].rearrange("p (h i) -> p h i", h=nh)
                        P.op("pool", lambda e, t1v=t1v, ev=ev, sinv=sinv: e.tensor_tensor(out=t1v, in0=ev, in1=sinv, op=ALU.mult), reads=[bqn, bcs], writes=[bt1])
                        P.op("pool", lambda e, t2v=t2v, od=od, cosv=cosv: e.tensor_tensor(out=t2v, in0=od, in1=cosv, op=ALU.mult), reads=[bqn, bcs], writes=[bt2])
                        P.op("dve", lambda e, sv=sv, t1v=t1v, t2v=t2v: e.tensor_tensor(out=sv[:, :, :, 1], in0=t1v, in1=t2v, op=ALU.add), reads=[bt1, bt2], writes=[bst])
                        if kind == "kv":
                            P.op("act", lambda e, ps=ps, st=st: e.copy(st[:, 256:512], ps[:, 256:512]), reads=[bps], writes=[bst])
                    P.dma("sp", lambda e, st=st, tt=tt, off=off: e.dma_start(out=self.TM[tt * 128:(tt + 1) * 128, off:off + 512], in_=st[:]), reads=[bst])

    def attn_res(self, S, nkmax, nch, dh):
        R = {}
        R["ps_s"] = S.ring_ps(2, [128, 512], F32, "pss")
        R["sc"] = S.ring_sb(2, [128, nkmax], F32, "sc")
        R["p"] = S.ring_sb(2, [128, nkmax], BF16, "p")
        R["ps_t"] = S.ring_ps(2, [128, 4, 128], BF16, "pst")
        R["pT"] = S.ring_sb(2, [128, nch, 128], BF16, "pT")
        R["ps_o"] = S.ring_ps(2, [128, dh], F32, "pso")
        R["sm"] = S.ring_sb(4, [128, 4], F32, "sm")
        R["st"] = S.ring_sb(2, [128, 8, 128], BF16, "fmst")
        R["k"] = 0
        return R

    def attend(self, R, q_ap, segs, vch, scale, out_ap, b_out, deps, dh):
        P = self.P
        sc, bsc = R["sc"].next()
        off = 0
        for (k_ap, n, bias_ap) in segs:
            ps, bps = R["ps_s"].next()
            P.op("pe", lambda e, ps=ps, k_ap=k_ap, n=n: e.matmul(ps[:, 0:n], q_ap, k_ap, start=True, stop=True), reads=deps, writes=[bps])
            if bias_ap is not None:
                P.op("dve", lambda e, ps=ps, n=n, off=off, bias_ap=bias_ap: e.scalar_tensor_tensor(out=sc[:, off:off + n], in0=ps[:, 0:n], scalar=scale, in1=bias_ap,
                                                                                                  op0=ALU.mult, op1=ALU.add), reads=[bps] + deps, writes=[bsc])
            else:
                P.op("act", lambda e, ps=ps, n=n, off=off: e.mul(sc[:, off:off + n], ps[:, 0:n], scale), reads=[bps], writes=[bsc])
            off += n
        NK = off
        sm, bsm = R["sm"].next()
        P.op("dve", lambda e: e.reduce_max(out=sm[:, 0:1], in_=sc[:, 0:NK], axis=AX.X), reads=[bsc], writes=[bsm])
        P.op("dve", lambda e: e.tensor_scalar_mul(sm[:, 1:2], sm[:, 0:1], -1.0), reads=[bsm], writes=[bsm])
        p, bp = R["p"].next()
        P.op("act", lambda e: e.activation(p[:, 0:NK], sc[:, 0:NK], AF.Exp, bias=sm[:, 1:2], scale=1.0), reads=[bsc, bsm], writes=[bp])
        P.op("dve", lambda e: e.reduce_sum(out=sm[:, 2:3], in_=p[:, 0:NK], axis=AX.X), reads=[bp], writes=[bsm])
        P.op("dve", lambda e: e.reciprocal(sm[:, 3:4], sm[:, 2:3]), reads=[bsm], writes=[bsm])
        pT, bpT = R["pT"].next()
        nch = len(vch)
        for g0 in range(0, nch, 4):
            pst, bpst = R["ps_t"].next()
            grp = vch[g0:g0 + 4]
            for j, (v_ap, sz, koff) in enumerate(grp):
                P.op("pe", lambda e, pst=pst, j=j, sz=sz, koff=koff: e.transpose(pst[0:sz, j, :], p[:, koff:koff + sz], self.ident_b[:]), reads=[bp], writes=[bpst])
            ng = len(grp)
            full = all(sz == 128 for (_, sz, _) in grp)
            R["k"] += 1
            if full:
                if R["k"] % 2 == 0:
                    P.op("dve", lambda e, pst=pst, g0=g0, ng=ng: e.tensor_copy(pT[:, g0:g0 + ng, :], pst[:, 0:ng, :]), reads=[bpst], writes=[bpT])
                else:
                    P.op("act", lambda e, pst=pst, g0=g0, ng=ng: e.copy(pT[:, g0:g0 + ng, :], pst[:, 0:ng, :]), reads=[bpst], writes=[bpT])
            else:
                for j, (v_ap, sz, koff) in enumerate(grp):
                    P.op("dve", lambda e, pst=pst, g0=g0, j=j, sz=sz: e.tensor_copy(pT[0:sz, g0 + j, :], pst[0:sz, j, :]), reads=[bpst], writes=[bpT])
        pso, bpso = R["ps_o"].next()
        for ci, (v_ap, sz, koff) in enumerate(vch):
            P.op("pe", lambda e, ci=ci, v_ap=v_ap, sz=sz: e.matmul(pso[:, 0:dh], pT[0:sz, ci, :], v_ap, start=(ci == 0), stop=(ci == nch - 1)),
                 reads=[bpT] + deps, writes=[bpso])
        P.op("dve", lambda e: e.tensor_scalar_mul(out_ap, pso[:, 0:dh], sm[:, 3:4]), reads=[bpso, bsm], writes=[b_out])

    def tm_to_fm(self, R, src_ap, b_src, dst, i):
        P = self.P
        st, bst = R["st"].next()
        for g in range(2):
            pst, bpst = R["ps_t"].next()
            for j in range(4):
                cc = 4 * g + j
                P.op("pe", lambda e, pst=pst, j=j, cc=cc: e.transpose(pst[:, j, :], src_ap[:, cc * 128:(cc + 1) * 128], self.ident_b[:]), reads=[b_src], writes=[bpst])
            if g == 0:
                P.op("dve", lambda e, pst=pst, g=g: e.tensor_copy(st[:, 4 * g:4 * g + 4, :], pst[:]), reads=[bpst], writes=[bst])
            else:
                P.op("act", lambda e, pst=pst, g=g: e.copy(st[:, 4 * g:4 * g + 4, :], pst[:]), reads=[bpst], writes=[bst])
        P.dma("sp", lambda e: e.dma_start(out=dst[:, :, i * 128:(i + 1) * 128].rearrange("c p t -> p c t"), in_=st[:]), reads=[bst])

    def stage_na(self, l, with_ctx):
        P = self.P
        with self.stage("na%d" % l) as S:
            z, bz = S.sb([128, 13824], F32, "z")
            P.op("pool", lambda e: e.memset(z[:], 0.0), writes=[bz])
            b_F = P.buf()
            P.dma("sp", lambda e: e.dma_start(out=self.FB.rearrange("(p n) -> p n", p=128), in_=z[:]), reads=[bz], writes=[b_F])
            for h in range(16):
                src = bass.AP(tensor=self.rpb[l].tensor, offset=h * 465, ap=[[31, 15], [0, 64], [1, 31]])
                dst = bass.AP(tensor=self.FB.tensor, offset=h * 18 * 6144 + 6144, ap=[[6144, 15], [96, 64], [1, 31]])
                P.dma("sp", lambda e, src=src, dst=dst: e.dma_start(out=dst, in_=src), writes=[b_F])
            mk, bmk = S.sb([128, 5, 576], F32, "mk")
            P.dma("sp", lambda e: e.dma_start(out=mk[:], in_=self.mask_in.rearrange("t p n -> p t n")), writes=[bmk])
            R = self.attn_res(S, 832, 7, 64)
            q_r = S.ring_sb(2, [128, T], BF16, "q")
            k_r = S.ring_sb(2, [128, T], BF16, "k")
            v_r = S.ring_sb(2, [128, TT, 128], BF16, "v")
            bias_r = S.ring_sb(2, [128, 5, 576], F32, "bias")
            a_all, b_a = S.sb([128, TT, 1024], BF16, "a_all")
            types = [(7, 8), (5, 8), (3, 9), (3, 8), (1, 8)]
            tiles = list(range(16)) + ([16, 17] if with_ctx else [])
            for hp in range(8):
                qT, bq = q_r.next()
                kT, bk = k_r.next()
                v, bv = v_r.next()
                P.dma("sp", lambda e, qT=qT, hp=hp: e.dma_start(out=qT[:], in_=self.FM[hp]), writes=[bq])
                P.dma("sp", lambda e, kT=kT, hp=hp: e.dma_start(out=kT[:], in_=self.FM[8 + hp]), writes=[bk])
                P.dma("sp", lambda e, v=v, hp=hp: e.dma_start(out=v[:], in_=self.TM[:, hp * 128:(hp + 1) * 128].rearrange("(t p) c -> p t c", p=128)), writes=[bv])
                for sub in range(2):
                    h = 2 * hp + sub
                    bt, bbt = bias_r.next()
                    for ty, (joff, nr) in enumerate(types):
                        for a in range(2):
                            src = bass.AP(tensor=self.FB.tensor, offset=h * 18 * 6144 + (joff - a + 1) * 6144 + 15, ap=[[95, 64], [6144, nr], [1, 64]])
                            dst = bt[a * 64:(a + 1) * 64, ty, 0:nr * 64].rearrange("p (r k) -> p r k", k=64)
                            P.dma("sp", lambda e, src=src, dst=dst: e.dma_start(out=dst, in_=src), reads=[b_F], writes=[bbt])
                    for ty, (joff, nr) in enumerate(types):
                        P.op("pool", lambda e, bt=bt, ty=ty, nr=nr: e.tensor_tensor(out=bt[:, ty, 0:nr * 64], in0=bt[:, ty, 0:nr * 64], in1=mk[:, ty, 0:nr * 64], op=ALU.add),
                             reads=[bbt, bmk], writes=[bbt])
                    ps0 = sub * 64
                    deps = [bq, bk, bv, bbt]
                    for i in tiles:
                        q_ap = qT[ps0:ps0 + 64, i * 128:(i + 1) * 128]
                        segs = []
                        vch = []
                        if i < 16:
                            if i == 0:
                                ty, base, nr = 0, 0, 8
                            elif i == 1:
                                ty, base, nr = 1, 0, 8
                            elif i == 14:
                                ty, base, nr = 3, 24, 8
                            elif i == 15:
                                ty, base, nr = 4, 24, 8
                            else:
                                ty, base, nr = 2, 2 * i - 4, 9
                            t0 = base * 64
                            segs.append((kT[ps0:ps0 + 64, t0:t0 + 512], 512, bt[:, ty, 0:512]))
                            if nr == 9:
                                segs.append((kT[ps0:ps0 + 64, t0 + 512:t0 + 576], 64, bt[:, ty, 512:576]))
                            for m in range(4):
                                vch.append((v[:, base // 2 + m, ps0:ps0 + 64], 128, m * 128))
                            if nr == 9:
                                vch.append((v[0:64, base // 2 + 4, ps0:ps0 + 64], 64, 512))
                        koff = nr * 64 if i < 16 else 0
                        segs.append((kT[ps0:ps0 + 64, S_LAT:T], 256, None))
                        vch.append((v[:, 16, ps0:ps0 + 64], 128, koff))
                        vch.append((v[:, 17, ps0:ps0 + 64], 128, koff + 128))
                        self.attend(R, q_ap, segs, vch, 0.125, a_all[:, i, h * 64:(h + 1) * 64], b_a, deps, 64)
            for i in tiles:
                self.tm_to_fm(R, a_all[:, i, :], b_a, self.AT, i)

    def stage_gqa(self, l, with_ctx):
        P = self.P
        with self.stage("gqa%d" % l) as S:
            R = self.attn_res(S, T, TT, 128)
            qT, bq = S.sb([128, 8, T], BF16, "qT")
            kT, bk = S.sb([128, 2, T], BF16, "kT")
            v, bv = S.sb([128, TT, 256], BF16, "v")
            c_all, b_c = S.sb([128, TT, 1024], BF16, "c_all")
            tq_r = S.ring_sb(2, [128, 1280], BF16, "tq")
            P.dma("sp", lambda e: e.dma_start(out=v[:], in_=self.TM[:, 4864:5120].rearrange("(t p) c -> p t c", p=128)), writes=[bv])
            kk = 0
            for tt in range(TT):
                tq, btq = tq_r.next()
                P.dma("sp", lambda e, tq=tq, tt=tt: e.dma_start(out=tq[:], in_=self.TM[tt * 128:(tt + 1) * 128, 3584:4864]), writes=[btq])
                for (g0, ng) in [(0, 4), (4, 4), (8, 2)]:
                    pst, bpst = R["ps_t"].next()
                    for j in range(ng):
                        cc = g0 + j
                        P.op("pe", lambda e, pst=pst, j=j, cc=cc, tq=tq: e.transpose(pst[:, j, :], tq[:, cc * 128:(cc + 1) * 128], self.ident_b[:]), reads=[btq], writes=[bpst])
                    if g0 < 8:
                        dstv, bd = qT[:, g0:g0 + 4, tt * 128:(tt + 1) * 128], bq
                    else:
                        dstv, bd = kT[:, 0:2, tt * 128:(tt + 1) * 128], bk
                    kk += 1
                    if kk % 2 == 0:
                        P.op("dve", lambda e, pst=pst, dstv=dstv, ng=ng: e.tensor_copy(dstv, pst[:, 0:ng, :]), reads=[bpst], writes=[bd])
                    else:
                        P.op("act", lambda e, pst=pst, dstv=dstv, ng=ng: e.copy(dstv, pst[:, 0:ng, :]), reads=[bpst], writes=[bd])
            tiles = list(range(16)) + ([16, 17] if with_ctx else [])
            deps = [bq, bk, bv]
            sc = 128.0 ** -0.5
            for i in tiles:
                for h in range(8):
                    g = h // 4
                    q_ap = qT[:, h, i * 128:(i + 1) * 128]
                    segs = []
                    vch = []
                    if i < 16:
                        for j in range(4):
                            segs.append((kT[:, g, j * 512:(j + 1) * 512], 512, None))
                        for t in range(16):
                            vch.append((v[:, t, g * 128:(g + 1) * 128], 128, t * 128))
                        koff = S_LAT
                    else:
                        koff = 0
                    segs.append((kT[:, g, S_LAT:T], 256, None))
                    vch.append((v[:, 16, g * 128:(g + 1) * 128], 128, koff))
                    vch.append((v[:, 17, g * 128:(g + 1) * 128], 128, koff + 128))
                    self.attend(R, q_ap, segs, vch, sc, c_all[:, i, h * 128:(h + 1) * 128], b_c, deps, 128)
                self.tm_to_fm(R, c_all[:, i, :], b_c, self.CT, i)

    def stage_gla(self, l, d, with_ctx):
        P = self.P
        with self.stage("gla%d%d" % (l, d)) as S:
            R = {"st": S.ring_sb(2, [128, 8, 128], BF16, "fmst"), "ps_t": S.ring_ps(2, [128, 4, 128], BF16, "pst")}
            lowa, blow = S.sb([17, T], F32, "lowa")
            P.op("pool", lambda e: e.memset(lowa[:], 1.0), writes=[blow])
            P.dma("sp", lambda e: e.dma_start(out=lowa[0:16, :], in_=self.LOW[d * 16:(d + 1) * 16, :]), writes=[blow])
            w2a, bw2 = S.sb([17, 512], F32, "w2a")
            P.dma("sp", lambda e: e.dma_start(out=w2a[0:16, :], in_=self.w_a2[l][d]), writes=[bw2])
            P.dma("sp", lambda e: e.dma_start(out=w2a[16:17, :], in_=self.b_a[l][d:d + 1, :]), writes=[bw2])
            tri, btri = S.sb([128, 4, 128], F32, "tri")
            P.dma("sp", lambda e: e.dma_start(out=tri[:], in_=self.tri_in.rearrange("f p n -> p f n")), writes=[btri])
            qTa, bqa = S.sb([128, 4, T], BF16, "qTa")
            kTa, bka = S.sb([128, 4, T], BF16, "kTa")
            P.dma("sp", lambda e: e.dma_start(out=qTa[:], in_=self.FM[16:20].rearrange("c p t -> p c t")), writes=[bqa])
            P.dma("sp", lambda e: e.dma_start(out=kTa[:], in_=self.FM[20:24].rearrange("c p t -> p c t")), writes=[bka])
            st32, bs32 = S.sb([128, 4, 256], F32, "st32")
            stb, bsb = S.sb([128, 4, 256], BF16, "stb")
            P.op("pool", lambda e: e.memset(st32[:], 0.0), writes=[bs32])
            P.op("pool", lambda e: e.memset(stb[:], 0.0), writes=[bsb])
            bs, ks = (0, 2) if d == 0 else (1, 3)
            lastcol = 127 if d == 0 else 0
            k_r = S.ring_sb(2, [128, 512], BF16, "ktm")
            v_r = S.ring_sb(2, [128, 1024], BF16, "vtm")
            pz_r = S.ring_ps(2, [128, 512], F32, "pz")
            pbT, bpbT = S.ps([128, 4, 128], F32, "pbT")
            pat, bpat = S.ps([128, 128], F32, "pat")
            po, bpo = S.ps([128, 256], F32, "po")
            pst_, bpst_ = S.ps([128, 256], F32, "pstt")
            f5 = {n: S.ring_sb(2, [128, 512], F32, n) for n in ("az", "e1", "l1", "mn", "la", "ek")}
            eT_r = S.ring_sb(2, [128, 4, 128], F32, "eT")
            enT_r = S.ring_sb(2, [128, 4, 128], F32, "enT")
            qt_r = S.ring_sb(2, [128, 4, 128], BF16, "qt")
            kt_r = S.ring_sb(2, [128, 4, 128], BF16, "kt")
            kh_r = S.ring_sb(2, [128, 512], BF16, "kh")
            at_r = S.ring_sb(2, [128, 128], BF16, "at")
            of_r = S.ring_sb(2, [128, 1024], F32, "of")
            if d == 1:
                os_r = S.ring_sb(2, [128, 1024], F32, "osum")
                sq_r = S.ring_sb(1, [128, 1024], F32, "sqo")
                og_r = S.ring_sb(2, [128, 1024], BF16, "og")
                sg_r = S.ring_sb(2, [128, 1024], F32, "sg")
                tn_r = S.ring_sb(1, [128, 1024], F32, "tn")
                bt_r = S.ring_sb(2, [128, 1024], BF16, "btile")
                ssq_r = S.ring_sb(2, [128, 4], F32, "ssq")
            order = [16, 17] + list(range(16)) if d == 0 else [17, 16] + list(range(15, -1, -1))
            for tt in order:
                need_o = with_ctx or tt < 16
                tc = slice(tt * 128, (tt + 1) * 128)
                ktm, bktm = k_r.next()
                vtm, bvtm = v_r.next()
                P.dma("sp", lambda e, ktm=ktm, tc=tc: e.dma_start(out=ktm[:], in_=self.TM[tc, 1024:1536]), writes=[bktm])
                P.dma("sp", lambda e, vtm=vtm, tc=tc: e.dma_start(out=vtm[:], in_=self.TM[tc, 1536:2560]), writes=[bvtm])
                pz, bpz = pz_r.next()
                P.op("pe", lambda e, pz=pz, tc=tc: e.matmul(pz[:], lowa[0:17, tc], w2a[0:17, :], start=True, stop=True), reads=[blow, bw2], writes=[bpz])
                az, baz = f5["az"].next(); e1, be1 = f5["e1"].next(); l1, bl1 = f5["l1"].next()
                mn, bmn = f5["mn"].next(); la, bla = f5["la"].next(); ek, bek = f5["ek"].next()
                P.op("dve", lambda e, mn=mn, pz=pz: e.tensor_scalar_min(mn[:], pz[:], 0.0), reads=[bpz], writes=[bmn])
                P.op("dve", lambda e, az=az, mn=mn, pz=pz: e.scalar_tensor_tensor(out=az[:], in0=mn[:], scalar=2.0, in1=pz[:], op0=ALU.mult, op1=ALU.subtract), reads=[bpz, bmn], writes=[baz])
                P.op("act", lambda e, e1=e1, az=az: e.activation(e1[:], az[:], AF.Exp), reads=[baz], writes=[be1])
                P.op("act", lambda e, l1=l1, e1=e1: e.activation(l1[:], e1[:], AF.Ln, bias=1.0), reads=[be1], writes=[bl1])
                P.op("dve", lambda e, la=la, mn=mn, l1=l1: e.tensor_tensor(out=la[:], in0=mn[:], in1=l1[:], op=ALU.subtract), reads=[bmn, bl1], writes=[bla])
                pk, bpk = pz_r.next()
                P.op("pe", lambda e, pk=pk, la=la: e.matmul(pk[:], tri[:, ks, :], la[:], start=True, stop=True), reads=[btri, bla], writes=[bpk])
                for h in range(4):
                    P.op("pe", lambda e, la=la, h=h: e.matmul(pbT[:, h, :], la[:, h * 128:(h + 1) * 128], tri[:, bs, :], start=True, stop=True), reads=[btri, bla], writes=[bpbT])
                eT, beT = eT_r.next(); enT, benT = enT_r.next()
                P.op("act", lambda e, eT=eT: e.activation(eT[:], pbT[:], AF.Exp, scale=1.0 / 16), reads=[bpbT], writes=[beT])
                P.op("act", lambda e, enT=enT: e.activation(enT[:], pbT[:], AF.Exp, scale=-1.0 / 16), reads=[bpbT], writes=[benT])
                P.op("act", lambda e, ek=ek, pk=pk: e.activation(ek[:], pk[:], AF.Exp, scale=1.0 / 16), reads=[bpk], writes=[bek])
                qt, bqt = qt_r.next(); kt, bkt = kt_r.next(); kh, bkh = kh_r.next()
                P.op("dve", lambda e, qt=qt, eT=eT, tc=tc: e.scalar_tensor_tensor(out=qt[:], in0=qTa[:, :, tc], scalar=128.0 ** -0.5, in1=eT[:], op0=ALU.mult, op1=ALU.mult),
                     reads=[bqa, beT], writes=[bqt])
                P.op("pool", lambda e, kt=kt, enT=enT, tc=tc: e.tensor_tensor(out=kt[:], in0=kTa[:, :, tc], in1=enT[:], op=ALU.mult), reads=[bka, benT], writes=[bkt])
                P.op("pool", lambda e, kh=kh, ktm=ktm, ek=ek: e.tensor_tensor(out=kh[:], in0=ktm[:], in1=ek[:], op=ALU.mult), reads=[bktm, bek], writes=[bkh])
                of, bof = of_r.next()
                if d == 1 and need_o:
                    P.dma("sp", lambda e, of=of, tc=tc: e.dma_start(out=of[:], in_=self.OF[tc, :]), writes=[bof])
                    osum, bos = os_r.next()
                for h in range(4):
                    hv = slice(h * 256, (h + 1) * 256)
                    if need_o:
                        at, bat = at_r.next()
                        P.op("pe", lambda e, kt=kt, qt=qt, h=h: e.matmul(pat[:], kt[:, h, :], qt[:, h, :], start=True, stop=True), reads=[bkt, bqt], writes=[bpat])
                        P.op("dve", lambda e, at=at: e.tensor_tensor(out=at[:], in0=pat[:], in1=tri[:, bs, :], op=ALU.mult), reads=[bpat, btri], writes=[bat])
                        P.op("pe", lambda e, at=at, vtm=vtm, hv=hv: e.matmul(po[:], at[:], vtm[:, hv], start=True, stop=False), reads=[bat, bvtm], writes=[bpo])
                        P.op("pe", lambda e, qt=qt, h=h: e.matmul(po[:], qt[:, h, :], stb[:, h, :], start=False, stop=True), reads=[bqt, bsb], writes=[bpo])
                        if d == 0:
                            P.op("act", lambda e, of=of, hv=hv: e.copy(of[:, hv], po[:]), reads=[bpo], writes=[bof])
                        else:
                            P.op("dve", lambda e, osum=osum, of=of, hv=hv: e.tensor_tensor(out=osum[:, hv], in0=po[:], in1=of[:, hv], op=ALU.add), reads=[bpo, bof], writes=[bos])
                    P.op("pe", lambda e, kh=kh, vtm=vtm, h=h, hv=hv: e.matmul(pst_[:], kh[:, h * 128:(h + 1) * 128], vtm[:, hv], start=True, stop=True), reads=[bkh, bvtm], writes=[bpst_])
                    P.op("dve", lambda e, eT=eT, h=h: e.scalar_tensor_tensor(out=st32[:, h, :], in0=st32[:, h, :], scalar=eT[:, h, lastcol:lastcol + 1], in1=pst_[:],
                                                                             op0=ALU.mult, op1=ALU.add), reads=[bs32, beT, bpst_], writes=[bs32])
                    P.op("act", lambda e, h=h: e.copy(stb[:, h, :], st32[:, h, :]), reads=[bs32], writes=[bsb])
                if not need_o:
                    continue
                if d == 0:
                    P.dma("sp", lambda e, of=of, tc=tc: e.dma_start(out=self.OF[tc, :], in_=of[:]), reads=[bof])
                else:
                    sq, bsq = sq_r.next(); ssq, bssq = ssq_r.next(); og, bog = og_r.next(); sg, bsg = sg_r.next()
                    tn, btn = tn_r.next(); btile, bbt = bt_r.next()
                    P.dma("sp", lambda e, og=og, tc=tc: e.dma_start(out=og[:], in_=self.TM[tc, 2560:3584]), writes=[bog])
                    P.op("act", lambda e, sq=sq, osum=osum: e.activation(sq[:], osum[:], AF.Square), reads=[bos], writes=[bsq])
                    P.op("dve", lambda e, ssq=ssq, sq=sq: e.tensor_reduce(out=ssq[:, 0:4], in_=sq[:].rearrange("p (h d) -> p h d", h=4), axis=AX.X, op=ALU.add), reads=[bsq], writes=[bssq])
                    P.op("dve", lambda e, ssq=ssq: e.tensor_scalar(ssq[:], ssq[:], 1.0 / 256, EPS, ALU.mult, ALU.add), reads=[bssq], writes=[bssq])
                    P.op("act", lambda e, ssq=ssq: e.sqrt(ssq[:], ssq[:]), reads=[bssq], writes=[bssq])
                    P.op("dve", lambda e, ssq=ssq: e.reciprocal(ssq[:], ssq[:]), reads=[bssq], writes=[bssq])
                    P.op("act", lambda e, sg=sg, og=og: e.activation(sg[:], og[:], AF.Silu), reads=[bog], writes=[bsg])
                    gng = self.gbc[:, 512 + 256 * l:768 + 256 * l]
                    for h in range(4):
                        hv = slice(h * 256, (h + 1) * 256)
                        P.op("dve", lambda e, tn=tn, osum=osum, ssq=ssq, h=h, hv=hv: e.scalar_tensor_tensor(out=tn[:, hv], in0=osum[:, hv], scalar=ssq[:, h:h + 1], in1=gng,
                                                                                                          op0=ALU.mult, op1=ALU.mult), reads=[bos, bssq], writes=[btn])
                    P.op("pool", lambda e, btile=btile, tn=tn, sg=sg: e.tensor_tensor(out=btile[:], in0=tn[:], in1=sg[:], op=ALU.mult), reads=[btn, bsg], writes=[bbt])
                    self.tm_to_fm(R, btile[:], bbt, self.BT, tt)

    def stage_merge_a(self, l, with_ctx):
        P = self.P
        with self.stage("mga%d" % l) as S:
            br = []
            for nm, src in (("a", self.AT), ("b", self.BT), ("c", self.CT)):
                t, b = S.sb([128, 8, T], BF16, nm + "T")
                P.dma("sp", lambda e, t=t, src=src: e.dma_start(out=t[:], in_=src.rearrange("c p t -> p c t")), writes=[b])
                br.append((t, b))
            wsrc = [self.w_pa[l], self.w_pb[l], self.w_pc[l]]
            w_r = [S.ring_sb(2, [128, 8, 128], BF16, "wp%d" % i) for i in range(3)]
            g_r = [S.ring_sb(2, [128, T], BF16, "g%d" % i) for i in range(3)]
            ps_r = [S.ring_ps(2, [128, 512], F32, "psm%d" % i) for i in range(3)]
            t_r = [S.ring_sb(2, [128, 512], F32, "tm%d" % i) for i in range(3)]
            m_r = S.ring_sb(2, [128, T], BF16, "mst")
            blks = BLK5 if with_ctx else BLK5[:4]
            for mc in range(DC):
                ws = []
                gs = []
                for i in range(3):
                    wt, bw = w_r[i].next()
                    P.dma("pool", lambda e, wt=wt, i=i, mc=mc: e.dma_start(out=wt[:], in_=wsrc[i].rearrange("(k p) n -> p k n", p=128)[:, :, mc * 128:(mc + 1) * 128]), writes=[bw])
                    ws.append((wt, bw))
                    gt, bg = g_r[i].next()
                    P.dma("sp", lambda e, gt=gt, i=i, mc=mc: e.dma_start(out=gt[:], in_=self.FM[24 + 16 * i + mc]), writes=[bg])
                    gs.append((gt, bg))
                mst, bm = m_r.next()
                for (t0, nb, w) in blks:
                    tts = []
                    for i in range(3):
                        ps, bps = ps_r[i].next()
                        for kk in range(8):
                            P.op("pe", lambda e, ps=ps, i=i, kk=kk, t0=t0, nb=nb, wt=ws[i][0]: e.matmul(ps[:, 0:nb], wt[:, kk, :], br[i][0][:, kk, t0:t0 + nb], start=(kk == 0), stop=(kk == 7)),
                                 reads=[ws[i][1], br[i][1]], writes=[bps])
                        tt_, btt = t_r[i].next()
                        P.op("dve", lambda e, tt_=tt_, ps=ps, nb=nb, t0=t0, gt=gs[i][0]: e.tensor_tensor(out=tt_[:, 0:nb], in0=ps[:, 0:nb], in1=gt[:, t0:t0 + nb], op=ALU.mult),
                             reads=[bps, gs[i][1]], writes=[btt])
                        tts.append((tt_, btt))
                    P.op("pool", lambda e, nb=nb, a=tts[0][0], b=tts[1][0]: e.tensor_tensor(out=a[:, 0:nb], in0=a[:, 0:nb], in1=b[:, 0:nb], op=ALU.add),
                         reads=[tts[0][1], tts[1][1]], writes=[tts[0][1]])
                    P.op("pool", lambda e, nb=nb, t0=t0, mst=mst, a=tts[0][0], c=tts[2][0]: e.tensor_tensor(out=mst[:, t0:t0 + nb], in0=a[:, 0:nb], in1=c[:, 0:nb], op=ALU.add),
                         reads=[tts[0][1], tts[2][1]], writes=[bm])
                ncol = T if with_ctx else S_LAT
                P.dma("sp", lambda e, mst=mst, mc=mc, ncol=ncol: e.dma_start(out=self.MT[mc][:, 0:ncol], in_=mst[:, 0:ncol]), reads=[bm])

    def stage_merge_b(self, l, with_ctx):
        P = self.P
        with self.stage("mgb%d" % l) as S:
            ncol = T if with_ctx else S_LAT
            wo, bwo = S.sb([128, DC, D], BF16, "wo")
            P.dma("pool", lambda e: e.dma_start(out=wo[:], in_=self.w_out[l].rearrange("(k p) n -> p k n", p=128)), writes=[bwo])
            mT, bmT = S.sb([128, DC, T], BF16, "mT")
            P.dma("sp", lambda e: e.dma_start(out=mT[:, :, 0:ncol], in_=self.MT[:, :, 0:ncol].rearrange("c p t -> p c t")), writes=[bmT])
            x_r = S.ring_sb(2, [128, T], F32, "xr")
            ps_r = S.ring_ps(4, [128, 512], F32, "pso")
            blks = BLK5 if with_ctx else BLK5[:4]
            G1 = self.modT[l][:, 32:48, :]
            for mc in range(DC):
                xr, bx = x_r.next()
                P.dma("sp", lambda e, xr=xr, mc=mc: e.dma_start(out=xr[:, 0:ncol], in_=self.XT[mc][:, 0:ncol]), writes=[bx])
                for (t0, nb, w) in blks:
                    ps, bps = ps_r.next()
                    for kk in range(DC):
                        P.op("pe", lambda e, ps=ps, kk=kk, mc=mc, t0=t0, nb=nb: e.matmul(ps[:, 0:nb], wo[:, kk, mc * 128:(mc + 1) * 128], mT[:, kk, t0:t0 + nb], start=(kk == 0), stop=(kk == DC - 1)),
                             reads=[bwo, bmT], writes=[bps])
                    P.op("dve", lambda e, ps=ps, xr=xr, mc=mc, t0=t0, nb=nb, w=w: e.scalar_tensor_tensor(out=xr[:, t0:t0 + nb], in0=ps[:, 0:nb], scalar=G1[:, mc, w:w + 1], in1=xr[:, t0:t0 + nb],
                                                                                                       op0=ALU.mult, op1=ALU.add), reads=[bps, bx], writes=[bx])
                P.dma("sp", lambda e, xr=xr, mc=mc: e.dma_start(out=self.XT[mc][:, 0:ncol], in_=xr[:, 0:ncol]), reads=[bx])

    def stage_ffn_block(self, l, t0, nb, w, moe):
        P = self.P
        with self.stage("ffn%d_%d" % (l, t0)) as S:
            hT, b_hT = S.sb([128, DC, nb], BF16, "hT")
            self.norm_blocks(S, hT, b_hT, self.A2[l], self.modT[l][:, 48:64, :], tok0=t0, ntok=nb, nring=1)
            actT, bact = S.sb([128, FC, nb], BF16, "actT")
            G2 = self.modT[l][:, 80:96, :]
            wg_r = S.ring_sb(2, [128, DC, 256], BF16, "wg")
            wu_r = S.ring_sb(2, [128, DC, 256], BF16, "wu")
            wd_r = S.ring_sb(2, [128, FC, 128], BF16, "wd")
            psg_r = S.ring_ps(2, [128, 512], F32, "psg")
            psu_r = S.ring_ps(2, [128, 512], F32, "psu")
            psd_r = S.ring_ps(2, [128, 512], F32, "psd")
            sg_r = S.ring_sb(2, [128, 512], F32, "sg")
            x_r = S.ring_sb(2, [128, 512], F32, "xr")
            if moe:
                yacc, byacc = S.sb([128, DC, nb], F32, "yacc")
                g8, bg8 = S.sb([8, nb], F32, "g8")
                P.dma("sp", lambda e: e.dma_start(out=g8[:], in_=self.GATE[:, t0:t0 + nb]), writes=[bg8])
                sel, bsel = S.sb([8, 1024], F32, "sel")
                P.dma("sp", lambda e: e.dma_start(out=sel[:], in_=self.sel_in), writes=[bsel])
                gb_r = S.ring_sb(2, [128, 512], F32, "gb")
                tmp_r = S.ring_sb(2, [128, 512], F32, "tmpa")
                experts = list(range(NE))
            else:
                experts = [None]
            for ei, ex in enumerate(experts):
                if moe:
                    wgs = self.mw_gate[ex].rearrange("(k p) n -> p k n", p=128)
                    wus = self.mw_up[ex].rearrange("(k p) n -> p k n", p=128)
                    wds = self.mw_down[ex].rearrange("(f p) n -> p f n", p=128)
                    gb, bgb = gb_r.next()
                    psb, bpsb = psd_r.next()
                    P.op("pe", lambda e, psb=psb, ex=ex: e.matmul(psb[:, 0:nb], sel[0:8, ex * 128:(ex + 1) * 128], g8[0:8, :], start=True, stop=True), reads=[bsel, bg8], writes=[bpsb])
                    P.op("act", lambda e, psb=psb, gb=gb: e.copy(gb[:, 0:nb], psb[:, 0:nb]), reads=[bpsb], writes=[bgb])
                else:
                    wgs = self.dw_gate.rearrange("(k p) n -> p k n", p=128)
                    wus = self.dw_up.rearrange("(k p) n -> p k n", p=128)
                    wds = self.dw_down.rearrange("(f p) n -> p f n", p=128)
                for fg in range(FC // 2):
                    wg, bwg = wg_r.next()
                    wu, bwu = wu_r.next()
                    P.dma("pool", lambda e, wg=wg, fg=fg, wgs=wgs: e.dma_start(out=wg[:], in_=wgs[:, :, fg * 256:(fg + 1) * 256]), writes=[bwg])
                    P.dma("pool", lambda e, wu=wu, fg=fg, wus=wus: e.dma_start(out=wu[:], in_=wus[:, :, fg * 256:(fg + 1) * 256]), writes=[bwu])
                    for j in range(2):
                        fc = 2 * fg + j
                        psg, bpsg = psg_r.next()
                        psu, bpsu = psu_r.next()
                        for kk in range(DC):
                            P.op("pe", lambda e, psg=psg, wg=wg, kk=kk, j=j: e.matmul(psg[:, 0:nb], wg[:, kk, j * 128:(j + 1) * 128], hT[:, kk, :], start=(kk == 0), stop=(kk == DC - 1)),
                                 reads=[bwg, b_hT], writes=[bpsg])
                        for kk in range(DC):
                            P.op("pe", lambda e, psu=psu, wu=wu, kk=kk, j=j: e.matmul(psu[:, 0:nb], wu[:, kk, j * 128:(j + 1) * 128], hT[:, kk, :], start=(kk == 0), stop=(kk == DC - 1)),
                                 reads=[bwu, b_hT], writes=[bpsu])
                        sg, bsg = sg_r.next()
                        P.op("act", lambda e, sg=sg, psg=psg: e.activation(sg[:, 0:nb], psg[:, 0:nb], AF.Silu), reads=[bpsg], writes=[bsg])
                        if moe:
                            tmp, btmp = tmp_r.next()
                            P.op("dve", lambda e, tmp=tmp, sg=sg, psu=psu: e.tensor_tensor(out=tmp[:, 0:nb], in0=sg[:, 0:nb], in1=psu[:, 0:nb], op=ALU.mult), reads=[bsg, bpsu], writes=[btmp])
                            P.op("pool", lambda e, tmp=tmp, gb=gb, fc=fc: e.tensor_tensor(out=actT[:, fc, :], in0=tmp[:, 0:nb], in1=gb[:, 0:nb], op=ALU.mult), reads=[btmp, bgb], writes=[bact])
                        else:
                            P.op("dve", lambda e, sg=sg, psu=psu, fc=fc: e.tensor_tensor(out=actT[:, fc, :], in0=sg[:, 0:nb], in1=psu[:, 0:nb], op=ALU.mult), reads=[bsg, bpsu], writes=[bact])
                for mc in range(DC):
                    wd, bwd = wd_r.next()
                    P.dma("pool", lambda e, wd=wd, mc=mc, wds=wds: e.dma_start(out=wd[:], in_=wds[:, :, mc * 128:(mc + 1) * 128]), writes=[bwd])
                    psd, bpsd = psd_r.next()
                    for fc in range(FC):
                        P.op("pe", lambda e, psd=psd, wd=wd, fc=fc: e.matmul(psd[:, 0:nb], wd[:, fc, :], actT[:, fc, :], start=(fc == 0), stop=(fc == FC - 1)),
                             reads=[bwd, bact], writes=[bpsd])
                    if moe:
                        if ei == 0:
                            P.op("act", lambda e, psd=psd, mc=mc: e.copy(yacc[:, mc, :], psd[:, 0:nb]), reads=[bpsd], writes=[byacc])
                        else:
                            P.op("dve", lambda e, psd=psd, mc=mc: e.tensor_tensor(out=yacc[:, mc, :], in0=yacc[:, mc, :], in1=psd[:, 0:nb], op=ALU.add), reads=[bpsd, byacc], writes=[byacc])
                    else:
                        xr, bx = x_r.next()
                        P.dma("sp", lambda e, xr=xr, mc=mc: e.dma_start(out=xr[:, 0:nb], in_=self.XT[mc][:, t0:t0 + nb]), writes=[bx])
                        P.op("dve", lambda e, psd=psd, xr=xr, mc=mc: e.scalar_tensor_tensor(out=xr[:, 0:nb], in0=psd[:, 0:nb], scalar=G2[:, mc, w:w + 1], in1=xr[:, 0:nb],
                                                                                             op0=ALU.mult, op1=ALU.add), reads=[bpsd, bx], writes=[bx])
                        P.dma("sp", lambda e, xr=xr, mc=mc: e.dma_start(out=self.XT[mc][:, t0:t0 + nb], in_=xr[:, 0:nb]), reads=[bx])
            if moe:
                for mc in range(DC):
                    xr, bx = x_r.next()
                    P.dma("sp", lambda e, xr=xr, mc=mc: e.dma_start(out=xr[:, 0:nb], in_=self.XT[mc][:, t0:t0 + nb]), writes=[bx])
                    P.op("dve", lambda e, xr=xr, mc=mc: e.scalar_tensor_tensor(out=xr[:, 0:nb], in0=yacc[:, mc, :], scalar=G2[:, mc, w:w + 1], in1=xr[:, 0:nb],
                                                                                op0=ALU.mult, op1=ALU.add), reads=[byacc, bx], writes=[bx])
                    P.dma("sp", lambda e, xr=xr, mc=mc: e.dma_start(out=self.XT[mc][:, t0:t0 + nb], in_=xr[:, 0:nb]), reads=[bx])

    def stage_router(self, l):
        P = self.P
        with self.stage("router") as S:
            rw, brw = S.sb([128, DC, NE], F32, "rw")
            P.dma("sp", lambda e: e.dma_start(out=rw[:], in_=self.router_w.rearrange("(k p) n -> p k n", p=128)), writes=[brw])
            rb, brb = S.sb([1, NE], F32, "rb")
            P.dma("sp", lambda e: e.dma_start(out=rb[:], in_=self.router_b), writes=[brb])
            gsb, bgsb = S.sb([8, S_LAT], F32, "gsb")
            pl_r = S.ring_ps(2, [128, NE], F32, "pl")
            pt_r = S.ring_ps(2, [8, 128], F32, "ptg")
            sm_r = S.ring_sb(2, [128, 8, 8], F32, "rsm")

            def cb(bi, t0, h32, b_h32):
                for s_ in range(2):
                    pl, bpl = pl_r.next()
                    for c in range(DC):
                        P.op("pe", lambda e, pl=pl, c=c, s_=s_, h32=h32: e.matmul(pl[:], h32[:, c, s_ * 128:(s_ + 1) * 128], rw[:, c, :], start=(c == 0), stop=False),
                             reads=[b_h32, brw], writes=[bpl])
                    P.op("pe", lambda e, pl=pl: e.matmul(pl[:], self.ones_f[0:1, :], rb[0:1, :], start=False, stop=True), reads=[brb], writes=[bpl])
                    sm, bsm = sm_r.next()
                    lg, eq1, lg2, eq2, gate, sc_ = sm[:, 0, :], sm[:, 1, :], sm[:, 2, :], sm[:, 3, :], sm[:, 4, :], sm[:, 5, :]
                    P.op("dve", lambda e, lg=lg, pl=pl: e.tensor_copy(lg, pl[:]), reads=[bpl], writes=[bsm])
                    P.op("dve", lambda e, lg=lg, sc_=sc_: e.reduce_max(out=sc_[:, 0:1], in_=lg, axis=AX.X), reads=[bsm], writes=[bsm])
                    P.op("dve", lambda e, sc_=sc_: e.tensor_scalar_mul(sc_[:, 1:2], sc_[:, 0:1], -1.0), reads=[bsm], writes=[bsm])
                    P.op("act", lambda e, lg=lg, eq1=eq1, sc_=sc_: e.sign(eq1, lg, bias=sc_[:, 1:2]), reads=[bsm], writes=[bsm])
                    P.op("dve", lambda e, eq1=eq1: e.tensor_scalar_add(eq1, eq1, 1.0), reads=[bsm], writes=[bsm])
                    P.op("dve", lambda e, lg=lg, eq1=eq1, lg2=lg2: e.scalar_tensor_tensor(out=lg2, in0=eq1, scalar=-1.0e30, in1=lg, op0=ALU.mult, op1=ALU.add), reads=[bsm], writes=[bsm])
                    P.op("dve", lambda e, lg2=lg2, sc_=sc_: e.reduce_max(out=sc_[:, 2:3], in_=lg2, axis=AX.X), reads=[bsm], writes=[bsm])
                    P.op("dve", lambda e, sc_=sc_: e.tensor_scalar_mul(sc_[:, 3:4], sc_[:, 2:3], -1.0), reads=[bsm], writes=[bsm])
                    P.op("act", lambda e, lg2=lg2, eq2=eq2, sc_=sc_: e.sign(eq2, lg2, bias=sc_[:, 3:4]), reads=[bsm], writes=[bsm])
                    P.op("dve", lambda e, eq2=eq2: e.tensor_scalar_add(eq2, eq2, 1.0), reads=[bsm], writes=[bsm])
                    P.op("dve", lambda e, sc_=sc_: e.tensor_tensor(out=sc_[:, 4:5], in0=sc_[:, 2:3], in1=sc_[:, 0:1], op=ALU.subtract), reads=[bsm], writes=[bsm])
                    P.op("act", lambda e, sc_=sc_: e.activation(sc_[:, 4:5], sc_[:, 4:5], AF.Exp), reads=[bsm], writes=[bsm])
                    P.op("dve", lambda e, sc_=sc_: e.tensor_scalar_add(sc_[:, 5:6], sc_[:, 4:5], 1.0), reads=[bsm], writes=[bsm])
                    P.op("dve", lambda e, sc_=sc_: e.reciprocal(sc_[:, 5:6], sc_[:, 5:6]), reads=[bsm], writes=[bsm])
                    P.op("dve", lambda e, sc_=sc_: e.tensor_tensor(out=sc_[:, 6:7], in0=sc_[:, 4:5], in1=sc_[:, 5:6], op=ALU.mult), reads=[bsm], writes=[bsm])
                    P.op("dve", lambda e, gate=gate, eq1=eq1, sc_=sc_: e.tensor_scalar_mul(gate, eq1, sc_[:, 5:6]), reads=[bsm], writes=[bsm])
                    P.op("dve", lambda e, gate=gate, eq2=eq2, sc_=sc_: e.scalar_tensor_tensor(out=gate, in0=eq2, scalar=sc_[:, 6:7], in1=gate, op0=ALU.mult, op1=ALU.add), reads=[bsm], writes=[bsm])
                    ptg, bptg = pt_r.next()
                    P.op("pe", lambda e, ptg=ptg, gate=gate: e.transpose(ptg[:], gate, self.ident_f[:]), reads=[bsm], writes=[bptg])
                    tcol = t0 + s_ * 128
                    P.op("act", lambda e, ptg=ptg, tcol=tcol: e.copy(gsb[:, tcol:tcol + 128], ptg[:]), reads=[bptg], writes=[bgsb])
            self.norm_blocks(S, None, None, self.A2[l], self.modT[l][:, 48:64, :], tok0=0, ntok=S_LAT, nring=2, cb=cb)
            P.dma("sp", lambda e: e.dma_start(out=self.GATE, in_=gsb[:]), reads=[bgsb])

    def stage_final(self):
        P = self.P
        with self.stage("final") as S:
            pt_r = S.ring_ps(2, [128, 4, 128], F32, "ptf")
            ot_r = S.ring_sb(2, [128, D], F32, "ot")
            cnt = [0]

            def cb(bi, t0, h32, b_h32):
                for s_ in range(2):
                    ot, bot = ot_r.next()
                    for g in range(4):
                        pt, bpt = pt_r.next()
                        for j in range(4):
                            cc = 4 * g + j
                            P.op("pe", lambda e, pt=pt, j=j, cc=cc, s_=s_, h32=h32: e.transpose(pt[:, j, :], h32[:, cc, s_ * 128:(s_ + 1) * 128], self.ident_f[:]), reads=[b_h32], writes=[bpt])
                        cnt[0] += 1
                        if cnt[0] % 2 == 0:
                            P.op("dve", lambda e, pt=pt, ot=ot, g=g: e.tensor_copy(ot[:, g * 512:(g + 1) * 512], pt[:].rearrange("p a b -> p (a b)")), reads=[bpt], writes=[bot])
                        else:
                            P.op("act", lambda e, pt=pt, ot=ot, g=g: e.copy(ot[:, g * 512:(g + 1) * 512], pt[:].rearrange("p a b -> p (a b)")), reads=[bpt], writes=[bot])
                    r0 = t0 + s_ * 128
                    P.dma("sp", lambda e, ot=ot, r0=r0: e.dma_start(out=self.OUT[r0:r0 + 128, :], in_=ot[:]), reads=[bot])
            self.norm_blocks(S, None, None, None, None, tok0=0, ntok=S_LAT, nring=2, gain_only=self.gT[:, 64:80], cb=cb)

    def build_all(self):
        self.stage_prep()
        for l in range(2):
            wc = (l == 0)
            self.stage_inproj(l)
            self.stage_na(l, wc)
            self.stage_gqa(l, wc)
            self.stage_gla(l, 0, wc)
            self.stage_gla(l, 1, wc)
            self.stage_merge_a(l, wc)
            self.stage_merge_b(l, wc)
            if l == 0:
                for (t0, nb, w) in BLK5:
                    self.stage_ffn_block(0, t0, nb, w, False)
            else:
                self.stage_router(1)
                for (t0, nb, w) in BLK5[:4]:
                    self.stage_ffn_block(1, t0, nb, w, True)
        self.stage_final()

    def finish(self):
        self.gst.__exit__(None, None, None)
        return self.nc


def make_consts():
    k = {}
    k["k_ident"] = np.eye(128, dtype=np.float32)
    j = np.arange(128)[:, None]
    i = np.arange(128)[None, :]
    tri = np.zeros((4, 128, 128), np.float32)
    tri[0] = (j <= i)
    tri[1] = (j >= i)
    tri[2] = (j > i)
    tri[3] = (j < i)
    k["k_tri"] = tri
    half = 64
    freqs = (10000.0 ** (-np.arange(0, half, 2, dtype=np.float32) / half)).astype(np.float32)
    t = np.arange(S_LAT)
    row = (t // 64).astype(np.float32)
    col = (t % 64).astype(np.float32)
    ang = np.concatenate([row[:, None] * freqs, col[:, None] * freqs], axis=-1).astype(np.float32)
    cos = np.ones((T, 64), np.float32)
    sin = np.zeros((T, 64), np.float32)
    cos[:S_LAT] = np.cos(ang)
    sin[:S_LAT] = np.sin(ang)
    cs = np.concatenate([np.tile(cos[:, None, :], (1, 4, 1)).reshape(T, 256), np.tile(sin[:, None, :], (1, 4, 1)).reshape(T, 256)], axis=1)
    k["k_cs"] = np.ascontiguousarray(cs, dtype=np.float32)
    mask = np.full((5, 128, 576), NEG, np.float32)
    qc = np.arange(64)
    cstart = np.clip(qc - 8, 0, 48)
    kc = np.arange(64)
    inwin = (kc[None, :] >= cstart[:, None]) & (kc[None, :] < cstart[:, None] + 16)
    for ty, i0 in enumerate([0, 1, 2, 14, 15]):
        rs0 = int(np.clip(2 * i0 - 4, 0, 24))
        rs1 = int(np.clip(2 * i0 + 1 - 4, 0, 24))
        base = rs0
        nr = rs1 + 8 - rs0
        for a in range(2):
            rs = rs0 if a == 0 else rs1
            for rho in range(nr):
                kr = base + rho
                if rs <= kr <= rs + 7:
                    blk = np.where(inwin, 0.0, NEG).astype(np.float32)
                    mask[ty, a * 64:(a + 1) * 64, rho * 64:(rho + 1) * 64] = blk
    k["k_namask"] = mask
    sel = np.zeros((8, 8, 128), np.float32)
    for e in range(8):
        sel[e, e, :] = 1.0
    k["k_sel"] = sel.reshape(8, 1024)
    return k


def make_inmap(inp, b, cfg, consts):
    m = dict(consts)
    f = lambda a: np.ascontiguousarray(a, dtype=np.float32)
    m["x"] = f(inp["x"][b])
    m["ctx"] = f(inp["ctx"][b])
    m["c"] = f(inp["c"][b:b + 1])
    m["c_ctx"] = f(inp["c_ctx"][None, :])
    m["w_ada"] = f(inp["w_ada"])
    m["b_ada"] = f(inp["b_ada"])
    m["norm1_g"] = f(inp["norm1_g"])
    m["norm2_g"] = f(inp["norm2_g"])
    m["final_norm_g"] = f(inp["final_norm_g"][None, :])
    m["gqa_qn_g"] = f(inp["gqa_qn_g"])
    m["gqa_kn_g"] = f(inp["gqa_kn_g"])
    m["gla_norm_g"] = f(inp["gla_norm_g"])
    for l in (cfg["layers"] if cfg.get("mixer", True) else []):
        m["w_in%d" % l] = f(inp["w_in"][l])
        m["na_rpb%d" % l] = f(inp["na_rpb"][l].reshape(16, 15 * 31))
        m["gla_w_a2_%d" % l] = f(inp["gla_w_a2"][l])
        m["gla_b_a%d" % l] = f(inp["gla_b_a"][l])
        m["w_pa%d" % l] = f(inp["w_pa"][l])
        m["w_pb%d" % l] = f(inp["w_pb"][l])
        m["w_pc%d" % l] = f(inp["w_pc"][l])
        m["w_out%d" % l] = f(inp["w_out"][l])
    if cfg.get("ffn", True):
        if 0 in cfg["layers"]:
            m["dense_w_gate"] = f(inp["dense_w_gate"][0])
            m["dense_w_up"] = f(inp["dense_w_up"][0])
            m["dense_w_down"] = f(inp["dense_w_down"][0])
        if 1 in cfg["layers"]:
            m["router_w"] = f(inp["router_w"][0])
            m["router_b"] = f(inp["router_b"])
            m["moe_w_gate"] = f(inp["moe_w_gate"][0])
            m["moe_w_up"] = f(inp["moe_w_up"][0])
            m["moe_w_down"] = f(inp["moe_w_down"][0])
    return m


FULL_CFG = {"layers": [0, 1], "ffn": True, "mixer": True, "debug": []}
N_CORES = 4


def kernel(**inputs):
    cfg = FULL_CFG
    B = Builder(cfg)
    B.declare()
    B.build_all()
    nc = B.finish()
    consts = make_consts()
    in_maps = []
    for b in range(N_CORES):
        m = make_inmap(inputs, b, cfg, consts)
        in_maps.append({k: v for k, v in m.items() if k in B.inputs})
    res = run_bass_kernel_spmd(nc, in_maps, core_ids=list(range(N_CORES)))
    out = np.stack([np.asarray(res.results[b]["out"], dtype=np.float32) for b in range(N_CORES)], 0)
    return out
```

```python
import numpy as np
import ml_dtypes
from contextlib import ExitStack
import concourse.bass as bass
import concourse.mybir as mybir
from concourse.bass_utils import run_bass_kernel_spmd

F32 = mybir.dt.float32
BF16 = mybir.dt.bfloat16
AF = mybir.ActivationFunctionType
ALU = mybir.AluOpType
AX = mybir.AxisListType

D = 2048
DC = 16
S_LAT = 2048
L_CTX = 256
T = S_LAT + L_CTX
TT = T // 128
D_IN = 13856
D_FF = 5632
FC = D_FF // 128
NE = 8
EPS = 1e-6
NEG = -30000.0
BLK5 = [(0, 512, 0), (512, 512, 0), (1024, 512, 0), (1536, 512, 0), (2048, 256, 1)]


class Sem:
    def __init__(self, h):
        self.h = h
        self.n = 0


class Buf:
    __slots__ = ("name", "w", "r", "dsem")

    def __init__(self, name=""):
        self.name = name
        self.w = None
        self.r = []
        self.dsem = None


class Prog:
    ENG = ("pe", "act", "dve", "pool", "sp")
    ENGN = {"pe": "tensor", "act": "scalar", "dve": "vector", "pool": "gpsimd", "sp": "sync"}

    def __init__(self, nc, stack, n_dma_sems=90):
        self.nc = nc
        self.ops = {e: [] for e in self.ENG}
        self.esem = {e: Sem(stack.enter_context(nc.semaphore("es_" + e))) for e in self.ENG}
        self.seen = {e: {} for e in self.ENG}
        self.free_dsems = [Sem(stack.enter_context(nc.semaphore("ds%d" % i))) for i in range(n_dma_sems)]
        self.stage_bufs = []
        self.bar = Sem(stack.enter_context(nc.semaphore("bar")))
        self.n_ops = 0

    def buf(self, name=""):
        b = Buf(name)
        self.stage_bufs.append(b)
        return b

    def bufs(self, n, name=""):
        return [self.buf("%s%d" % (name, i)) for i in range(n)]

    def _dsem(self, b):
        if b.dsem is None:
            b.dsem = self.free_dsems.pop()
        return b.dsem

    def _waits_for(self, eng, reads, writes):
        need = {}

        def add(m):
            if m is None:
                return
            s, v = m
            if need.get(id(s), (s, 0))[1] < v:
                need[id(s)] = (s, v)
        for b in reads:
            add(b.w)
        for b in writes:
            add(b.w)
            for m in b.r:
                add(m)
        out = []
        seen = self.seen[eng]
        for s, v in need.values():
            if s is self.esem[eng] and eng == "pe":
                continue
            if seen.get(id(s), 0) >= v:
                continue
            seen[id(s)] = v
            out.append((s.h, v))
        return out

    def op(self, eng, emit, reads=(), writes=()):
        waits = self._waits_for(eng, reads, writes)
        s = self.esem[eng]
        s.n += 1
        mark = (s, s.n)
        self.ops[eng].append((waits, emit, [(s.h, 1)]))
        for b in reads:
            b.r.append(mark)
            if len(b.r) > 24:
                b.r = b.r[-24:] if False else b.r
        for b in writes:
            b.w = mark
            b.r = []
        self.n_ops += 1

    def dma(self, q, emit, reads=(), writes=(), n=1):
        waits = self._waits_for(q, reads, writes)
        prim = writes[0] if len(writes) else reads[0]
        s = self._dsem(prim)
        s.n += 16 * n
        mark = (s, s.n)
        self.ops[q].append((waits, emit, [(s.h, 16)]))
        for b in reads:
            b.r.append(mark)
        for b in writes:
            b.w = mark
            b.r = []
        self.n_ops += 1

    def raw_dma(self, q, emit, sem):
        sem.n += 16
        self.ops[q].append(([], emit, [(sem.h, 16)]))
        self.n_ops += 1

    def barrier(self):
        waits = []
        for e in self.ENG:
            if e != "sp" and self.esem[e].n > 0:
                waits.append((self.esem[e].h, self.esem[e].n))
        for b in self.stage_bufs:
            if b.dsem is not None:
                waits.append((b.dsem.h, b.dsem.n))
        self.bar.n += 1
        v = self.bar.n
        self.ops["sp"].append((waits, lambda e: e.nop(), [(self.bar.h, 1)]))
        for e in self.ENG:
            if e != "sp":
                self.ops[e].append(([(self.bar.h, v)], None, []))
        for b in self.stage_bufs:
            if b.dsem is not None:
                self.free_dsems.append(b.dsem)
                b.dsem = None
            b.w = None
            b.r = []
        self.stage_bufs = []
        for e in self.ENG:
            self.seen[e] = {}

    def flush(self):
        nc = self.nc
        with nc.Block() as block:
            for e in self.ENG:
                ops = self.ops[e]

                def body(eng, ops=ops):
                    for waits, emit, incs in ops:
                        for (sh, v) in waits:
                            eng.wait_ge(sh, v)
                        if emit is None:
                            continue
                        r = emit(eng)
                        rs = r if isinstance(r, (list, tuple)) else [r]
                        for ins in rs:
                            for (sh, amt) in incs:
                                ins.then_inc(sh, amt)
                getattr(block, self.ENGN[e])(body)
        self.ops = {e: [] for e in self.ENG}


class Ring:
    def __init__(self, tiles, bufs):
        self.t = tiles
        self.b = bufs
        self.i = 0

    def next(self):
        k = self.i % len(self.t)
        self.i += 1
        return self.t[k], self.b[k]


class Stage:
    def __init__(self, B, name):
        self.B = B
        self.name = name
        self.st = ExitStack()

    def __enter__(self):
        self.st.__enter__()
        self.B.issue_conv(self.name)
        return self

    def __exit__(self, *a):
        self.B.P.barrier()
        self.B.P.flush()
        return self.st.__exit__(*a)

    def sb(self, shape, dt, name=None):
        B = self.B
        B.uid += 1
        t = self.st.enter_context(B.nc.sbuf_tensor("%s_%s%d" % (self.name, name or "t", B.uid), list(shape), dt))
        return t, B.P.buf()

    def ps(self, shape, dt=F32, name=None):
        B = self.B
        B.uid += 1
        t = self.st.enter_context(B.nc.psum_tensor("%s_%s%d" % (self.name, name or "p", B.uid), list(shape), dt))
        return t, B.P.buf()

    def ring_sb(self, n, shape, dt, name=None):
        ts = [self.sb(shape, dt, name) for _ in range(n)]
        return Ring([t for t, _ in ts], [b for _, b in ts])

    def ring_ps(self, n, shape, dt=F32, name=None):
        ts = [self.ps(shape, dt, name) for _ in range(n)]
        return Ring([t for t, _ in ts], [b for _, b in ts])


class Builder:
    def __init__(self, cfg):
        self.cfg = cfg
        self.dbg = set(cfg.get("debug", ()))
        self.nc = bass.Bass("TRN2", target_bir_lowering=False)
        self.gst = ExitStack()
        self.gst.__enter__()
        self.P = Prog(self.nc, self.gst)
        self.uid = 0
        self.inputs = {}
        self.outputs = []

    def din(self, name, shape, dt=F32):
        ap = self.nc.dram_tensor(name, list(shape), dt, kind="ExternalInput").ap()
        self.inputs[name] = ap
        return ap

    def dscr(self, name, shape, dt):
        if name in self.dbg:
            self.outputs.append(name)
            return self.nc.dram_tensor(name, list(shape), dt, kind="ExternalOutput").ap()
        return self.nc.dram_tensor(name, list(shape), dt).ap()

    def gsb(self, name, shape, dt):
        return self.gst.enter_context(self.nc.sbuf_tensor(name, list(shape), dt))

    def stage(self, name):
        return Stage(self, name)

    def setup_conv(self):
        self.conv_sem = self.P.free_dsems.pop()
        self.conv_jobs = []
        if not hasattr(self, "mw_gate"):
            return
        for ex in range(NE):
            wgs = self.mw_gate[ex].rearrange("(k p) n -> p k n", p=128)
            wus = self.mw_up[ex].rearrange("(k p) n -> p k n", p=128)
            wds = self.mw_down[ex].rearrange("(f p) n -> p f n", p=128)
            for fg in range(FC // 2):
                self.conv_jobs.append(lambda e, ex=ex, fg=fg, wgs=wgs: e.dma_start(out=self.WGB[ex, fg].rearrange("p (k n) -> p k n", k=DC), in_=wgs[:, :, fg * 256:(fg + 1) * 256]))
                self.conv_jobs.append(lambda e, ex=ex, fg=fg, wus=wus: e.dma_start(out=self.WUB[ex, fg].rearrange("p (k n) -> p k n", k=DC), in_=wus[:, :, fg * 256:(fg + 1) * 256]))
            for mc in range(DC):
                self.conv_jobs.append(lambda e, ex=ex, mc=mc, wds=wds: e.dma_start(out=self.WDB[ex, mc].rearrange("p (f n) -> p f n", f=FC), in_=wds[:, :, mc * 128:(mc + 1) * 128]))
        self.conv_per_stage = 24

    def issue_conv(self, stage_name):
        if not getattr(self, "conv_jobs", None):
            return
        if stage_name == "prep":
            return
        n = len(self.conv_jobs) if (stage_name.startswith("ffn1") or stage_name == "router") else self.conv_per_stage
        for _ in range(min(n, len(self.conv_jobs))):
            job = self.conv_jobs.pop(0)
            self.P.raw_dma("pool", job, self.conv_sem)

    def wait_conv(self):
        for e in ("sp",):
            self.P.ops[e].append(([(self.conv_sem.h, self.conv_sem.n)], None, []))

    def declare(self):
        c = self.cfg
        L = c["layers"]
        self.x_in = self.din("x", [S_LAT, D])
        self.ctx_in = self.din("ctx", [L_CTX, D])
        self.c_in = self.din("c", [1, D])
        self.cctx_in = self.din("c_ctx", [1, D])
        self.ident_in = self.din("k_ident", [128, 128])
        self.tri_in = self.din("k_tri", [4, 128, 128])
        self.cs_in = self.din("k_cs", [T, 512])
        self.mask_in = self.din("k_namask", [5, 128, 576])
        self.sel_in = self.din("k_sel", [8, 8 * 128])
        self.w_ada = self.din("w_ada", [2, D, 6 * D])
        self.b_ada = self.din("b_ada", [2, 6 * D])
        self.norm1_g = self.din("norm1_g", [2, D])
        self.norm2_g = self.din("norm2_g", [2, D])
        self.final_g = self.din("final_norm_g", [1, D])
        self.gqa_qn = self.din("gqa_qn_g", [2, 128])
        self.gqa_kn = self.din("gqa_kn_g", [2, 128])
        self.gla_ng = self.din("gla_norm_g", [2, 256])
        self.w_in = {}; self.rpb = {}; self.w_a2 = {}; self.b_a = {}
        self.w_pa = {}; self.w_pb = {}; self.w_pc = {}; self.w_out = {}
        for l in (L if c.get("mixer", True) else []):
            self.w_in[l] = self.din("w_in%d" % l, [D, D_IN])
            self.rpb[l] = self.din("na_rpb%d" % l, [16, 15 * 31])
            self.w_a2[l] = self.din("gla_w_a2_%d" % l, [2, 16, 512])
            self.b_a[l] = self.din("gla_b_a%d" % l, [2, 512])
            self.w_pa[l] = self.din("w_pa%d" % l, [1024, D])
            self.w_pb[l] = self.din("w_pb%d" % l, [1024, D])
            self.w_pc[l] = self.din("w_pc%d" % l, [1024, D])
            self.w_out[l] = self.din("w_out%d" % l, [D, D])
        if 0 in L and c.get("ffn", True):
            self.dw_gate = self.din("dense_w_gate", [D, D_FF])
            self.dw_up = self.din("dense_w_up", [D, D_FF])
            self.dw_down = self.din("dense_w_down", [D_FF, D])
        if 1 in L and c.get("ffn", True):
            self.router_w = self.din("router_w", [D, NE])
            self.router_b = self.din("router_b", [1, NE])
            self.mw_gate = self.din("moe_w_gate", [NE, D, D_FF])
            self.mw_up = self.din("moe_w_up", [NE, D, D_FF])
            self.mw_down = self.din("moe_w_down", [NE, D_FF, D])
        self.XT = self.dscr("XT", [DC, 128, T], F32)
        self.FM = self.dscr("FM", [72, 128, T], BF16)
        self.LOW = self.dscr("LOW", [32, T], F32)
        self.TM = self.dscr("TM", [T, 5120], BF16)
        self.AT = self.dscr("AT", [8, 128, T], BF16)
        self.BT = self.dscr("BT", [8, 128, T], BF16)
        self.CT = self.dscr("CT", [8, 128, T], BF16)
        self.MT = self.dscr("MT", [DC, 128, T], BF16)
        self.OF = self.dscr("OF", [T, 1024], F32)
        self.FB = self.dscr("FB", [16 * 18 * 64 * 96], F32)
        self.MODT = self.dscr("MODT", [2, 128, 96 * 2], F32)
        self.GATE = self.dscr("GATE", [8, S_LAT], F32)
        if hasattr(self, "mw_gate"):
            self.WGB = self.dscr("WGB", [NE, FC // 2, 128, DC * 256], BF16)
            self.WUB = self.dscr("WUB", [NE, FC // 2, 128, DC * 256], BF16)
            self.WDB = self.dscr("WDB", [NE, DC, 128, FC * 128], BF16)
        self.setup_conv()
        self.OUT = self.nc.dram_tensor("out", [S_LAT, D], F32, kind="ExternalOutput").ap()
        self.outputs.append("out")
        self.ident_f = self.gsb("ident_f", [128, 128], F32)
        self.ident_b = self.gsb("ident_b", [128, 128], BF16)
        self.ones_f = self.gsb("ones_f", [128, 128], F32)
        self.cactT = self.gsb("cactT", [128, 32], F32)
        self.modT = [self.gsb("modT%d" % l, [128, 96, 2], F32) for l in range(2)]
        self.A1 = [self.gsb("A1_%d" % l, [128, 16, 2], F32) for l in range(2)]
        self.A2 = [self.gsb("A2_%d" % l, [128, 16, 2], F32) for l in range(2)]
        self.gT = self.gsb("gT", [128, 80], F32)
        self.gbc = self.gsb("gbc", [128, 1024], F32)

    def stage_prep(self):
        P = self.P
        nc = self.nc
        with self.stage("prep") as S:
            b_id = P.buf()
            P.dma("sp", lambda e: e.dma_start(out=self.ident_f[:], in_=self.ident_in), writes=[b_id])
            P.op("dve", lambda e: e.tensor_copy(self.ident_b[:], self.ident_f[:]), reads=[b_id], writes=[P.buf()])
            b_ones = P.buf()
            P.op("pool", lambda e: e.memset(self.ones_f[:], 1.0), writes=[b_ones])
            xs_r = S.ring_sb(2, [128, D], F32, "xs")
            xo_r = S.ring_sb(2, [128, DC, 128], F32, "xo")
            pt_r = S.ring_ps(2, [128, 4, 128], F32, "pt")
            k = 0
            for tt in range(TT):
                src = self.x_in[tt * 128:(tt + 1) * 128, :] if tt < 16 else self.ctx_in[(tt - 16) * 128:(tt - 15) * 128, :]
                xs, bxs = xs_r.next()
                xo, bxo = xo_r.next()
                P.dma("sp", lambda e, xs=xs, src=src: e.dma_start(out=xs[:], in_=src), writes=[bxs])
                for g in range(4):
                    pt, bpt = pt_r.next()
                    for j in range(4):
                        cc = 4 * g + j
                        P.op("pe", lambda e, pt=pt, xs=xs, j=j, cc=cc: e.transpose(pt[:, j, :], xs[:, cc * 128:(cc + 1) * 128], self.ident_f[:]),
                             reads=[bxs, b_id], writes=[bpt])
                    if k % 2 == 0:
                        P.op("dve", lambda e, pt=pt, xo=xo, g=g: e.tensor_copy(xo[:, 4 * g:4 * g + 4, :], pt[:]), reads=[bpt], writes=[bxo])
                    else:
                        P.op("act", lambda e, pt=pt, xo=xo, g=g: e.copy(xo[:, 4 * g:4 * g + 4, :], pt[:]), reads=[bpt], writes=[bxo])
                    k += 1
                dst = self.XT[:, :, tt * 128:(tt + 1) * 128].rearrange("c p t -> p c t")
                P.dma("sp", lambda e, xo=xo, dst=dst: e.dma_start(out=dst, in_=xo[:]), reads=[bxo])
            cc_t, bcc = S.sb([32, 128], F32, "cc")
            P.dma("sp", lambda e: e.dma_start(out=cc_t[0:16, :], in_=self.c_in.rearrange("o (c p) -> (o c) p", p=128)), writes=[bcc])
            P.dma("sp", lambda e: e.dma_start(out=cc_t[16:32, :], in_=self.cctx_in.rearrange("o (c p) -> (o c) p", p=128)), writes=[bcc])
            cs_t, bcs = S.sb([32, 128], F32, "cs")
            P.op("act", lambda e: e.activation(cs_t[:], cc_t[:], AF.Silu), reads=[bcc], writes=[bcs])
            pm, bpm = S.ps([128, 128], F32, "pm")
            P.op("pe", lambda e: e.transpose(pm[:, 0:32], cs_t[:], self.ident_f[0:32, 0:32]), reads=[bcs, b_id], writes=[bpm])
            b_cact = P.buf()
            P.op("dve", lambda e: e.tensor_copy(self.cactT[:], pm[:, 0:32]), reads=[bpm], writes=[b_cact])
            gr, bgr = S.sb([80, 128], F32, "gr")
            srcs = [self.norm1_g[0:1, :], self.norm2_g[0:1, :], self.norm1_g[1:2, :], self.norm2_g[1:2, :], self.final_g[0:1, :]]
            for v, sap in enumerate(srcs):
                P.dma("sp", lambda e, v=v, sap=sap: e.dma_start(out=gr[v * 16:(v + 1) * 16, :], in_=sap.rearrange("o (c p) -> (o c) p", p=128)), writes=[bgr])
            P.op("pe", lambda e: e.transpose(pm[:, 0:80], gr[:], self.ident_f[0:80, 0:80]), reads=[bgr, b_id], writes=[bpm])
            b_gT = P.buf()
            P.op("dve", lambda e: e.tensor_copy(self.gT[:], pm[:, 0:80]), reads=[bpm], writes=[b_gT])
            grow, bgrow = S.sb([1, 1024], F32, "grow")
            rs = [(self.gqa_qn[0:1, :], 0, 128), (self.gqa_kn[0:1, :], 128, 128), (self.gqa_qn[1:2, :], 256, 128),
                  (self.gqa_kn[1:2, :], 384, 128), (self.gla_ng[0:1, :], 512, 256), (self.gla_ng[1:2, :], 768, 256)]
            for sap, o, n in rs:
                P.dma("sp", lambda e, sap=sap, o=o, n=n: e.dma_start(out=grow[0:1, o:o + n], in_=sap), writes=[bgrow])
            pb, bpb = S.ps([128, 512], F32, "pb")
            b_gbc = P.buf()
            for hh in range(2):
                P.op("pe", lambda e, hh=hh: e.matmul(pb[:], self.ones_f[0:1, :], grow[0:1, hh * 512:(hh + 1) * 512], start=True, stop=True),
                     reads=[bgrow, b_ones], writes=[bpb])
                P.op("dve", lambda e, hh=hh: e.tensor_copy(self.gbc[:, hh * 512:(hh + 1) * 512], pb[:]), reads=[bpb], writes=[b_gbc])
            wt_r = S.ring_sb(2, [128, DC, 512], F32, "wada")
            pmod_r = S.ring_ps(2, [128, 4, 2], F32, "pmod")
            bad, bbad = S.sb([96, 128], F32, "bad")
            badT, bbadT = S.sb([128, 96], F32, "badT")
            for l in range(2):
                P.dma("sp", lambda e, l=l: e.dma_start(out=bad[:], in_=self.b_ada[l:l + 1, :].rearrange("o (c p) -> (o c) p", p=128)), writes=[bbad])
                P.op("pe", lambda e: e.transpose(pm[:, 0:96], bad[:], self.ident_f[0:96, 0:96]), reads=[bbad, b_id], writes=[bpm])
                P.op("dve", lambda e: e.tensor_copy(badT[:], pm[:, 0:96]), reads=[bpm], writes=[bbadT])
                b_mod = P.buf()
                wv = self.w_ada[l].rearrange("(k p) n -> p k n", p=128)
                for g in range(24):
                    wt, bwt = wt_r.next()
                    P.dma("sp", lambda e, wt=wt, g=g, wv=wv: e.dma_start(out=wt[:], in_=wv[:, :, g * 512:(g + 1) * 512]), writes=[bwt])
                    pmod, bpmod = pmod_r.next()
                    for j in range(4):
                        for kk in range(DC):
                            P.op("pe", lambda e, pmod=pmod, wt=wt, j=j, kk=kk: e.matmul(pmod[:, j, :], wt[:, kk, j * 128:(j + 1) * 128], self.cactT[:, kk:32:16],
                                                                                       start=(kk == 0), stop=(kk == DC - 1)),
                                 reads=[bwt, b_cact], writes=[bpmod])
                    for w in range(2):
                        P.op("dve", lambda e, pmod=pmod, g=g, w=w, l=l: e.tensor_tensor(out=self.modT[l][:, 4 * g:4 * g + 4, w], in0=pmod[:, :, w],
                                                                                          in1=badT[:, 4 * g:4 * g + 4], op=ALU.add),
                             reads=[bpmod, bbadT], writes=[b_mod])
                for w in range(2):
                    P.op("dve", lambda e, l=l, w=w: e.scalar_tensor_tensor(out=self.A1[l][:, :, w], in0=self.modT[l][:, 16:32, w], scalar=1.0,
                                                                           in1=self.gT[:, (2 * l) * 16:(2 * l + 1) * 16], op0=ALU.add, op1=ALU.mult),
                         reads=[b_mod, b_gT], writes=[P.buf()])
                    P.op("dve", lambda e, l=l, w=w: e.scalar_tensor_tensor(out=self.A2[l][:, :, w], in0=self.modT[l][:, 64:80, w], scalar=1.0,
                                                                           in1=self.gT[:, (2 * l + 1) * 16:(2 * l + 2) * 16], op0=ALU.add, op1=ALU.mult),
                         reads=[b_mod, b_gT], writes=[P.buf()])
                if "MODT" in self.dbg:
                    P.dma("sp", lambda e, l=l: e.dma_start(out=self.MODT[l], in_=self.modT[l][:].rearrange("p a b -> p (a b)")), reads=[b_mod])

    def norm_blocks(self, S, hT, b_hT, Acol, shcol, tok0=0, ntok=T, nring=2, gain_only=None, cb=None, want32=False):
        P = self.P
        xb_r = S.ring_sb(nring, [128, DC, 256], F32, "xb")
        sq_r = S.ring_sb(2, [128, 256], F32, "sq")
        tmp_r = S.ring_sb(2, [128, 256], F32, "tmp")
        rstd_r = S.ring_sb(2, [128, 256], F32, "rstd")
        ss_r = S.ring_ps(1, [128, 256], F32, "ss")
        h32_r = S.ring_sb(2, [128, DC, 256], F32, "h32") if cb is not None else None
        for bi in range(ntok // 256):
            t0 = tok0 + bi * 256
            o0 = bi * 256
            w = 0 if t0 < S_LAT else 1
            xb, bxb = xb_r.next()
            P.dma("sp", lambda e, xb=xb, t0=t0: e.dma_start(out=xb[:], in_=self.XT[:, :, t0:t0 + 256].rearrange("c p t -> p c t")), writes=[bxb])
            ss, bss = ss_r.next()
            for c in range(DC):
                sq, bsq = sq_r.next()
                P.op("act", lambda e, sq=sq, xb=xb, c=c: e.activation(sq[:], xb[:, c, :], AF.Square), reads=[bxb], writes=[bsq])
                P.op("pe", lambda e, ss=ss, sq=sq, c=c: e.matmul(ss[:], self.ones_f[:], sq[:], start=(c == 0), stop=(c == DC - 1)), reads=[bsq], writes=[bss])
            rstd, brs = rstd_r.next()
            P.op("dve", lambda e, rstd=rstd, ss=ss: e.tensor_scalar(rstd[:], ss[:], 1.0 / D, EPS, ALU.mult, ALU.add), reads=[bss], writes=[brs])
            P.op("act", lambda e, rstd=rstd: e.sqrt(rstd[:], rstd[:]), reads=[brs], writes=[brs])
            P.op("dve", lambda e, rstd=rstd: e.reciprocal(rstd[:], rstd[:]), reads=[brs], writes=[brs])
            if cb is not None:
                h32, b_h32 = h32_r.next()
            for c in range(DC):
                tmp, btmp = tmp_r.next()
                P.op("pool", lambda e, tmp=tmp, xb=xb, c=c, rstd=rstd: e.tensor_tensor(out=tmp[:], in0=xb[:, c, :], in1=rstd[:], op=ALU.mult),
                     reads=[bxb, brs], writes=[btmp])
                if gain_only is not None:
                    P.op("dve", lambda e, tmp=tmp, c=c, h32=h32: e.tensor_scalar_mul(h32[:, c, :], tmp[:], gain_only[:, c:c + 1]), reads=[btmp], writes=[b_h32])
                    continue
                if hT is not None:
                    P.op("dve", lambda e, tmp=tmp, c=c, w=w, o0=o0: e.tensor_scalar(hT[:, c, o0:o0 + 256], tmp[:], Acol[:, c, w:w + 1], shcol[:, c, w:w + 1], ALU.mult, ALU.add),
                         reads=[btmp], writes=[b_hT])
                if cb is not None:
                    P.op("act", lambda e, tmp=tmp, c=c, w=w, h32=h32: e.activation(h32[:, c, :], tmp[:], AF.Identity, bias=shcol[:, c, w:w + 1], scale=Acol[:, c, w:w + 1]),
                         reads=[btmp], writes=[b_h32])
            if cb is not None:
                cb(bi, t0, h32, b_h32)

    def stage_inproj(self, l):
        P = self.P
        with self.stage("ip%d" % l) as S:
            hT, b_hT = S.sb([128, DC, T], BF16, "hT")
            self.norm_blocks(S, hT, b_hT, self.A1[l], self.modT[l][:, 0:16, :])
            wv = self.w_in[l].rearrange("(k p) n -> p k n", p=128)
            wt_r = S.ring_sb(2, [128, DC, 512], BF16, "wt")
            ps_r = S.ring_ps(4, [128, 512], F32, "ps")
            st_r = S.ring_sb(2, [128, T], BF16, "stf")
            fm_groups = [(0, 4, 0, False), (3072, 2, 16, False), (7712, 12, 24, True)]
            k = 0
            for (c0, ng, f0, sig) in fm_groups:
                for g in range(ng):
                    wt, bwt = wt_r.next()
                    P.dma("pool", lambda e, wt=wt, c0=c0, g=g: e.dma_start(out=wt[:], in_=wv[:, :, c0 + g * 512:c0 + (g + 1) * 512]), writes=[bwt])
                    for j in range(4):
                        st, bst = st_r.next()
                        for (t0, nb, w) in BLK5:
                            ps, bps = ps_r.next()
                            for kk in range(DC):
                                P.op("pe", lambda e, ps=ps, wt=wt, j=j, kk=kk, t0=t0, nb=nb: e.matmul(ps[:, 0:nb], wt[:, kk, j * 128:(j + 1) * 128], hT[:, kk, t0:t0 + nb],
                                                                                                    start=(kk == 0), stop=(kk == DC - 1)),
                                     reads=[bwt, b_hT], writes=[bps])
                            if sig:
                                P.op("act", lambda e, ps=ps, st=st, t0=t0, nb=nb: e.activation(st[:, t0:t0 + nb], ps[:, 0:nb], AF.Sigmoid), reads=[bps], writes=[bst])
                            elif k % 2 == 0:
                                P.op("dve", lambda e, ps=ps, st=st, t0=t0, nb=nb: e.tensor_copy(st[:, t0:t0 + nb], ps[:, 0:nb]), reads=[bps], writes=[bst])
                            else:
                                P.op("act", lambda e, ps=ps, st=st, t0=t0, nb=nb: e.copy(st[:, t0:t0 + nb], ps[:, 0:nb]), reads=[bps], writes=[bst])
                            k += 1
                        fi = f0 + g * 4 + j
                        P.dma("sp", lambda e, st=st, fi=fi: e.dma_start(out=self.FM[fi], in_=st[:]), reads=[bst])
            wl, bwl = S.sb([128, DC, 32], BF16, "wl")
            P.dma("pool", lambda e: e.dma_start(out=wl[:], in_=wv[:, :, 6144:6176]), writes=[bwl])
            lo, blo = S.sb([32, T], F32, "lo")
            for (t0, nb, w) in BLK5:
                ps, bps = ps_r.next()
                for kk in range(DC):
                    P.op("pe", lambda e, ps=ps, kk=kk, t0=t0, nb=nb: e.matmul(ps[0:32, 0:nb], wl[:, kk, :], hT[:, kk, t0:t0 + nb], start=(kk == 0), stop=(kk == DC - 1)),
                         reads=[bwl, b_hT], writes=[bps])
                P.op("dve", lambda e, ps=ps, t0=t0, nb=nb: e.tensor_copy(lo[:, t0:t0 + nb], ps[0:32, 0:nb]), reads=[bps], writes=[blo])
            P.dma("sp", lambda e: e.dma_start(out=self.LOW, in_=lo[:]), reads=[blo])
            tm_groups = [(2048, 0, "c"), (2560, 512, "c"), (3584, 1024, "c"), (4096, 1536, "c"), (4608, 2048, "c"),
                         (5120, 2560, "c"), (5632, 3072, "c"), (6176, 3584, "q"), (6688, 4096, "q"), (7200, 4608, "kv")]
            stt_r = S.ring_sb(3, [128, 512], BF16, "stt")
            cs_r = S.ring_sb(2, [128, 512], F32, "cs")
            sq_r = S.ring_sb(2, [128, 512], F32, "sq2")
            qn_r = S.ring_sb(2, [128, 512], F32, "qn")
            t1_r = S.ring_sb(2, [128, 256], F32, "t1")
            t2_r = S.ring_sb(2, [128, 256], F32, "t2")
            ss_r = S.ring_sb(2, [128, 4], F32, "ssq")
            qoff = 256 * l
            for (c0, off, kind) in tm_groups:
                wt, bwt = wt_r.next()
                P.dma("pool", lambda e, wt=wt, c0=c0: e.dma_start(out=wt[:], in_=wv[:, :, c0:c0 + 512]), writes=[bwt])
                for tt in range(TT):
                    ps, bps = ps_r.next()
                    for kk in range(DC):
                        P.op("pe", lambda e, ps=ps, wt=wt, kk=kk, tt=tt: e.matmul(ps[:], hT[:, kk, tt * 128:(tt + 1) * 128], wt[:, kk, :], start=(kk == 0), stop=(kk == DC - 1)),
                             reads=[bwt, b_hT], writes=[bps])
                    st, bst = stt_r.next()
                    if kind == "c":
                        if k % 2 == 0:
                            P.op("dve", lambda e, ps=ps, st=st: e.tensor_copy(st[:], ps[:]), reads=[bps], writes=[bst])
                        else:
                            P.op("act", lambda e, ps=ps, st=st: e.copy(st[:], ps[:]), reads=[bps], writes=[bst])
                        k += 1
                    else:
                        nh = 4 if kind == "q" else 2
                        gb = self.gbc[:, qoff:qoff + 128] if kind == "q" else self.gbc[:, qoff + 128:qoff + 256]
                        nw = nh * 128
                        cs, bcs = cs_r.next()
                        P.dma("sp", lambda e, cs=cs, tt=tt: e.dma_start(out=cs[:], in_=self.cs_in[tt * 128:(tt + 1) * 128, :]), writes=[bcs])
                        sq, bsq = sq_r.next()
                        P.op("act", lambda e, sq=sq, ps=ps, nw=nw: e.activation(sq[:, 0:nw], ps[:, 0:nw], AF.Square), reads=[bps], writes=[bsq])
                        ssq, bssq = ss_r.next()
                        P.op("dve", lambda e, ssq=ssq, sq=sq, nh=nh, nw=nw: e.tensor_reduce(out=ssq[:, 0:nh], in_=sq[:, 0:nw].rearrange("p (h d) -> p h d", h=nh), axis=AX.X, op=ALU.add),
                             reads=[bsq], writes=[bssq])
                        P.op("dve", lambda e, ssq=ssq, nh=nh: e.tensor_scalar(ssq[:, 0:nh], ssq[:, 0:nh], 1.0 / 128, EPS, ALU.mult, ALU.add), reads=[bssq], writes=[bssq])
                        P.op("act", lambda e, ssq=ssq, nh=nh: e.sqrt(ssq[:, 0:nh], ssq[:, 0:nh]), reads=[bssq], writes=[bssq])
                        P.op("dve", lambda e, ssq=ssq, nh=nh: e.reciprocal(ssq[:, 0:nh], ssq[:, 0:nh]), reads=[bssq], writes=[bssq])
                        qn, bqn = qn_r.next()
                        for h in range(nh):
                            P.op("dve", lambda e, qn=qn, ps=ps, ssq=ssq, h=h, gb=gb: e.scalar_tensor_tensor(out=qn[:, h * 128:(h + 1) * 128], in0=ps[:, h * 128:(h + 1) * 128],
                                                                                                           scalar=ssq[:, h:h + 1], in1=gb, op0=ALU.mult, op1=ALU.mult),
                                 reads=[bps, bssq], writes=[bqn])
                        qv = qn[:, 0:nw].rearrange("p (h i two) -> p h i two", h=nh, two=2)
                        ev, od = qv[:, :, :, 0], qv[:, :, :, 1]
                        cosv = cs[:, 0:nh * 64].rearrange("p (h i) -> p h i", h=nh)
                        sinv = cs[:, 256:256 + nh * 64].rearrange("p (h i) -> p h i", h=nh)
                        sv = st[:, 0:nw].rearrange("p (h i two) -> p h i two", h=nh, two=2)
                        t1, bt1 = t1_r.next()
                        t2, bt2 = t2_r.next()
                        t1v = t1[:, 0:nh * 64].rearrange("p (h i) -> p h i", h=nh)
                        t2v = t2[:, 0:nh * 64].rearrange("p (h i) -> p h i", h=nh)
                        P.op("pool", lambda e, t1v=t1v, ev=ev, cosv=cosv: e.tensor_tensor(out=t1v, in0=ev, in1=cosv, op=ALU.mult), reads=[bqn, bcs], writes=[bt1])
                        P.op("pool", lambda e, t2v=t2v, od=od, sinv=sinv: e.tensor_tensor(out=t2v, in0=od, in1=sinv, op=ALU.mult), reads=[bqn, bcs], writes=[bt2])
                        P.op("dve", lambda e, sv=sv, t1v=t1v, t2v=t2v: e.tensor_tensor(out=sv[:, :, :, 0], in0=t1v, in1=t2v, op=ALU.subtract), reads=[bt1, bt2], writes=[bst])
                        t1, bt1 = t1_r.next()
                        t2, bt2 = t2_r.next()
                        t1v = t1[:, 0:nh * 64].rearrange("p (h i) -> p h i", h=nh)
                        t2v = t2[:, 0:nh * 64].rearrange("p (h i) -> p h i", h=nh)
                        P.op("pool", lambda e, t1v=t1v, ev=ev, sinv=sinv: e.tensor_tensor(out=t1v, in0=ev, in1=sinv, op=ALU.mult), reads=[bqn, bcs], writes=[bt1])
                        P.op("pool", lambda e, t2v=t2v, od=od, cosv=cosv: e.tensor_tensor(out=t2v, in0=od, in1=cosv, op=ALU.mult), reads=[bqn, bcs], writes=[bt2])
                        P.op("dve", lambda e, sv=sv, t1v=t1v, t2v=t2v: e.tensor_tensor(out=sv[:, :, :, 1], in0=t1v, in1=t2v, op=ALU.add), reads=[bt1, bt2], writes=[bst])
                        if kind == "kv":
                            P.op("act", lambda e, ps=ps, st=st: e.copy(st[:, 256:512], ps[:, 256:512]), reads=[bps], writes=[bst])
                    P.dma("sp", lambda e, st=st, tt=tt, off=off: e.dma_start(out=self.TM[tt * 128:(tt + 1) * 128, off:off + 512], in_=st[:]), reads=[bst])

    def attn_res(self, S, nkmax, nch, dh):
        R = {}
        R["ps_s"] = S.ring_ps(2, [128, 512], F32, "pss")
        R["sc"] = S.ring_sb(2, [128, nkmax], F32, "sc")
        R["p"] = S.ring_sb(2, [128, nkmax], BF16, "p")
        R["ps_t"] = S.ring_ps(2, [128, 4, 128], BF16, "pst")
        R["pT"] = S.ring_sb(2, [128, nch, 128], BF16, "pT")
        R["ps_o"] = S.ring_ps(2, [128, dh], F32, "pso")
        R["sm"] = S.ring_sb(4, [128, 4], F32, "sm")
        R["st"] = S.ring_sb(2, [128, 8, 128], BF16, "fmst")
        R["k"] = 0
        return R

    def attend(self, R, q_ap, segs, vch, scale, out_ap, b_out, deps, dh):
        P = self.P
        sc, bsc = R["sc"].next()
        off = 0
        for (k_ap, n, bias_ap) in segs:
            ps, bps = R["ps_s"].next()
            P.op("pe", lambda e, ps=ps, k_ap=k_ap, n=n: e.matmul(ps[:, 0:n], q_ap, k_ap, start=True, stop=True), reads=deps, writes=[bps])
            if bias_ap is not None:
                P.op("dve", lambda e, ps=ps, n=n, off=off, bias_ap=bias_ap: e.scalar_tensor_tensor(out=sc[:, off:off + n], in0=ps[:, 0:n], scalar=scale, in1=bias_ap,
                                                                                                  op0=ALU.mult, op1=ALU.add), reads=[bps] + deps, writes=[bsc])
            else:
                P.op("act", lambda e, ps=ps, n=n, off=off: e.mul(sc[:, off:off + n], ps[:, 0:n], scale), reads=[bps], writes=[bsc])
            off += n
        NK = off
        sm, bsm = R["sm"].next()
        P.op("dve", lambda e: e.reduce_max(out=sm[:, 0:1], in_=sc[:, 0:NK], axis=AX.X), reads=[bsc], writes=[bsm])
        P.op("dve", lambda e: e.tensor_scalar_mul(sm[:, 1:2], sm[:, 0:1], -1.0), reads=[bsm], writes=[bsm])
        p, bp = R["p"].next()
        P.op("act", lambda e: e.activation(p[:, 0:NK], sc[:, 0:NK], AF.Exp, bias=sm[:, 1:2], scale=1.0), reads=[bsc, bsm], writes=[bp])
        P.op("dve", lambda e: e.reduce_sum(out=sm[:, 2:3], in_=p[:, 0:NK], axis=AX.X), reads=[bp], writes=[bsm])
        P.op("dve", lambda e: e.reciprocal(sm[:, 3:4], sm[:, 2:3]), reads=[bsm], writes=[bsm])
        pT, bpT = R["pT"].next()
        nch = len(vch)
        for g0 in range(0, nch, 4):
            pst, bpst = R["ps_t"].next()
            grp = vch[g0:g0 + 4]
            for j, (v_ap, sz, koff) in enumerate(grp):
                P.op("pe", lambda e, pst=pst, j=j, sz=sz, koff=koff: e.transpose(pst[0:sz, j, :], p[:, koff:koff + sz], self.ident_b[:]), reads=[bp], writes=[bpst])
            ng = len(grp)
            full = all(sz == 128 for (_, sz, _) in grp)
            R["k"] += 1
            if full:
                if R["k"] % 2 == 0:
                    P.op("dve", lambda e, pst=pst, g0=g0, ng=ng: e.tensor_copy(pT[:, g0:g0 + ng, :], pst[:, 0:ng, :]), reads=[bpst], writes=[bpT])
                else:
                    P.op("act", lambda e, pst=pst, g0=g0, ng=ng: e.copy(pT[:, g0:g0 + ng, :], pst[:, 0:ng, :]), reads=[bpst], writes=[bpT])
            else:
                for j, (v_ap, sz, koff) in enumerate(grp):
                    P.op("dve", lambda e, pst=pst, g0=g0, j=j, sz=sz: e.tensor_copy(pT[0:sz, g0 + j, :], pst[0:sz, j, :]), reads=[bpst], writes=[bpT])
        pso, bpso = R["ps_o"].next()
        for ci, (v_ap, sz, koff) in enumerate(vch):
            P.op("pe", lambda e, ci=ci, v_ap=v_ap, sz=sz: e.matmul(pso[:, 0:dh], pT[0:sz, ci, :], v_ap, start=(ci == 0), stop=(ci == nch - 1)),
                 reads=[bpT] + deps, writes=[bpso])
        P.op("dve", lambda e: e.tensor_scalar_mul(out_ap, pso[:, 0:dh], sm[:, 3:4]), reads=[bpso, bsm], writes=[b_out])

    def tm_to_fm(self, R, src_ap, b_src, dst, i):
        P = self.P
        st, bst = R["st"].next()
        for g in range(2):
            pst, bpst = R["ps_t"].next()
            for j in range(4):
                cc = 4 * g + j
                P.op("pe", lambda e, pst=pst, j=j, cc=cc: e.transpose(pst[:, j, :], src_ap[:, cc * 128:(cc + 1) * 128], self.ident_b[:]), reads=[b_src], writes=[bpst])
            if g == 0:
                P.op("dve", lambda e, pst=pst, g=g: e.tensor_copy(st[:, 4 * g:4 * g + 4, :], pst[:]), reads=[bpst], writes=[bst])
            else:
                P.op("act", lambda e, pst=pst, g=g: e.copy(st[:, 4 * g:4 * g + 4, :], pst[:]), reads=[bpst], writes=[bst])
        P.dma("sp", lambda e: e.dma_start(out=dst[:, :, i * 128:(i + 1) * 128].rearrange("c p t -> p c t"), in_=st[:]), reads=[bst])

    def stage_na(self, l, with_ctx):
        P = self.P
        with self.stage("na%d" % l) as S:
            z, bz = S.sb([128, 13824], F32, "z")
            P.op("pool", lambda e: e.memset(z[:], 0.0), writes=[bz])
            b_F = P.buf()
            P.dma("sp", lambda e: e.dma_start(out=self.FB.rearrange("(p n) -> p n", p=128), in_=z[:]), reads=[bz], writes=[b_F])
            for h in range(16):
                src = bass.AP(tensor=self.rpb[l].tensor, offset=h * 465, ap=[[31, 15], [0, 64], [1, 31]])
                dst = bass.AP(tensor=self.FB.tensor, offset=h * 18 * 6144 + 6144, ap=[[6144, 15], [96, 64], [1, 31]])
                P.dma("sp", lambda e, src=src, dst=dst: e.dma_start(out=dst, in_=src), writes=[b_F])
            mk, bmk = S.sb([128, 5, 576], F32, "mk")
            P.dma("sp", lambda e: e.dma_start(out=mk[:], in_=self.mask_in.rearrange("t p n -> p t n")), writes=[bmk])
            R = self.attn_res(S, 832, 7, 64)
            q_r = S.ring_sb(2, [128, T], BF16, "q")
            k_r = S.ring_sb(2, [128, T], BF16, "k")
            v_r = S.ring_sb(2, [128, TT, 128], BF16, "v")
            bias_r = S.ring_sb(2, [128, 5, 576], F32, "bias")
            a_all, b_a = S.sb([128, TT, 1024], BF16, "a_all")
            types = [(7, 8), (5, 8), (3, 9), (3, 8), (1, 8)]
            tiles = list(range(16)) + ([16, 17] if with_ctx else [])
            for hp in range(8):
                qT, bq = q_r.next()
                kT, bk = k_r.next()
                v, bv = v_r.next()
                P.dma("sp", lambda e, qT=qT, hp=hp: e.dma_start(out=qT[:], in_=self.FM[hp]), writes=[bq])
                P.dma("sp", lambda e, kT=kT, hp=hp: e.dma_start(out=kT[:], in_=self.FM[8 + hp]), writes=[bk])
                P.dma("sp", lambda e, v=v, hp=hp: e.dma_start(out=v[:], in_=self.TM[:, hp * 128:(hp + 1) * 128].rearrange("(t p) c -> p t c", p=128)), writes=[bv])
                for sub in range(2):
                    h = 2 * hp + sub
                    bt, bbt = bias_r.next()
                    for ty, (joff, nr) in enumerate(types):
                        for a in range(2):
                            src = bass.AP(tensor=self.FB.tensor, offset=h * 18 * 6144 + (joff - a + 1) * 6144 + 15, ap=[[95, 64], [6144, nr], [1, 64]])
                            dst = bt[a * 64:(a + 1) * 64, ty, 0:nr * 64].rearrange("p (r k) -> p r k", k=64)
                            P.dma("sp", lambda e, src=src, dst=dst: e.dma_start(out=dst, in_=src), reads=[b_F], writes=[bbt])
                    for ty, (joff, nr) in enumerate(types):
                        P.op("pool", lambda e, bt=bt, ty=ty, nr=nr: e.tensor_tensor(out=bt[:, ty, 0:nr * 64], in0=bt[:, ty, 0:nr * 64], in1=mk[:, ty, 0:nr * 64], op=ALU.add),
                             reads=[bbt, bmk], writes=[bbt])
                    ps0 = sub * 64
                    deps = [bq, bk, bv, bbt]
                    for i in tiles:
                        q_ap = qT[ps0:ps0 + 64, i * 128:(i + 1) * 128]
                        segs = []
                        vch = []
                        if i < 16:
                            if i == 0:
                                ty, base, nr = 0, 0, 8
                            elif i == 1:
                                ty, base, nr = 1, 0, 8
                            elif i == 14:
                                ty, base, nr = 3, 24, 8
                            elif i == 15:
                                ty, base, nr = 4, 24, 8
                            else:
                                ty, base, nr = 2, 2 * i - 4, 9
                            t0 = base * 64
                            segs.append((kT[ps0:ps0 + 64, t0:t0 + 512], 512, bt[:, ty, 0:512]))
                            if nr == 9:
                                segs.append((kT[ps0:ps0 + 64, t0 + 512:t0 + 576], 64, bt[:, ty, 512:576]))
                            for m in range(4):
                                vch.append((v[:, base // 2 + m, ps0:ps0 + 64], 128, m * 128))
                            if nr == 9:
                                vch.append((v[0:64, base // 2 + 4, ps0:ps0 + 64], 64, 512))
                        koff = nr * 64 if i < 16 else 0
                        segs.append((kT[ps0:ps0 + 64, S_LAT:T], 256, None))
                        vch.append((v[:, 16, ps0:ps0 + 64], 128, koff))
                        vch.append((v[:, 17, ps0:ps0 + 64], 128, koff + 128))
                        self.attend(R, q_ap, segs, vch, 0.125, a_all[:, i, h * 64:(h + 1) * 64], b_a, deps, 64)
            for i in tiles:
                self.tm_to_fm(R, a_all[:, i, :], b_a, self.AT, i)

    def stage_gqa(self, l, with_ctx):
        P = self.P
        with self.stage("gqa%d" % l) as S:
            R = self.attn_res(S, T, TT, 128)
            qT, bq = S.sb([128, 8, T], BF16, "qT")
            kT, bk = S.sb([128, 2, T], BF16, "kT")
            v, bv = S.sb([128, TT, 256], BF16, "v")
            c_all, b_c = S.sb([128, TT, 1024], BF16, "c_all")
            tq_r = S.ring_sb(2, [128, 1280], BF16, "tq")
            P.dma("sp", lambda e: e.dma_start(out=v[:], in_=self.TM[:, 4864:5120].rearrange("(t p) c -> p t c", p=128)), writes=[bv])
            kk = 0
            for tt in range(TT):
                tq, btq = tq_r.next()
                P.dma("sp", lambda e, tq=tq, tt=tt: e.dma_start(out=tq[:], in_=self.TM[tt * 128:(tt + 1) * 128, 3584:4864]), writes=[btq])
                for (g0, ng) in [(0, 4), (4, 4), (8, 2)]:
                    pst, bpst = R["ps_t"].next()
                    for j in range(ng):
                        cc = g0 + j
                        P.op("pe", lambda e, pst=pst, j=j, cc=cc, tq=tq: e.transpose(pst[:, j, :], tq[:, cc * 128:(cc + 1) * 128], self.ident_b[:]), reads=[btq], writes=[bpst])
                    if g0 < 8:
                        dstv, bd = qT[:, g0:g0 + 4, tt * 128:(tt + 1) * 128], bq
                    else:
                        dstv, bd = kT[:, 0:2, tt * 128:(tt + 1) * 128], bk
                    kk += 1
                    if kk % 2 == 0:
                        P.op("dve", lambda e, pst=pst, dstv=dstv, ng=ng: e.tensor_copy(dstv, pst[:, 0:ng, :]), reads=[bpst], writes=[bd])
                    else:
                        P.op("act", lambda e, pst=pst, dstv=dstv, ng=ng: e.copy(dstv, pst[:, 0:ng, :]), reads=[bpst], writes=[bd])
            tiles = list(range(16)) + ([16, 17] if with_ctx else [])
            deps = [bq, bk, bv]
            sc = 128.0 ** -0.5
            for i in tiles:
                for h in range(8):
                    g = h // 4
                    q_ap = qT[:, h, i * 128:(i + 1) * 128]
                    segs = []
                    vch = []
                    if i < 16:
                        for j in range(4):
                            segs.append((kT[:, g, j * 512:(j + 1) * 512], 512, None))
                        for t in range(16):
                            vch.append((v[:, t, g * 128:(g + 1) * 128], 128, t * 128))
                        koff = S_LAT
                    else:
                        koff = 0
                    segs.append((kT[:, g, S_LAT:T], 256, None))
                    vch.append((v[:, 16, g * 128:(g + 1) * 128], 128, koff))
                    vch.append((v[:, 17, g * 128:(g + 1) * 128], 128, koff + 128))
                    self.attend(R, q_ap, segs, vch, sc, c_all[:, i, h * 128:(h + 1) * 128], b_c, deps, 128)
                self.tm_to_fm(R, c_all[:, i, :], b_c, self.CT, i)

    def stage_gla(self, l, d, with_ctx):
        P = self.P
        with self.stage("gla%d%d" % (l, d)) as S:
            R = {"st": S.ring_sb(2, [128, 8, 128], BF16, "fmst"), "ps_t": S.ring_ps(2, [128, 4, 128], BF16, "pst")}
            lowa, blow = S.sb([17, T], F32, "lowa")
            P.op("pool", lambda e: e.memset(lowa[:], 1.0), writes=[blow])
            P.dma("sp", lambda e: e.dma_start(out=lowa[0:16, :], in_=self.LOW[d * 16:(d + 1) * 16, :]), writes=[blow])
            w2a, bw2 = S.sb([17, 512], F32, "w2a")
            P.dma("sp", lambda e: e.dma_start(out=w2a[0:16, :], in_=self.w_a2[l][d]), writes=[bw2])
            P.dma("sp", lambda e: e.dma_start(out=w2a[16:17, :], in_=self.b_a[l][d:d + 1, :]), writes=[bw2])
            tri, btri = S.sb([128, 4, 128], F32, "tri")
            P.dma("sp", lambda e: e.dma_start(out=tri[:], in_=self.tri_in.rearrange("f p n -> p f n")), writes=[btri])
            qTa, bqa = S.sb([128, 4, T], BF16, "qTa")
            kTa, bka = S.sb([128, 4, T], BF16, "kTa")
            P.dma("sp", lambda e: e.dma_start(out=qTa[:], in_=self.FM[16:20].rearrange("c p t -> p c t")), writes=[bqa])
            P.dma("sp", lambda e: e.dma_start(out=kTa[:], in_=self.FM[20:24].rearrange("c p t -> p c t")), writes=[bka])
            st32, bs32 = S.sb([128, 4, 256], F32, "st32")
            stb, bsb = S.sb([128, 4, 256], BF16, "stb")
            P.op("pool", lambda e: e.memset(st32[:], 0.0), writes=[bs32])
            P.op("pool", lambda e: e.memset(stb[:], 0.0), writes=[bsb])
            bs, ks = (0, 2) if d == 0 else (1, 3)
            lastcol = 127 if d == 0 else 0
            k_r = S.ring_sb(2, [128, 512], BF16, "ktm")
            v_r = S.ring_sb(2, [128, 1024], BF16, "vtm")
            pz_r = S.ring_ps(2, [128, 512], F32, "pz")
            pbT, bpbT = S.ps([128, 4, 128], F32, "pbT")
            pat, bpat = S.ps([128, 128], F32, "pat")
            po, bpo = S.ps([128, 256], F32, "po")
            pst_, bpst_ = S.ps([128, 256], F32, "pstt")
            f5 = {n: S.ring_sb(2, [128, 512], F32, n) for n in ("az", "e1", "l1", "mn", "la", "ek")}
            eT_r = S.ring_sb(2, [128, 4, 128], F32, "eT")
            enT_r = S.ring_sb(2, [128, 4, 128], F32, "enT")
            qt_r = S.ring_sb(2, [128, 4, 128], BF16, "qt")
            kt_r = S.ring_sb(2, [128, 4, 128], BF16, "kt")
            kh_r = S.ring_sb(2, [128, 512], BF16, "kh")
            at_r = S.ring_sb(2, [128, 128], BF16, "at")
            of_r = S.ring_sb(2, [128, 1024], F32, "of")
            if d == 1:
                os_r = S.ring_sb(2, [128, 1024], F32, "osum")
                sq_r = S.ring_sb(1, [128, 1024], F32, "sqo")
                og_r = S.ring_sb(2, [128, 1024], BF16, "og")
                sg_r = S.ring_sb(2, [128, 1024], F32, "sg")
                tn_r = S.ring_sb(1, [128, 1024], F32, "tn")
                bt_r = S.ring_sb(2, [128, 1024], BF16, "btile")
                ssq_r = S.ring_sb(2, [128, 4], F32, "ssq")
            order = [16, 17] + list(range(16)) if d == 0 else [17, 16] + list(range(15, -1, -1))
            for tt in order:
                need_o = with_ctx or tt < 16
                tc = slice(tt * 128, (tt + 1) * 128)
                ktm, bktm = k_r.next()
                vtm, bvtm = v_r.next()
                P.dma("sp", lambda e, ktm=ktm, tc=tc: e.dma_start(out=ktm[:], in_=self.TM[tc, 1024:1536]), writes=[bktm])
                P.dma("sp", lambda e, vtm=vtm, tc=tc: e.dma_start(out=vtm[:], in_=self.TM[tc, 1536:2560]), writes=[bvtm])
                pz, bpz = pz_r.next()
                P.op("pe", lambda e, pz=pz, tc=tc: e.matmul(pz[:], lowa[0:17, tc], w2a[0:17, :], start=True, stop=True), reads=[blow, bw2], writes=[bpz])
                az, baz = f5["az"].next(); e1, be1 = f5["e1"].next(); l1, bl1 = f5["l1"].next()
                mn, bmn = f5["mn"].next(); la, bla = f5["la"].next(); ek, bek = f5["ek"].next()
                P.op("dve", lambda e, mn=mn, pz=pz: e.tensor_scalar_min(mn[:], pz[:], 0.0), reads=[bpz], writes=[bmn])
                P.op("dve", lambda e, az=az, mn=mn, pz=pz: e.scalar_tensor_tensor(out=az[:], in0=mn[:], scalar=2.0, in1=pz[:], op0=ALU.mult, op1=ALU.subtract), reads=[bpz, bmn], writes=[baz])
                P.op("act", lambda e, e1=e1, az=az: e.activation(e1[:], az[:], AF.Exp), reads=[baz], writes=[be1])
                P.op("act", lambda e, l1=l1, e1=e1: e.activation(l1[:], e1[:], AF.Ln, bias=1.0), reads=[be1], writes=[bl1])
                P.op("dve", lambda e, la=la, mn=mn, l1=l1: e.tensor_tensor(out=la[:], in0=mn[:], in1=l1[:], op=ALU.subtract), reads=[bmn, bl1], writes=[bla])
                pk, bpk = pz_r.next()
                P.op("pe", lambda e, pk=pk, la=la: e.matmul(pk[:], tri[:, ks, :], la[:], start=True, stop=True), reads=[btri, bla], writes=[bpk])
                for h in range(4):
                    P.op("pe", lambda e, la=la, h=h: e.matmul(pbT[:, h, :], la[:, h * 128:(h + 1) * 128], tri[:, bs, :], start=True, stop=True), reads=[btri, bla], writes=[bpbT])
                eT, beT = eT_r.next(); enT, benT = enT_r.next()
                P.op("act", lambda e, eT=eT: e.activation(eT[:], pbT[:], AF.Exp, scale=1.0 / 16), reads=[bpbT], writes=[beT])
                P.op("act", lambda e, enT=enT: e.activation(enT[:], pbT[:], AF.Exp, scale=-1.0 / 16), reads=[bpbT], writes=[benT])
                P.op("act", lambda e, ek=ek, pk=pk: e.activation(ek[:], pk[:], AF.Exp, scale=1.0 / 16), reads=[bpk], writes=[bek])
                qt, bqt = qt_r.next(); kt, bkt = kt_r.next(); kh, bkh = kh_r.next()
                P.op("dve", lambda e, qt=qt, eT=eT, tc=tc: e.scalar_tensor_tensor(out=qt[:], in0=qTa[:, :, tc], scalar=128.0 ** -0.5, in1=eT[:], op0=ALU.mult, op1=ALU.mult),
                     reads=[bqa, beT], writes=[bqt])
                P.op("pool", lambda e, kt=kt, enT=enT, tc=tc: e.tensor_tensor(out=kt[:], in0=kTa[:, :, tc], in1=enT[:], op=ALU.mult), reads=[bka, benT], writes=[bkt])
                P.op("pool", lambda e, kh=kh, ktm=ktm, ek=ek: e.tensor_tensor(out=kh[:], in0=ktm[:], in1=ek[:], op=ALU.mult), reads=[bktm, bek], writes=[bkh])
                of, bof = of_r.next()
                if d == 1 and need_o:
                    P.dma("sp", lambda e, of=of, tc=tc: e.dma_start(out=of[:], in_=self.OF[tc, :]), writes=[bof])
                    osum, bos = os_r.next()
                for h in range(4):
                    hv = slice(h * 256, (h + 1) * 256)
                    if need_o:
                        at, bat = at_r.next()
                        P.op("pe", lambda e, kt=kt, qt=qt, h=h: e.matmul(pat[:], kt[:, h, :], qt[:, h, :], start=True, stop=True), reads=[bkt, bqt], writes=[bpat])
                        P.op("dve", lambda e, at=at: e.tensor_tensor(out=at[:], in0=pat[:], in1=tri[:, bs, :], op=ALU.mult), reads=[bpat, btri], writes=[bat])
                        P.op("pe", lambda e, at=at, vtm=vtm, hv=hv: e.matmul(po[:], at[:], vtm[:, hv], start=True, stop=False), reads=[bat, bvtm], writes=[bpo])
                        P.op("pe", lambda e, qt=qt, h=h: e.matmul(po[:], qt[:, h, :], stb[:, h, :], start=False, stop=True), reads=[bqt, bsb], writes=[bpo])
                        if d == 0:
                            P.op("act", lambda e, of=of, hv=hv: e.copy(of[:, hv], po[:]), reads=[bpo], writes=[bof])
                        else:
                            P.op("dve", lambda e, osum=osum, of=of, hv=hv: e.tensor_tensor(out=osum[:, hv], in0=po[:], in1=of[:, hv], op=ALU.add), reads=[bpo, bof], writes=[bos])
                    P.op("pe", lambda e, kh=kh, vtm=vtm, h=h, hv=hv: e.matmul(pst_[:], kh[:, h * 128:(h + 1) * 128], vtm[:, hv], start=True, stop=True), reads=[bkh, bvtm], writes=[bpst_])
                    P.op("dve", lambda e, eT=eT, h=h: e.scalar_tensor_tensor(out=st32[:, h, :], in0=st32[:, h, :], scalar=eT[:, h, lastcol:lastcol + 1], in1=pst_[:],
                                                                             op0=ALU.mult, op1=ALU.add), reads=[bs32, beT, bpst_], writes=[bs32])
                    P.op("act", lambda e, h=h: e.copy(stb[:, h, :], st32[:, h, :]), reads=[bs32], writes=[bsb])
                if not need_o:
                    continue
                if d == 0:
                    P.dma("sp", lambda e, of=of, tc=tc: e.dma_start(out=self.OF[tc, :], in_=of[:]), reads=[bof])
                else:
                    sq, bsq = sq_r.next(); ssq, bssq = ssq_r.next(); og, bog = og_r.next(); sg, bsg = sg_r.next()
                    tn, btn = tn_r.next(); btile, bbt = bt_r.next()
                    P.dma("sp", lambda e, og=og, tc=tc: e.dma_start(out=og[:], in_=self.TM[tc, 2560:3584]), writes=[bog])
                    P.op("act", lambda e, sq=sq, osum=osum: e.activation(sq[:], osum[:], AF.Square), reads=[bos], writes=[bsq])
                    P.op("dve", lambda e, ssq=ssq, sq=sq: e.tensor_reduce(out=ssq[:, 0:4], in_=sq[:].rearrange("p (h d) -> p h d", h=4), axis=AX.X, op=ALU.add), reads=[bsq], writes=[bssq])
                    P.op("dve", lambda e, ssq=ssq: e.tensor_scalar(ssq[:], ssq[:], 1.0 / 256, EPS, ALU.mult, ALU.add), reads=[bssq], writes=[bssq])
                    P.op("act", lambda e, ssq=ssq: e.sqrt(ssq[:], ssq[:]), reads=[bssq], writes=[bssq])
                    P.op("dve", lambda e, ssq=ssq: e.reciprocal(ssq[:], ssq[:]), reads=[bssq], writes=[bssq])
                    P.op("act", lambda e, sg=sg, og=og: e.activation(sg[:], og[:], AF.Silu), reads=[bog], writes=[bsg])
                    gng = self.gbc[:, 512 + 256 * l:768 + 256 * l]
                    for h in range(4):
                        hv = slice(h * 256, (h + 1) * 256)
                        P.op("dve", lambda e, tn=tn, osum=osum, ssq=ssq, h=h, hv=hv: e.scalar_tensor_tensor(out=tn[:, hv], in0=osum[:, hv], scalar=ssq[:, h:h + 1], in1=gng,
                                                                                                          op0=ALU.mult, op1=ALU.mult), reads=[bos, bssq], writes=[btn])
                    P.op("pool", lambda e, btile=btile, tn=tn, sg=sg: e.tensor_tensor(out=btile[:], in0=tn[:], in1=sg[:], op=ALU.mult), reads=[btn, bsg], writes=[bbt])
                    self.tm_to_fm(R, btile[:], bbt, self.BT, tt)

    def stage_merge_a(self, l, with_ctx):
        P = self.P
        with self.stage("mga%d" % l) as S:
            br = []
            for nm, src in (("a", self.AT), ("b", self.BT), ("c", self.CT)):
                t, b = S.sb([128, 8, T], BF16, nm + "T")
                P.dma("sp", lambda e, t=t, src=src: e.dma_start(out=t[:], in_=src.rearrange("c p t -> p c t")), writes=[b])
                br.append((t, b))
            wsrc = [self.w_pa[l], self.w_pb[l], self.w_pc[l]]
            w_r = [S.ring_sb(2, [128, 8, 128], BF16, "wp%d" % i) for i in range(3)]
            g_r = [S.ring_sb(2, [128, T], BF16, "g%d" % i) for i in range(3)]
            ps_r = [S.ring_ps(2, [128, 512], F32, "psm%d" % i) for i in range(3)]
            t_r = [S.ring_sb(2, [128, 512], F32, "tm%d" % i) for i in range(3)]
            m_r = S.ring_sb(2, [128, T], BF16, "mst")
            blks = BLK5 if with_ctx else BLK5[:4]
            for mc in range(DC):
                ws = []
                gs = []
                for i in range(3):
                    wt, bw = w_r[i].next()
                    P.dma("pool", lambda e, wt=wt, i=i, mc=mc: e.dma_start(out=wt[:], in_=wsrc[i].rearrange("(k p) n -> p k n", p=128)[:, :, mc * 128:(mc + 1) * 128]), writes=[bw])
                    ws.append((wt, bw))
                    gt, bg = g_r[i].next()
                    P.dma("sp", lambda e, gt=gt, i=i, mc=mc: e.dma_start(out=gt[:], in_=self.FM[24 + 16 * i + mc]), writes=[bg])
                    gs.append((gt, bg))
                mst, bm = m_r.next()
                for (t0, nb, w) in blks:
                    tts = []
                    for i in range(3):
                        ps, bps = ps_r[i].next()
                        for kk in range(8):
                            P.op("pe", lambda e, ps=ps, i=i, kk=kk, t0=t0, nb=nb, wt=ws[i][0]: e.matmul(ps[:, 0:nb], wt[:, kk, :], br[i][0][:, kk, t0:t0 + nb], start=(kk == 0), stop=(kk == 7)),
                                 reads=[ws[i][1], br[i][1]], writes=[bps])
                        tt_, btt = t_r[i].next()
                        P.op("dve", lambda e, tt_=tt_, ps=ps, nb=nb, t0=t0, gt=gs[i][0]: e.tensor_tensor(out=tt_[:, 0:nb], in0=ps[:, 0:nb], in1=gt[:, t0:t0 + nb], op=ALU.mult),
                             reads=[bps, gs[i][1]], writes=[btt])
                        tts.append((tt_, btt))
                    P.op("pool", lambda e, nb=nb, a=tts[0][0], b=tts[1][0]: e.tensor_tensor(out=a[:, 0:nb], in0=a[:, 0:nb], in1=b[:, 0:nb], op=ALU.add),
                         reads=[tts[0][1], tts[1][1]], writes=[tts[0][1]])
                    P.op("pool", lambda e, nb=nb, t0=t0, mst=mst, a=tts[0][0], c=tts[2][0]: e.tensor_tensor(out=mst[:, t0:t0 + nb], in0=a[:, 0:nb], in1=c[:, 0:nb], op=ALU.add),
                         reads=[tts[0][1], tts[2][1]], writes=[bm])
                ncol = T if with_ctx else S_LAT
                P.dma("sp", lambda e, mst=mst, mc=mc, ncol=ncol: e.dma_start(out=self.MT[mc][:, 0:ncol], in_=mst[:, 0:ncol]), reads=[bm])

    def stage_merge_b(self, l, with_ctx):
        P = self.P
        with self.stage("mgb%d" % l) as S:
            ncol = T if with_ctx else S_LAT
            wo, bwo = S.sb([128, DC, D], BF16, "wo")
            P.dma("pool", lambda e: e.dma_start(out=wo[:], in_=self.w_out[l].rearrange("(k p) n -> p k n", p=128)), writes=[bwo])
            mT, bmT = S.sb([128, DC, T], BF16, "mT")
            P.dma("sp", lambda e: e.dma_start(out=mT[:, :, 0:ncol], in_=self.MT[:, :, 0:ncol].rearrange("c p t -> p c t")), writes=[bmT])
            x_r = S.ring_sb(2, [128, T], F32, "xr")
            ps_r = S.ring_ps(4, [128, 512], F32, "pso")
            blks = BLK5 if with_ctx else BLK5[:4]
            G1 = self.modT[l][:, 32:48, :]
            for mc in range(DC):
                xr, bx = x_r.next()
                P.dma("sp", lambda e, xr=xr, mc=mc: e.dma_start(out=xr[:, 0:ncol], in_=self.XT[mc][:, 0:ncol]), writes=[bx])
                for (t0, nb, w) in blks:
                    ps, bps = ps_r.next()
                    for kk in range(DC):
                        P.op("pe", lambda e, ps=ps, kk=kk, mc=mc, t0=t0, nb=nb: e.matmul(ps[:, 0:nb], wo[:, kk, mc * 128:(mc + 1) * 128], mT[:, kk, t0:t0 + nb], start=(kk == 0), stop=(kk == DC - 1)),
                             reads=[bwo, bmT], writes=[bps])
                    P.op("dve", lambda e, ps=ps, xr=xr, mc=mc, t0=t0, nb=nb, w=w: e.scalar_tensor_tensor(out=xr[:, t0:t0 + nb], in0=ps[:, 0:nb], scalar=G1[:, mc, w:w + 1], in1=xr[:, t0:t0 + nb],
                                                                                                       op0=ALU.mult, op1=ALU.add), reads=[bps, bx], writes=[bx])
                P.dma("sp", lambda e, xr=xr, mc=mc: e.dma_start(out=self.XT[mc][:, 0:ncol], in_=xr[:, 0:ncol]), reads=[bx])

    def stage_ffn_block(self, l, t0, nb, w, moe):
        P = self.P
        with self.stage("ffn%d_%d" % (l, t0)) as S:
            hT, b_hT = S.sb([128, DC, nb], BF16, "hT")
            self.norm_blocks(S, hT, b_hT, self.A2[l], self.modT[l][:, 48:64, :], tok0=t0, ntok=nb, nring=1)
            actT, bact = S.sb([128, FC, nb], BF16, "actT")
            G2 = self.modT[l][:, 80:96, :]
            wg_r = S.ring_sb(2, [128, DC, 256], BF16, "wg")
            wu_r = S.ring_sb(2, [128, DC, 256], BF16, "wu")
            wd_r = S.ring_sb(2, [128, FC, 128], BF16, "wd")
            psg_r = S.ring_ps(2, [128, 512], F32, "psg")
            psu_r = S.ring_ps(2, [128, 512], F32, "psu")
            psd_r = S.ring_ps(2, [128, 512], F32, "psd")
            sg_r = S.ring_sb(2, [128, 512], F32, "sg")
            x_r = S.ring_sb(2, [128, 512], F32, "xr")
            if moe:
                self.wait_conv()
                yacc, byacc = S.sb([128, DC, nb], F32, "yacc")
                g8, bg8 = S.sb([8, nb], F32, "g8")
                P.dma("sp", lambda e: e.dma_start(out=g8[:], in_=self.GATE[:, t0:t0 + nb]), writes=[bg8])
                sel, bsel = S.sb([8, 1024], F32, "sel")
                P.dma("sp", lambda e: e.dma_start(out=sel[:], in_=self.sel_in), writes=[bsel])
                gb_r = S.ring_sb(2, [128, 512], F32, "gb")
                tmp_r = S.ring_sb(2, [128, 512], F32, "tmpa")
                experts = list(range(NE))
            else:
                experts = [None]
            for ei, ex in enumerate(experts):
                if moe:
                    wgs = self.mw_gate[ex].rearrange("(k p) n -> p k n", p=128)
                    wus = self.mw_up[ex].rearrange("(k p) n -> p k n", p=128)
                    wds = self.mw_down[ex].rearrange("(f p) n -> p f n", p=128)
                    gb, bgb = gb_r.next()
                    psb, bpsb = psd_r.next()
                    P.op("pe", lambda e, psb=psb, ex=ex: e.matmul(psb[:, 0:nb], sel[0:8, ex * 128:(ex + 1) * 128], g8[0:8, :], start=True, stop=True), reads=[bsel, bg8], writes=[bpsb])
                    P.op("act", lambda e, psb=psb, gb=gb: e.copy(gb[:, 0:nb], psb[:, 0:nb]), reads=[bpsb], writes=[bgb])
                else:
                    wgs = self.dw_gate.rearrange("(k p) n -> p k n", p=128)
                    wus = self.dw_up.rearrange("(k p) n -> p k n", p=128)
                    wds = self.dw_down.rearrange("(f p) n -> p f n", p=128)
                for fg in range(FC // 2):
                    wg, bwg = wg_r.next()
                    wu, bwu = wu_r.next()
                    if moe:
                        P.dma("sp", lambda e, wg=wg, fg=fg, ex=ex: e.dma_start(out=wg[:], in_=self.WGB[ex, fg].rearrange("p (k n) -> p k n", k=DC)), writes=[bwg])
                        P.dma("sp", lambda e, wu=wu, fg=fg, ex=ex: e.dma_start(out=wu[:], in_=self.WUB[ex, fg].rearrange("p (k n) -> p k n", k=DC)), writes=[bwu])
                    else:
                        P.dma("pool", lambda e, wg=wg, fg=fg, wgs=wgs: e.dma_start(out=wg[:], in_=wgs[:, :, fg * 256:(fg + 1) * 256]), writes=[bwg])
                        P.dma("pool", lambda e, wu=wu, fg=fg, wus=wus: e.dma_start(out=wu[:], in_=wus[:, :, fg * 256:(fg + 1) * 256]), writes=[bwu])
                    for j in range(2):
                        fc = 2 * fg + j
                        psg, bpsg = psg_r.next()
                        psu, bpsu = psu_r.next()
                        for kk in range(DC):
                            P.op("pe", lambda e, psg=psg, wg=wg, kk=kk, j=j: e.matmul(psg[:, 0:nb], wg[:, kk, j * 128:(j + 1) * 128], hT[:, kk, :], start=(kk == 0), stop=(kk == DC - 1)),
                                 reads=[bwg, b_hT], writes=[bpsg])
                        for kk in range(DC):
                            P.op("pe", lambda e, psu=psu, wu=wu, kk=kk, j=j: e.matmul(psu[:, 0:nb], wu[:, kk, j * 128:(j + 1) * 128], hT[:, kk, :], start=(kk == 0), stop=(kk == DC - 1)),
                                 reads=[bwu, b_hT], writes=[bpsu])
                        sg, bsg = sg_r.next()
                        P.op("act", lambda e, sg=sg, psg=psg: e.activation(sg[:, 0:nb], psg[:, 0:nb], AF.Silu), reads=[bpsg], writes=[bsg])
                        if moe:
                            tmp, btmp = tmp_r.next()
                            P.op("dve", lambda e, tmp=tmp, sg=sg, psu=psu: e.tensor_tensor(out=tmp[:, 0:nb], in0=sg[:, 0:nb], in1=psu[:, 0:nb], op=ALU.mult), reads=[bsg, bpsu], writes=[btmp])
                            P.op("pool", lambda e, tmp=tmp, gb=gb, fc=fc: e.tensor_tensor(out=actT[:, fc, :], in0=tmp[:, 0:nb], in1=gb[:, 0:nb], op=ALU.mult), reads=[btmp, bgb], writes=[bact])
                        else:
                            P.op("dve", lambda e, sg=sg, psu=psu, fc=fc: e.tensor_tensor(out=actT[:, fc, :], in0=sg[:, 0:nb], in1=psu[:, 0:nb], op=ALU.mult), reads=[bsg, bpsu], writes=[bact])
                for mc in range(DC):
                    wd, bwd = wd_r.next()
                    if moe:
                        P.dma("sp", lambda e, wd=wd, mc=mc, ex=ex: e.dma_start(out=wd[:], in_=self.WDB[ex, mc].rearrange("p (f n) -> p f n", f=FC)), writes=[bwd])
                    else:
                        P.dma("pool", lambda e, wd=wd, mc=mc, wds=wds: e.dma_start(out=wd[:], in_=wds[:, :, mc * 128:(mc + 1) * 128]), writes=[bwd])
                    psd, bpsd = psd_r.next()
                    for fc in range(FC):
                        P.op("pe", lambda e, psd=psd, wd=wd, fc=fc: e.matmul(psd[:, 0:nb], wd[:, fc, :], actT[:, fc, :], start=(fc == 0), stop=(fc == FC - 1)),
                             reads=[bwd, bact], writes=[bpsd])
                    if moe:
                        if ei == 0:
                            P.op("act", lambda e, psd=psd, mc=mc: e.copy(yacc[:, mc, :], psd[:, 0:nb]), reads=[bpsd], writes=[byacc])
                        else:
                            P.op("dve", lambda e, psd=psd, mc=mc: e.tensor_tensor(out=yacc[:, mc, :], in0=yacc[:, mc, :], in1=psd[:, 0:nb], op=ALU.add), reads=[bpsd, byacc], writes=[byacc])
                    else:
                        xr, bx = x_r.next()
                        P.dma("sp", lambda e, xr=xr, mc=mc: e.dma_start(out=xr[:, 0:nb], in_=self.XT[mc][:, t0:t0 + nb]), writes=[bx])
                        P.op("dve", lambda e, psd=psd, xr=xr, mc=mc: e.scalar_tensor_tensor(out=xr[:, 0:nb], in0=psd[:, 0:nb], scalar=G2[:, mc, w:w + 1], in1=xr[:, 0:nb],
                                                                                             op0=ALU.mult, op1=ALU.add), reads=[bpsd, bx], writes=[bx])
                        P.dma("sp", lambda e, xr=xr, mc=mc: e.dma_start(out=self.XT[mc][:, t0:t0 + nb], in_=xr[:, 0:nb]), reads=[bx])
            if moe:
                for mc in range(DC):
                    xr, bx = x_r.next()
                    P.dma("sp", lambda e, xr=xr, mc=mc: e.dma_start(out=xr[:, 0:nb], in_=self.XT[mc][:, t0:t0 + nb]), writes=[bx])
                    P.op("dve", lambda e, xr=xr, mc=mc: e.scalar_tensor_tensor(out=xr[:, 0:nb], in0=yacc[:, mc, :], scalar=G2[:, mc, w:w + 1], in1=xr[:, 0:nb],
                                                                                op0=ALU.mult, op1=ALU.add), reads=[byacc, bx], writes=[bx])
                    P.dma("sp", lambda e, xr=xr, mc=mc: e.dma_start(out=self.XT[mc][:, t0:t0 + nb], in_=xr[:, 0:nb]), reads=[bx])

    def stage_router(self, l):
        P = self.P
        with self.stage("router") as S:
            rw, brw = S.sb([128, DC, NE], F32, "rw")
            P.dma("sp", lambda e: e.dma_start(out=rw[:], in_=self.router_w.rearrange("(k p) n -> p k n", p=128)), writes=[brw])
            rb, brb = S.sb([1, NE], F32, "rb")
            P.dma("sp", lambda e: e.dma_start(out=rb[:], in_=self.router_b), writes=[brb])
            gsb, bgsb = S.sb([8, S_LAT], F32, "gsb")
            pl_r = S.ring_ps(2, [128, NE], F32, "pl")
            pt_r = S.ring_ps(2, [8, 128], F32, "ptg")
            sm_r = S.ring_sb(2, [128, 8, 8], F32, "rsm")

            def cb(bi, t0, h32, b_h32):
                for s_ in range(2):
                    pl, bpl = pl_r.next()
                    for c in range(DC):
                        P.op("pe", lambda e, pl=pl, c=c, s_=s_, h32=h32: e.matmul(pl[:], h32[:, c, s_ * 128:(s_ + 1) * 128], rw[:, c, :], start=(c == 0), stop=False),
                             reads=[b_h32, brw], writes=[bpl])
                    P.op("pe", lambda e, pl=pl: e.matmul(pl[:], self.ones_f[0:1, :], rb[0:1, :], start=False, stop=True), reads=[brb], writes=[bpl])
                    sm, bsm = sm_r.next()
                    lg, eq1, lg2, eq2, gate, sc_ = sm[:, 0, :], sm[:, 1, :], sm[:, 2, :], sm[:, 3, :], sm[:, 4, :], sm[:, 5, :]
                    P.op("dve", lambda e, lg=lg, pl=pl: e.tensor_copy(lg, pl[:]), reads=[bpl], writes=[bsm])
                    P.op("dve", lambda e, lg=lg, sc_=sc_: e.reduce_max(out=sc_[:, 0:1], in_=lg, axis=AX.X), reads=[bsm], writes=[bsm])
                    P.op("dve", lambda e, sc_=sc_: e.tensor_scalar_mul(sc_[:, 1:2], sc_[:, 0:1], -1.0), reads=[bsm], writes=[bsm])
                    P.op("act", lambda e, lg=lg, eq1=eq1, sc_=sc_: e.sign(eq1, lg, bias=sc_[:, 1:2]), reads=[bsm], writes=[bsm])
                    P.op("dve", lambda e, eq1=eq1: e.tensor_scalar_add(eq1, eq1, 1.0), reads=[bsm], writes=[bsm])
                    P.op("dve", lambda e, lg=lg, eq1=eq1, lg2=lg2: e.scalar_tensor_tensor(out=lg2, in0=eq1, scalar=-1.0e30, in1=lg, op0=ALU.mult, op1=ALU.add), reads=[bsm], writes=[bsm])
                    P.op("dve", lambda e, lg2=lg2, sc_=sc_: e.reduce_max(out=sc_[:, 2:3], in_=lg2, axis=AX.X), reads=[bsm], writes=[bsm])
                    P.op("dve", lambda e, sc_=sc_: e.tensor_scalar_mul(sc_[:, 3:4], sc_[:, 2:3], -1.0), reads=[bsm], writes=[bsm])
                    P.op("act", lambda e, lg2=lg2, eq2=eq2, sc_=sc_: e.sign(eq2, lg2, bias=sc_[:, 3:4]), reads=[bsm], writes=[bsm])
                    P.op("dve", lambda e, eq2=eq2: e.tensor_scalar_add(eq2, eq2, 1.0), reads=[bsm], writes=[bsm])
                    P.op("dve", lambda e, sc_=sc_: e.tensor_tensor(out=sc_[:, 4:5], in0=sc_[:, 2:3], in1=sc_[:, 0:1], op=ALU.subtract), reads=[bsm], writes=[bsm])
                    P.op("act", lambda e, sc_=sc_: e.activation(sc_[:, 4:5], sc_[:, 4:5], AF.Exp), reads=[bsm], writes=[bsm])
                    P.op("dve", lambda e, sc_=sc_: e.tensor_scalar_add(sc_[:, 5:6], sc_[:, 4:5], 1.0), reads=[bsm], writes=[bsm])
                    P.op("dve", lambda e, sc_=sc_: e.reciprocal(sc_[:, 5:6], sc_[:, 5:6]), reads=[bsm], writes=[bsm])
                    P.op("dve", lambda e, sc_=sc_: e.tensor_tensor(out=sc_[:, 6:7], in0=sc_[:, 4:5], in1=sc_[:, 5:6], op=ALU.mult), reads=[bsm], writes=[bsm])
                    P.op("dve", lambda e, gate=gate, eq1=eq1, sc_=sc_: e.tensor_scalar_mul(gate, eq1, sc_[:, 5:6]), reads=[bsm], writes=[bsm])
                    P.op("dve", lambda e, gate=gate, eq2=eq2, sc_=sc_: e.scalar_tensor_tensor(out=gate, in0=eq2, scalar=sc_[:, 6:7], in1=gate, op0=ALU.mult, op1=ALU.add), reads=[bsm], writes=[bsm])
                    ptg, bptg = pt_r.next()
                    P.op("pe", lambda e, ptg=ptg, gate=gate: e.transpose(ptg[:], gate, self.ident_f[:]), reads=[bsm], writes=[bptg])
                    tcol = t0 + s_ * 128
                    P.op("act", lambda e, ptg=ptg, tcol=tcol: e.copy(gsb[:, tcol:tcol + 128], ptg[:]), reads=[bptg], writes=[bgsb])
            self.norm_blocks(S, None, None, self.A2[l], self.modT[l][:, 48:64, :], tok0=0, ntok=S_LAT, nring=2, cb=cb)
            P.dma("sp", lambda e: e.dma_start(out=self.GATE, in_=gsb[:]), reads=[bgsb])

    def stage_final(self):
        P = self.P
        with self.stage("final") as S:
            pt_r = S.ring_ps(2, [128, 4, 128], F32, "ptf")
            ot_r = S.ring_sb(2, [128, D], F32, "ot")
            cnt = [0]

            def cb(bi, t0, h32, b_h32):
                for s_ in range(2):
                    ot, bot = ot_r.next()
                    for g in range(4):
                        pt, bpt = pt_r.next()
                        for j in range(4):
                            cc = 4 * g + j
                            P.op("pe", lambda e, pt=pt, j=j, cc=cc, s_=s_, h32=h32: e.transpose(pt[:, j, :], h32[:, cc, s_ * 128:(s_ + 1) * 128], self.ident_f[:]), reads=[b_h32], writes=[bpt])
                        cnt[0] += 1
                        if cnt[0] % 2 == 0:
                            P.op("dve", lambda e, pt=pt, ot=ot, g=g: e.tensor_copy(ot[:, g * 512:(g + 1) * 512], pt[:].rearrange("p a b -> p (a b)")), reads=[bpt], writes=[bot])
                        else:
                            P.op("act", lambda e, pt=pt, ot=ot, g=g: e.copy(ot[:, g * 512:(g + 1) * 512], pt[:].rearrange("p a b -> p (a b)")), reads=[bpt], writes=[bot])
                    r0 = t0 + s_ * 128
                    P.dma("sp", lambda e, ot=ot, r0=r0: e.dma_start(out=self.OUT[r0:r0 + 128, :], in_=ot[:]), reads=[bot])
            self.norm_blocks(S, None, None, None, None, tok0=0, ntok=S_LAT, nring=2, gain_only=self.gT[:, 64:80], cb=cb)

    def build_all(self):
        self.stage_prep()
        for l in range(2):
            wc = (l == 0)
            self.stage_inproj(l)
            self.stage_na(l, wc)
            self.stage_gqa(l, wc)
            self.stage_gla(l, 0, wc)
            self.stage_gla(l, 1, wc)
            self.stage_merge_a(l, wc)
            self.stage_merge_b(l, wc)
            if l == 0:
                for (t0, nb, w) in BLK5:
                    self.stage_ffn_block(0, t0, nb, w, False)
            else:
                self.stage_router(1)
                for (t0, nb, w) in BLK5[:4]:
                    self.stage_ffn_block(1, t0, nb, w, True)
        self.stage_final()

    def finish(self):
        self.gst.__exit__(None, None, None)
        return self.nc


def make_consts():
    k = {}
    k["k_ident"] = np.eye(128, dtype=np.float32)
    j = np.arange(128)[:, None]
    i = np.arange(128)[None, :]
    tri = np.zeros((4, 128, 128), np.float32)
    tri[0] = (j <= i)
    tri[1] = (j >= i)
    tri[2] = (j > i)
    tri[3] = (j < i)
    k["k_tri"] = tri
    half = 64
    freqs = (10000.0 ** (-np.arange(0, half, 2, dtype=np.float32) / half)).astype(np.float32)
    t = np.arange(S_LAT)
    row = (t // 64).astype(np.float32)
    col = (t % 64).astype(np.float32)
    ang = np.concatenate([row[:, None] * freqs, col[:, None] * freqs], axis=-1).astype(np.float32)
    cos = np.ones((T, 64), np.float32)
    sin = np.zeros((T, 64), np.float32)
    cos[:S_LAT] = np.cos(ang)
    sin[:S_LAT] = np.sin(ang)
    cs = np.concatenate([np.tile(cos[:, None, :], (1, 4, 1)).reshape(T, 256), np.tile(sin[:, None, :], (1, 4, 1)).reshape(T, 256)], axis=1)
    k["k_cs"] = np.ascontiguousarray(cs, dtype=np.float32)
    mask = np.full((5, 128, 576), NEG, np.float32)
    qc = np.arange(64)
    cstart = np.clip(qc - 8, 0, 48)
    kc = np.arange(64)
    inwin = (kc[None, :] >= cstart[:, None]) & (kc[None, :] < cstart[:, None] + 16)
    for ty, i0 in enumerate([0, 1, 2, 14, 15]):
        rs0 = int(np.clip(2 * i0 - 4, 0, 24))
        rs1 = int(np.clip(2 * i0 + 1 - 4, 0, 24))
        base = rs0
        nr = rs1 + 8 - rs0
        for a in range(2):
            rs = rs0 if a == 0 else rs1
            for rho in range(nr):
                kr = base + rho
                if rs <= kr <= rs + 7:
                    blk = np.where(inwin, 0.0, NEG).astype(np.float32)
                    mask[ty, a * 64:(a + 1) * 64, rho * 64:(rho + 1) * 64] = blk
    k["k_namask"] = mask
    sel = np.zeros((8, 8, 128), np.float32)
    for e in range(8):
        sel[e, e, :] = 1.0
    k["k_sel"] = sel.reshape(8, 1024)
    return k


def make_inmap(inp, b, cfg, consts):
    m = dict(consts)
    f = lambda a: np.ascontiguousarray(a, dtype=np.float32)
    m["x"] = f(inp["x"][b])
    m["ctx"] = f(inp["ctx"][b])
    m["c"] = f(inp["c"][b:b + 1])
    m["c_ctx"] = f(inp["c_ctx"][None, :])
    m["w_ada"] = f(inp["w_ada"])
    m["b_ada"] = f(inp["b_ada"])
    m["norm1_g"] = f(inp["norm1_g"])
    m["norm2_g"] = f(inp["norm2_g"])
    m["final_norm_g"] = f(inp["final_norm_g"][None, :])
    m["gqa_qn_g"] = f(inp["gqa_qn_g"])
    m["gqa_kn_g"] = f(inp["gqa_kn_g"])
    m["gla_norm_g"] = f(inp["gla_norm_g"])
    for l in (cfg["layers"] if cfg.get("mixer", True) else []):
        m["w_in%d" % l] = f(inp["w_in"][l])
        m["na_rpb%d" % l] = f(inp["na_rpb"][l].reshape(16, 15 * 31))
        m["gla_w_a2_%d" % l] = f(inp["gla_w_a2"][l])
        m["gla_b_a%d" % l] = f(inp["gla_b_a"][l])
        m["w_pa%d" % l] = f(inp["w_pa"][l])
        m["w_pb%d" % l] = f(inp["w_pb"][l])
        m["w_pc%d" % l] = f(inp["w_pc"][l])
        m["w_out%d" % l] = f(inp["w_out"][l])
    if cfg.get("ffn", True):
        if 0 in cfg["layers"]:
            m["dense_w_gate"] = f(inp["dense_w_gate"][0])
            m["dense_w_up"] = f(inp["dense_w_up"][0])
            m["dense_w_down"] = f(inp["dense_w_down"][0])
        if 1 in cfg["layers"]:
            m["router_w"] = f(inp["router_w"][0])
            m["router_b"] = f(inp["router_b"])
            m["moe_w_gate"] = f(inp["moe_w_gate"][0])
            m["moe_w_up"] = f(inp["moe_w_up"][0])
            m["moe_w_down"] = f(inp["moe_w_down"][0])
    return m


FULL_CFG = {"layers": [0, 1], "ffn": True, "mixer": True, "debug": []}
N_CORES = 4


def kernel(**inputs):
    cfg = FULL_CFG
    B = Builder(cfg)
    B.declare()
    B.build_all()
    nc = B.finish()
    consts = make_consts()
    in_maps = []
    for b in range(N_CORES):
        m = make_inmap(inputs, b, cfg, consts)
        in_maps.append({k: v for k, v in m.items() if k in B.inputs})
    res = run_bass_kernel_spmd(nc, in_maps, core_ids=list(range(N_CORES)))
    out = np.stack([np.asarray(res.results[b]["out"], dtype=np.float32) for b in range(N_CORES)], 0)
    return out
```

```python
import numpy as np
import ml_dtypes
from contextlib import ExitStack
import concourse.bass as bass
import concourse.mybir as mybir
from concourse.bass_utils import run_bass_kernel_spmd

F32 = mybir.dt.float32
BF16 = mybir.dt.bfloat16
AF = mybir.ActivationFunctionType
ALU = mybir.AluOpType
AX = mybir.AxisListType

D = 2048
DC = 16
S_LAT = 2048
L_CTX = 256
T = S_LAT + L_CTX
TT = T // 128
D_IN = 13856
D_FF = 5632
FC = D_FF // 128
NE = 8
EPS = 1e-6
NEG = -30000.0
HALF = S_LAT // 2
BLK5 = [(0, 512, 0), (512, 512, 0), (1024, 512, 0), (1536, 512, 0), (2048, 256, 1)]


class Sem:
    def __init__(self, h):
        self.h = h
        self.n = 0


class Buf:
    __slots__ = ("name", "w", "r", "dsem")

    def __init__(self, name=""):
        self.name = name
        self.w = None
        self.r = []
        self.dsem = None


class Prog:
    ENG = ("pe", "act", "dve", "pool", "sp")
    ENGN = {"pe": "tensor", "act": "scalar", "dve": "vector", "pool": "gpsimd", "sp": "sync"}

    def __init__(self, nc, stack, n_dma_sems=90):
        self.nc = nc
        self.ops = {e: [] for e in self.ENG}
        self.esem = {e: Sem(stack.enter_context(nc.semaphore("es_" + e))) for e in self.ENG}
        self.seen = {e: {} for e in self.ENG}
        self.free_dsems = [Sem(stack.enter_context(nc.semaphore("ds%d" % i))) for i in range(n_dma_sems)]
        self.stage_bufs = []
        self.bar = Sem(stack.enter_context(nc.semaphore("bar")))
        self.n_ops = 0

    def buf(self, name=""):
        b = Buf(name)
        self.stage_bufs.append(b)
        return b

    def bufs(self, n, name=""):
        return [self.buf("%s%d" % (name, i)) for i in range(n)]

    def _dsem(self, b):
        if b.dsem is None:
            b.dsem = self.free_dsems.pop()
        return b.dsem

    def _waits_for(self, eng, reads, writes):
        need = {}

        def add(m):
            if m is None:
                return
            s, v = m
            if need.get(id(s), (s, 0))[1] < v:
                need[id(s)] = (s, v)
        for b in reads:
            add(b.w)
        for b in writes:
            add(b.w)
            for m in b.r:
                add(m)
        out = []
        seen = self.seen[eng]
        for s, v in need.values():
            if s is self.esem[eng] and eng == "pe":
                continue
            if seen.get(id(s), 0) >= v:
                continue
            seen[id(s)] = v
            out.append((s.h, v))
        return out

    def op(self, eng, emit, reads=(), writes=()):
        waits = self._waits_for(eng, reads, writes)
        s = self.esem[eng]
        s.n += 1
        mark = (s, s.n)
        self.ops[eng].append((waits, emit, [(s.h, 1)]))
        for b in reads:
            b.r.append(mark)
            if len(b.r) > 24:
                b.r = b.r[-24:] if False else b.r
        for b in writes:
            b.w = mark
            b.r = []
        self.n_ops += 1

    def dma(self, q, emit, reads=(), writes=(), n=1):
        waits = self._waits_for(q, reads, writes)
        prim = writes[0] if len(writes) else reads[0]
        s = self._dsem(prim)
        s.n += 16 * n
        mark = (s, s.n)
        self.ops[q].append((waits, emit, [(s.h, 16)]))
        for b in reads:
            b.r.append(mark)
        for b in writes:
            b.w = mark
            b.r = []
        self.n_ops += 1

    def raw_dma(self, q, emit, sem):
        sem.n += 16
        self.ops[q].append(([], emit, [(sem.h, 16)]))
        self.n_ops += 1

    def barrier(self):
        waits = []
        for e in self.ENG:
            if e != "sp" and self.esem[e].n > 0:
                waits.append((self.esem[e].h, self.esem[e].n))
        for b in self.stage_bufs:
            if b.dsem is not None:
                waits.append((b.dsem.h, b.dsem.n))
        self.bar.n += 1
        v = self.bar.n
        self.ops["sp"].append((waits, lambda e: e.nop(), [(self.bar.h, 1)]))
        for e in self.ENG:
            if e != "sp":
                self.ops[e].append(([(self.bar.h, v)], None, []))
        for b in self.stage_bufs:
            if b.dsem is not None:
                self.free_dsems.append(b.dsem)
                b.dsem = None
            b.w = None
            b.r = []
        self.stage_bufs = []
        for e in self.ENG:
            self.seen[e] = {}

    def flush(self):
        nc = self.nc
        with nc.Block() as block:
            for e in self.ENG:
                ops = self.ops[e]

                def body(eng, ops=ops):
                    for waits, emit, incs in ops:
                        for (sh, v) in waits:
                            eng.wait_ge(sh, v)
                        if emit is None:
                            continue
                        r = emit(eng)
                        rs = r if isinstance(r, (list, tuple)) else [r]
                        for ins in rs:
                            for (sh, amt) in incs:
                                ins.then_inc(sh, amt)
                getattr(block, self.ENGN[e])(body)
        self.ops = {e: [] for e in self.ENG}


class Ring:
    def __init__(self, tiles, bufs):
        self.t = tiles
        self.b = bufs
        self.i = 0

    def next(self):
        k = self.i % len(self.t)
        self.i += 1
        return self.t[k], self.b[k]


class Stage:
    def __init__(self, B, name):
        self.B = B
        self.name = name
        self.st = ExitStack()

    def __enter__(self):
        self.st.__enter__()
        self.B.issue_conv(self.name)
        return self

    def __exit__(self, *a):
        self.B.P.barrier()
        self.B.P.flush()
        return self.st.__exit__(*a)

    def sb(self, shape, dt, name=None):
        B = self.B
        B.uid += 1
        t = self.st.enter_context(B.nc.sbuf_tensor("%s_%s%d" % (self.name, name or "t", B.uid), list(shape), dt))
        return t, B.P.buf()

    def ps(self, shape, dt=F32, name=None):
        B = self.B
        B.uid += 1
        t = self.st.enter_context(B.nc.psum_tensor("%s_%s%d" % (self.name, name or "p", B.uid), list(shape), dt))
        return t, B.P.buf()

    def ring_sb(self, n, shape, dt, name=None):
        ts = [self.sb(shape, dt, name) for _ in range(n)]
        return Ring([t for t, _ in ts], [b for _, b in ts])

    def ring_ps(self, n, shape, dt=F32, name=None):
        ts = [self.ps(shape, dt, name) for _ in range(n)]
        return Ring([t for t, _ in ts], [b for _, b in ts])


class Builder:
    def __init__(self, cfg):
        self.cfg = cfg
        self.dbg = set(cfg.get("debug", ()))
        self.nc = bass.Bass("TRN2", target_bir_lowering=False)
        self.gst = ExitStack()
        self.gst.__enter__()
        self.P = Prog(self.nc, self.gst)
        self.uid = 0
        self.inputs = {}
        self.outputs = []

    def din(self, name, shape, dt=F32):
        ap = self.nc.dram_tensor(name, list(shape), dt, kind="ExternalInput").ap()
        self.inputs[name] = ap
        return ap

    def dscr(self, name, shape, dt):
        if name in self.dbg:
            self.outputs.append(name)
            return self.nc.dram_tensor(name, list(shape), dt, kind="ExternalOutput").ap()
        return self.nc.dram_tensor(name, list(shape), dt).ap()

    def gsb(self, name, shape, dt):
        return self.gst.enter_context(self.nc.sbuf_tensor(name, list(shape), dt))

    def stage(self, name):
        return Stage(self, name)

    def setup_conv(self):
        self.conv_sem = self.P.free_dsems.pop()
        self.conv_jobs = []
        if not hasattr(self, "mw_gate"):
            return
        for ex in range(NE):
            wgs = self.mw_gate[ex].rearrange("(k p) n -> p k n", p=128)
            wus = self.mw_up[ex].rearrange("(k p) n -> p k n", p=128)
            wds = self.mw_down[ex].rearrange("(f p) n -> p f n", p=128)
            for fg in range(FC // 2):
                self.conv_jobs.append(lambda e, ex=ex, fg=fg, wgs=wgs: e.dma_start(out=self.WGB[ex, fg].rearrange("p (k n) -> p k n", k=DC), in_=wgs[:, :, fg * 256:(fg + 1) * 256]))
                self.conv_jobs.append(lambda e, ex=ex, fg=fg, wus=wus: e.dma_start(out=self.WUB[ex, fg].rearrange("p (k n) -> p k n", k=DC), in_=wus[:, :, fg * 256:(fg + 1) * 256]))
            for mc in range(DC):
                self.conv_jobs.append(lambda e, ex=ex, mc=mc, wds=wds: e.dma_start(out=self.WDB[ex, mc].rearrange("p (f n) -> p f n", f=FC), in_=wds[:, :, mc * 128:(mc + 1) * 128]))
        self.conv_per_stage = 24

    def issue_conv(self, stage_name):
        if not getattr(self, "conv_jobs", None):
            return
        if stage_name == "prep":
            return
        n = len(self.conv_jobs) if (stage_name.startswith("ffn1") or stage_name == "router") else self.conv_per_stage
        for _ in range(min(n, len(self.conv_jobs))):
            job = self.conv_jobs.pop(0)
            self.P.raw_dma("pool", job, self.conv_sem)

    def wait_conv(self):
        for e in ("sp",):
            self.P.ops[e].append(([(self.conv_sem.h, self.conv_sem.n)], None, []))

    def declare(self):
        c = self.cfg
        L = c["layers"]
        self.x_in = self.din("x", [S_LAT, D])
        self.ctx_in = self.din("ctx", [L_CTX, D])
        self.c_in = self.din("c", [1, D])
        self.cctx_in = self.din("c_ctx", [1, D])
        self.ident_in = self.din("k_ident", [128, 128])
        self.tri_in = self.din("k_tri", [4, 128, 128])
        self.cs_in = self.din("k_cs", [T, 512])
        self.mask_in = self.din("k_namask", [5, 128, 576])
        self.sel_in = self.din("k_sel", [8, 8 * 128])
        self.half_in = self.din("k_half", [128, 2])
        self.w_ada = self.din("w_ada", [2, D, 6 * D])
        self.b_ada = self.din("b_ada", [2, 6 * D])
        self.norm1_g = self.din("norm1_g", [2, D])
        self.norm2_g = self.din("norm2_g", [2, D])
        self.final_g = self.din("final_norm_g", [1, D])
        self.gqa_qn = self.din("gqa_qn_g", [2, 128])
        self.gqa_kn = self.din("gqa_kn_g", [2, 128])
        self.gla_ng = self.din("gla_norm_g", [2, 256])
        self.w_in = {}; self.rpb = {}; self.w_a2 = {}; self.b_a = {}
        self.w_pa = {}; self.w_pb = {}; self.w_pc = {}; self.w_out = {}
        for l in (L if c.get("mixer", True) else []):
            self.w_in[l] = self.din("w_in%d" % l, [D, D_IN])
            self.rpb[l] = self.din("na_rpb%d" % l, [16, 15 * 31])
            self.w_a2[l] = self.din("gla_w_a2_%d" % l, [2, 16, 512])
            self.b_a[l] = self.din("gla_b_a%d" % l, [2, 512])
            self.w_pa[l] = self.din("w_pa%d" % l, [1024, D])
            self.w_pb[l] = self.din("w_pb%d" % l, [1024, D])
            self.w_pc[l] = self.din("w_pc%d" % l, [1024, D])
            self.w_out[l] = self.din("w_out%d" % l, [D, D])
        if 0 in L and c.get("ffn", True):
            self.dw_gate = self.din("dense_w_gate", [D, D_FF])
            self.dw_up = self.din("dense_w_up", [D, D_FF])
            self.dw_down = self.din("dense_w_down", [D_FF, D])
        if 1 in L and c.get("ffn", True):
            self.router_w = self.din("router_w", [D, NE])
            self.router_b = self.din("router_b", [1, NE])
            self.mw_gate = self.din("moe_w_gate", [NE, D, D_FF])
            self.mw_up = self.din("moe_w_up", [NE, D, D_FF])
            self.mw_down = self.din("moe_w_down", [NE, D_FF, D])
        self.XT = self.dscr("XT", [DC, 128, T], F32)
        self.FM = self.dscr("FM", [72, 128, T], BF16)
        self.LOW = self.dscr("LOW", [32, T], F32)
        self.TM = self.dscr("TM", [T, 5120], BF16)
        self.AT = self.dscr("AT", [8, 128, T], BF16)
        self.BT = self.dscr("BT", [8, 128, T], BF16)
        self.CT = self.dscr("CT", [8, 128, T], BF16)
        self.MT = self.dscr("MT", [DC, 128, T], BF16)
        self.OF = self.dscr("OF", [T, 1024], F32)
        self.FB = self.dscr("FB", [16 * 18 * 64 * 96], F32)
        self.MODT = self.dscr("MODT", [2, 128, 96 * 2], F32)
        self.GATE = self.dscr("GATE", [8, HALF], F32)
        self.XH = self.dscr("XH", [DC, 128, HALF], F32)
        if hasattr(self, "mw_gate"):
            self.WGB = self.dscr("WGB", [NE, FC // 2, 128, DC * 256], BF16)
            self.WUB = self.dscr("WUB", [NE, FC // 2, 128, DC * 256], BF16)
            self.WDB = self.dscr("WDB", [NE, DC, 128, FC * 128], BF16)
        self.setup_conv()
        self.OUT = self.nc.dram_tensor("out", [HALF, D], F32, kind="ExternalOutput").ap()
        self.outputs.append("out")
        self.ident_f = self.gsb("ident_f", [128, 128], F32)
        self.ident_b = self.gsb("ident_b", [128, 128], BF16)
        self.ones_f = self.gsb("ones_f", [128, 128], F32)
        self.cactT = self.gsb("cactT", [128, 32], F32)
        self.modT = [self.gsb("modT%d" % l, [128, 96, 2], F32) for l in range(2)]
        self.A1 = [self.gsb("A1_%d" % l, [128, 16, 2], F32) for l in range(2)]
        self.A2 = [self.gsb("A2_%d" % l, [128, 16, 2], F32) for l in range(2)]
        self.gT = self.gsb("gT", [128, 80], F32)
        self.gbc = self.gsb("gbc", [128, 1024], F32)

    def stage_prep(self):
        P = self.P
        nc = self.nc
        with self.stage("prep") as S:
            b_id = P.buf()
            P.dma("sp", lambda e: e.dma_start(out=self.ident_f[:], in_=self.ident_in), writes=[b_id])
            P.op("dve", lambda e: e.tensor_copy(self.ident_b[:], self.ident_f[:]), reads=[b_id], writes=[P.buf()])
            b_ones = P.buf()
            P.op("pool", lambda e: e.memset(self.ones_f[:], 1.0), writes=[b_ones])
            xs_r = S.ring_sb(2, [128, D], F32, "xs")
            xo_r = S.ring_sb(2, [128, DC, 128], F32, "xo")
            pt_r = S.ring_ps(2, [128, 4, 128], F32, "pt")
            k = 0
            for tt in range(TT):
                src = self.x_in[tt * 128:(tt + 1) * 128, :] if tt < 16 else self.ctx_in[(tt - 16) * 128:(tt - 15) * 128, :]
                xs, bxs = xs_r.next()
                xo, bxo = xo_r.next()
                P.dma("sp", lambda e, xs=xs, src=src: e.dma_start(out=xs[:], in_=src), writes=[bxs])
                for g in range(4):
                    pt, bpt = pt_r.next()
                    for j in range(4):
                        cc = 4 * g + j
                        P.op("pe", lambda e, pt=pt, xs=xs, j=j, cc=cc: e.transpose(pt[:, j, :], xs[:, cc * 128:(cc + 1) * 128], self.ident_f[:]),
                             reads=[bxs, b_id], writes=[bpt])
                    if k % 2 == 0:
                        P.op("dve", lambda e, pt=pt, xo=xo, g=g: e.tensor_copy(xo[:, 4 * g:4 * g + 4, :], pt[:]), reads=[bpt], writes=[bxo])
                    else:
                        P.op("act", lambda e, pt=pt, xo=xo, g=g: e.copy(xo[:, 4 * g:4 * g + 4, :], pt[:]), reads=[bpt], writes=[bxo])
                    k += 1
                dst = self.XT[:, :, tt * 128:(tt + 1) * 128].rearrange("c p t -> p c t")
                P.dma("sp", lambda e, xo=xo, dst=dst: e.dma_start(out=dst, in_=xo[:]), reads=[bxo])
            cc_t, bcc = S.sb([32, 128], F32, "cc")
            P.dma("sp", lambda e: e.dma_start(out=cc_t[0:16, :], in_=self.c_in.rearrange("o (c p) -> (o c) p", p=128)), writes=[bcc])
            P.dma("sp", lambda e: e.dma_start(out=cc_t[16:32, :], in_=self.cctx_in.rearrange("o (c p) -> (o c) p", p=128)), writes=[bcc])
            cs_t, bcs = S.sb([32, 128], F32, "cs")
            P.op("act", lambda e: e.activation(cs_t[:], cc_t[:], AF.Silu), reads=[bcc], writes=[bcs])
            pm, bpm = S.ps([128, 128], F32, "pm")
            P.op("pe", lambda e: e.transpose(pm[:, 0:32], cs_t[:], self.ident_f[0:32, 0:32]), reads=[bcs, b_id], writes=[bpm])
            b_cact = P.buf()
            P.op("dve", lambda e: e.tensor_copy(self.cactT[:], pm[:, 0:32]), reads=[bpm], writes=[b_cact])
            gr, bgr = S.sb([80, 128], F32, "gr")
            srcs = [self.norm1_g[0:1, :], self.norm2_g[0:1, :], self.norm1_g[1:2, :], self.norm2_g[1:2, :], self.final_g[0:1, :]]
            for v, sap in enumerate(srcs):
                P.dma("sp", lambda e, v=v, sap=sap: e.dma_start(out=gr[v * 16:(v + 1) * 16, :], in_=sap.rearrange("o (c p) -> (o c) p", p=128)), writes=[bgr])
            P.op("pe", lambda e: e.transpose(pm[:, 0:80], gr[:], self.ident_f[0:80, 0:80]), reads=[bgr, b_id], writes=[bpm])
            b_gT = P.buf()
            P.op("dve", lambda e: e.tensor_copy(self.gT[:], pm[:, 0:80]), reads=[bpm], writes=[b_gT])
            grow, bgrow = S.sb([1, 1024], F32, "grow")
            rs = [(self.gqa_qn[0:1, :], 0, 128), (self.gqa_kn[0:1, :], 128, 128), (self.gqa_qn[1:2, :], 256, 128),
                  (self.gqa_kn[1:2, :], 384, 128), (self.gla_ng[0:1, :], 512, 256), (self.gla_ng[1:2, :], 768, 256)]
            for sap, o, n in rs:
                P.dma("sp", lambda e, sap=sap, o=o, n=n: e.dma_start(out=grow[0:1, o:o + n], in_=sap), writes=[bgrow])
            pb, bpb = S.ps([128, 512], F32, "pb")
            b_gbc = P.buf()
            for hh in range(2):
                P.op("pe", lambda e, hh=hh: e.matmul(pb[:], self.ones_f[0:1, :], grow[0:1, hh * 512:(hh + 1) * 512], start=True, stop=True),
                     reads=[bgrow, b_ones], writes=[bpb])
                P.op("dve", lambda e, hh=hh: e.tensor_copy(self.gbc[:, hh * 512:(hh + 1) * 512], pb[:]), reads=[bpb], writes=[b_gbc])
            wt_r = S.ring_sb(2, [128, DC, 512], F32, "wada")
            pmod_r = S.ring_ps(2, [128, 4, 2], F32, "pmod")
            bad, bbad = S.sb([96, 128], F32, "bad")
            badT, bbadT = S.sb([128, 96], F32, "badT")
            for l in range(2):
                P.dma("sp", lambda e, l=l: e.dma_start(out=bad[:], in_=self.b_ada[l:l + 1, :].rearrange("o (c p) -> (o c) p", p=128)), writes=[bbad])
                P.op("pe", lambda e: e.transpose(pm[:, 0:96], bad[:], self.ident_f[0:96, 0:96]), reads=[bbad, b_id], writes=[bpm])
                P.op("dve", lambda e: e.tensor_copy(badT[:], pm[:, 0:96]), reads=[bpm], writes=[bbadT])
                b_mod = P.buf()
                wv = self.w_ada[l].rearrange("(k p) n -> p k n", p=128)
                for g in range(24):
                    wt, bwt = wt_r.next()
                    P.dma("sp", lambda e, wt=wt, g=g, wv=wv: e.dma_start(out=wt[:], in_=wv[:, :, g * 512:(g + 1) * 512]), writes=[bwt])
                    pmod, bpmod = pmod_r.next()
                    for j in range(4):
                        for kk in range(DC):
                            P.op("pe", lambda e, pmod=pmod, wt=wt, j=j, kk=kk: e.matmul(pmod[:, j, :], wt[:, kk, j * 128:(j + 1) * 128], self.cactT[:, kk:32:16],
                                                                                       start=(kk == 0), stop=(kk == DC - 1)),
                                 reads=[bwt, b_cact], writes=[bpmod])
                    for w in range(2):
                        P.op("dve", lambda e, pmod=pmod, g=g, w=w, l=l: e.tensor_tensor(out=self.modT[l][:, 4 * g:4 * g + 4, w], in0=pmod[:, :, w],
                                                                                          in1=badT[:, 4 * g:4 * g + 4], op=ALU.add),
                             reads=[bpmod, bbadT], writes=[b_mod])
                for w in range(2):
                    P.op("dve", lambda e, l=l, w=w: e.scalar_tensor_tensor(out=self.A1[l][:, :, w], in0=self.modT[l][:, 16:32, w], scalar=1.0,
                                                                           in1=self.gT[:, (2 * l) * 16:(2 * l + 1) * 16], op0=ALU.add, op1=ALU.mult),
                         reads=[b_mod, b_gT], writes=[P.buf()])
                    P.op("dve", lambda e, l=l, w=w: e.scalar_tensor_tensor(out=self.A2[l][:, :, w], in0=self.modT[l][:, 64:80, w], scalar=1.0,
                                                                           in1=self.gT[:, (2 * l + 1) * 16:(2 * l + 2) * 16], op0=ALU.add, op1=ALU.mult),
                         reads=[b_mod, b_gT], writes=[P.buf()])
                if "MODT" in self.dbg:
                    P.dma("sp", lambda e, l=l: e.dma_start(out=self.MODT[l], in_=self.modT[l][:].rearrange("p a b -> p (a b)")), reads=[b_mod])

    def norm_blocks(self, S, hT, b_hT, Acol, shcol, tok0=0, ntok=T, nring=2, gain_only=None, cb=None, want32=False, src=None):
        P = self.P
        XS = self.XT if src is None else src
        xb_r = S.ring_sb(nring, [128, DC, 256], F32, "xb")
        sq_r = S.ring_sb(2, [128, 256], F32, "sq")
        tmp_r = S.ring_sb(2, [128, 256], F32, "tmp")
        rstd_r = S.ring_sb(2, [128, 256], F32, "rstd")
        ss_r = S.ring_ps(1, [128, 256], F32, "ss")
        h32_r = S.ring_sb(2, [128, DC, 256], F32, "h32") if cb is not None else None
        for bi in range(ntok // 256):
            t0 = tok0 + bi * 256
            o0 = bi * 256
            w = 0 if t0 < S_LAT else 1
            xb, bxb = xb_r.next()
            P.dma("sp", lambda e, xb=xb, t0=t0: e.dma_start(out=xb[:], in_=XS[:, :, t0:t0 + 256].rearrange("c p t -> p c t")), writes=[bxb])
            ss, bss = ss_r.next()
            for c in range(DC):
                sq, bsq = sq_r.next()
                P.op("act", lambda e, sq=sq, xb=xb, c=c: e.activation(sq[:], xb[:, c, :], AF.Square), reads=[bxb], writes=[bsq])
                P.op("pe", lambda e, ss=ss, sq=sq, c=c: e.matmul(ss[:], self.ones_f[:], sq[:], start=(c == 0), stop=(c == DC - 1)), reads=[bsq], writes=[bss])
            rstd, brs = rstd_r.next()
            P.op("dve", lambda e, rstd=rstd, ss=ss: e.tensor_scalar(rstd[:], ss[:], 1.0 / D, EPS, ALU.mult, ALU.add), reads=[bss], writes=[brs])
            P.op("act", lambda e, rstd=rstd: e.sqrt(rstd[:], rstd[:]), reads=[brs], writes=[brs])
            P.op("dve", lambda e, rstd=rstd: e.reciprocal(rstd[:], rstd[:]), reads=[brs], writes=[brs])
            if cb is not None:
                h32, b_h32 = h32_r.next()
            for c in range(DC):
                tmp, btmp = tmp_r.next()
                P.op("pool", lambda e, tmp=tmp, xb=xb, c=c, rstd=rstd: e.tensor_tensor(out=tmp[:], in0=xb[:, c, :], in1=rstd[:], op=ALU.mult),
                     reads=[bxb, brs], writes=[btmp])
                if gain_only is not None:
                    P.op("dve", lambda e, tmp=tmp, c=c, h32=h32: e.tensor_scalar_mul(h32[:, c, :], tmp[:], gain_only[:, c:c + 1]), reads=[btmp], writes=[b_h32])
                    continue
                if hT is not None:
                    P.op("dve", lambda e, tmp=tmp, c=c, w=w, o0=o0: e.tensor_scalar(hT[:, c, o0:o0 + 256], tmp[:], Acol[:, c, w:w + 1], shcol[:, c, w:w + 1], ALU.mult, ALU.add),
                         reads=[btmp], writes=[b_hT])
                if cb is not None:
                    P.op("act", lambda e, tmp=tmp, c=c, w=w, h32=h32: e.activation(h32[:, c, :], tmp[:], AF.Identity, bias=shcol[:, c, w:w + 1], scale=Acol[:, c, w:w + 1]),
                         reads=[btmp], writes=[b_h32])
            if cb is not None:
                cb(bi, t0, h32, b_h32)

    def stage_inproj(self, l):
        P = self.P
        with self.stage("ip%d" % l) as S:
            hT, b_hT = S.sb([128, DC, T], BF16, "hT")
            self.norm_blocks(S, hT, b_hT, self.A1[l], self.modT[l][:, 0:16, :])
            wv = self.w_in[l].rearrange("(k p) n -> p k n", p=128)
            wt_r = S.ring_sb(2, [128, DC, 512], BF16, "wt")
            ps_r = S.ring_ps(4, [128, 512], F32, "ps")
            st_r = S.ring_sb(2, [128, T], BF16, "stf")
            fm_groups = [(0, 4, 0, False), (3072, 2, 16, False), (7712, 12, 24, True)]
            k = 0
            for (c0, ng, f0, sig) in fm_groups:
                for g in range(ng):
                    wt, bwt = wt_r.next()
                    P.dma("pool", lambda e, wt=wt, c0=c0, g=g: e.dma_start(out=wt[:], in_=wv[:, :, c0 + g * 512:c0 + (g + 1) * 512]), writes=[bwt])
                    for j in range(4):
                        st, bst = st_r.next()
                        for (t0, nb, w) in BLK5:
                            ps, bps = ps_r.next()
                            for kk in range(DC):
                                P.op("pe", lambda e, ps=ps, wt=wt, j=j, kk=kk, t0=t0, nb=nb: e.matmul(ps[:, 0:nb], wt[:, kk, j * 128:(j + 1) * 128], hT[:, kk, t0:t0 + nb],
                                                                                                    start=(kk == 0), stop=(kk == DC - 1)),
                                     reads=[bwt, b_hT], writes=[bps])
                            if sig:
                                P.op("act", lambda e, ps=ps, st=st, t0=t0, nb=nb: e.activation(st[:, t0:t0 + nb], ps[:, 0:nb], AF.Sigmoid), reads=[bps], writes=[bst])
                            elif k % 2 == 0:
                                P.op("dve", lambda e, ps=ps, st=st, t0=t0, nb=nb: e.tensor_copy(st[:, t0:t0 + nb], ps[:, 0:nb]), reads=[bps], writes=[bst])
                            else:
                                P.op("act", lambda e, ps=ps, st=st, t0=t0, nb=nb: e.copy(st[:, t0:t0 + nb], ps[:, 0:nb]), reads=[bps], writes=[bst])
                            k += 1
                        fi = f0 + g * 4 + j
                        P.dma("sp", lambda e, st=st, fi=fi: e.dma_start(out=self.FM[fi], in_=st[:]), reads=[bst])
            wl, bwl = S.sb([128, DC, 32], BF16, "wl")
            P.dma("pool", lambda e: e.dma_start(out=wl[:], in_=wv[:, :, 6144:6176]), writes=[bwl])
            lo, blo = S.sb([32, T], F32, "lo")
            for (t0, nb, w) in BLK5:
                ps, bps = ps_r.next()
                for kk in range(DC):
                    P.op("pe", lambda e, ps=ps, kk=kk, t0=t0, nb=nb: e.matmul(ps[0:32, 0:nb], wl[:, kk, :], hT[:, kk, t0:t0 + nb], start=(kk == 0), stop=(kk == DC - 1)),
                         reads=[bwl, b_hT], writes=[bps])
                P.op("dve", lambda e, ps=ps, t0=t0, nb=nb: e.tensor_copy(lo[:, t0:t0 + nb], ps[0:32, 0:nb]), reads=[bps], writes=[blo])
            P.dma("sp", lambda e: e.dma_start(out=self.LOW, in_=lo[:]), reads=[blo])
            tm_groups = [(2048, 0, "c"), (2560, 512, "c"), (3584, 1024, "c"), (4096, 1536, "c"), (4608, 2048, "c"),
                         (5120, 2560, "c"), (5632, 3072, "c"), (6176, 3584, "q"), (6688, 4096, "q"), (7200, 4608, "kv")]
            stt_r = S.ring_sb(3, [128, 512], BF16, "stt")
            cs_r = S.ring_sb(2, [128, 512], F32, "cs")
            sq_r = S.ring_sb(2, [128, 512], F32, "sq2")
            qn_r = S.ring_sb(2, [128, 512], F32, "qn")
            t1_r = S.ring_sb(2, [128, 256], F32, "t1")
            t2_r = S.ring_sb(2, [128, 256], F32, "t2")
            ss_r = S.ring_sb(2, [128, 4], F32, "ssq")
            qoff = 256 * l
            for (c0, off, kind) in tm_groups:
                wt, bwt = wt_r.next()
                P.dma("pool", lambda e, wt=wt, c0=c0: e.dma_start(out=wt[:], in_=wv[:, :, c0:c0 + 512]), writes=[bwt])
                for tt in range(TT):
                    ps, bps = ps_r.next()
                    for kk in range(DC):
                        P.op("pe", lambda e, ps=ps, wt=wt, kk=kk, tt=tt: e.matmul(ps[:], hT[:, kk, tt * 128:(tt + 1) * 128], wt[:, kk, :], start=(kk == 0), stop=(kk == DC - 1)),
                             reads=[bwt, b_hT], writes=[bps])
                    st, bst = stt_r.next()
                    if kind == "c":
                        if k % 2 == 0:
                            P.op("dve", lambda e, ps=ps, st=st: e.tensor_copy(st[:], ps[:]), reads=[bps], writes=[bst])
                        else:
                            P.op("act", lambda e, ps=ps, st=st: e.copy(st[:], ps[:]), reads=[bps], writes=[bst])
                        k += 1
                    else:
                        nh = 4 if kind == "q" else 2
                        gb = self.gbc[:, qoff:qoff + 128] if kind == "q" else self.gbc[:, qoff + 128:qoff + 256]
                        nw = nh * 128
                        cs, bcs = cs_r.next()
                        P.dma("sp", lambda e, cs=cs, tt=tt: e.dma_start(out=cs[:], in_=self.cs_in[tt * 128:(tt + 1) * 128, :]), writes=[bcs])
                        sq, bsq = sq_r.next()
                        P.op("act", lambda e, sq=sq, ps=ps, nw=nw: e.activation(sq[:, 0:nw], ps[:, 0:nw], AF.Square), reads=[bps], writes=[bsq])
                        ssq, bssq = ss_r.next()
                        P.op("dve", lambda e, ssq=ssq, sq=sq, nh=nh, nw=nw: e.tensor_reduce(out=ssq[:, 0:nh], in_=sq[:, 0:nw].rearrange("p (h d) -> p h d", h=nh), axis=AX.X, op=ALU.add),
                             reads=[bsq], writes=[bssq])
                        P.op("dve", lambda e, ssq=ssq, nh=nh: e.tensor_scalar(ssq[:, 0:nh], ssq[:, 0:nh], 1.0 / 128, EPS, ALU.mult, ALU.add), reads=[bssq], writes=[bssq])
                        P.op("act", lambda e, ssq=ssq, nh=nh: e.sqrt(ssq[:, 0:nh], ssq[:, 0:nh]), reads=[bssq], writes=[bssq])
                        P.op("dve", lambda e, ssq=ssq, nh=nh: e.reciprocal(ssq[:, 0:nh], ssq[:, 0:nh]), reads=[bssq], writes=[bssq])
                        qn, bqn = qn_r.next()
                        for h in range(nh):
                            P.op("dve", lambda e, qn=qn, ps=ps, ssq=ssq, h=h, gb=gb: e.scalar_tensor_tensor(out=qn[:, h * 128:(h + 1) * 128], in0=ps[:, h * 128:(h + 1) * 128],
                                                                                                           scalar=ssq[:, h:h + 1], in1=gb, op0=ALU.mult, op1=ALU.mult),
                                 reads=[bps, bssq], writes=[bqn])
                        qv = qn[:, 0:nw].rearrange("p (h i two) -> p h i two", h=nh, two=2)
                        ev, od = qv[:, :, :, 0], qv[:, :, :, 1]
                        cosv = cs[:, 0:nh * 64].rearrange("p (h i) -> p h i", h=nh)
                        sinv = cs[:, 256:256 + nh * 64].rearrange("p (h i) -> p h i", h=nh)
                        sv = st[:, 0:nw].rearrange("p (h i two) -> p h i two", h=nh, two=2)
                        t1, bt1 = t1_r.next()
                        t2, bt2 = t2_r.next()
                        t1v = t1[:, 0:nh * 64].rearrange("p (h i) -> p h i", h=nh)
                        t2v = t2[:, 0:nh * 64].rearrange("p (h i) -> p h i", h=nh)
                        P.op("pool", lambda e, t1v=t1v, ev=ev, cosv=cosv: e.tensor_tensor(out=t1v, in0=ev, in1=cosv, op=ALU.mult), reads=[bqn, bcs], writes=[bt1])
                        P.op("pool", lambda e, t2v=t2v, od=od, sinv=sinv: e.tensor_tensor(out=t2v, in0=od, in1=sinv, op=ALU.mult), reads=[bqn, bcs], writes=[bt2])
                        P.op("dve", lambda e, sv=sv, t1v=t1v, t2v=t2v: e.tensor_tensor(out=sv[:, :, :, 0], in0=t1v, in1=t2v, op=ALU.subtract), reads=[bt1, bt2], writes=[bst])
                        t1, bt1 = t1_r.next()
                        t2, bt2 = t2_r.next()
                        t1v = t1[:, 0:nh * 64].rearrange("p (h i) -> p h i", h=nh)
                        t2v = t2[:, 0:nh * 64].rearrange("p (h i) -> p h i", h=nh)
                        P.op("pool", lambda e, t1v=t1v, ev=ev, sinv=sinv: e.tensor_tensor(out=t1v, in0=ev, in1=sinv, op=ALU.mult), reads=[bqn, bcs], writes=[bt1])
                        P.op("pool", lambda e, t2v=t2v, od=od, cosv=cosv: e.tensor_tensor(out=t2v, in0=od, in1=cosv, op=ALU.mult), reads=[bqn, bcs], writes=[bt2])
                        P.op("dve", lambda e, sv=sv, t1v=t1v, t2v=t2v: e.tensor_tensor(out=sv[:, :, :, 1], in0=t1v, in1=t2v, op=ALU.add), reads=[bt1, bt2], writes=[bst])
                        if kind == "kv":
                            P.op("act", lambda e, ps=ps, st=st: e.copy(st[:, 256:512], ps[:, 256:512]), reads=[bps], writes=[bst])
                    P.dma("sp", lambda e, st=st, tt=tt, off=off: e.dma_start(out=self.TM[tt * 128:(tt + 1) * 128, off:off + 512], in_=st[:]), reads=[bst])

    def attn_res(self, S, nkmax, nch, dh):
        R = {}
        R["ps_s"] = S.ring_ps(2, [128, 512], F32, "pss")
        R["sc"] = S.ring_sb(2, [128, nkmax], F32, "sc")
        R["p"] = S.ring_sb(2, [128, nkmax], BF16, "p")
        R["ps_t"] = S.ring_ps(2, [128, 4, 128], BF16, "pst")
        R["pT"] = S.ring_sb(2, [128, nch, 128], BF16, "pT")
        R["ps_o"] = S.ring_ps(2, [128, dh], F32, "pso")
        R["sm"] = S.ring_sb(4, [128, 4], F32, "sm")
        R["st"] = S.ring_sb(2, [128, 8, 128], BF16, "fmst")
        R["k"] = 0
        return R

    def attend(self, R, q_ap, segs, vch, scale, out_ap, b_out, deps, dh):
        P = self.P
        sc, bsc = R["sc"].next()
        off = 0
        for (k_ap, n, bias_ap) in segs:
            ps, bps = R["ps_s"].next()
            P.op("pe", lambda e, ps=ps, k_ap=k_ap, n=n: e.matmul(ps[:, 0:n], q_ap, k_ap, start=True, stop=True), reads=deps, writes=[bps])
            if bias_ap is not None:
                P.op("dve", lambda e, ps=ps, n=n, off=off, bias_ap=bias_ap: e.scalar_tensor_tensor(out=sc[:, off:off + n], in0=ps[:, 0:n], scalar=scale, in1=bias_ap,
                                                                                                  op0=ALU.mult, op1=ALU.add), reads=[bps] + deps, writes=[bsc])
            else:
                P.op("act", lambda e, ps=ps, n=n, off=off: e.mul(sc[:, off:off + n], ps[:, 0:n], scale), reads=[bps], writes=[bsc])
            off += n
        NK = off
        sm, bsm = R["sm"].next()
        P.op("dve", lambda e: e.reduce_max(out=sm[:, 0:1], in_=sc[:, 0:NK], axis=AX.X), reads=[bsc], writes=[bsm])
        P.op("dve", lambda e: e.tensor_scalar_mul(sm[:, 1:2], sm[:, 0:1], -1.0), reads=[bsm], writes=[bsm])
        p, bp = R["p"].next()
        P.op("act", lambda e: e.activation(p[:, 0:NK], sc[:, 0:NK], AF.Exp, bias=sm[:, 1:2], scale=1.0), reads=[bsc, bsm], writes=[bp])
        P.op("dve", lambda e: e.reduce_sum(out=sm[:, 2:3], in_=p[:, 0:NK], axis=AX.X), reads=[bp], writes=[bsm])
        P.op("dve", lambda e: e.reciprocal(sm[:, 3:4], sm[:, 2:3]), reads=[bsm], writes=[bsm])
        pT, bpT = R["pT"].next()
        nch = len(vch)
        for g0 in range(0, nch, 4):
            pst, bpst = R["ps_t"].next()
            grp = vch[g0:g0 + 4]
            for j, (v_ap, sz, koff) in enumerate(grp):
                P.op("pe", lambda e, pst=pst, j=j, sz=sz, koff=koff: e.transpose(pst[0:sz, j, :], p[:, koff:koff + sz], self.ident_b[:]), reads=[bp], writes=[bpst])
            ng = len(grp)
            full = all(sz == 128 for (_, sz, _) in grp)
            R["k"] += 1
            if full:
                if R["k"] % 2 == 0:
                    P.op("dve", lambda e, pst=pst, g0=g0, ng=ng: e.tensor_copy(pT[:, g0:g0 + ng, :], pst[:, 0:ng, :]), reads=[bpst], writes=[bpT])
                else:
                    P.op("act", lambda e, pst=pst, g0=g0, ng=ng: e.copy(pT[:, g0:g0 + ng, :], pst[:, 0:ng, :]), reads=[bpst], writes=[bpT])
            else:
                for j, (v_ap, sz, koff) in enumerate(grp):
                    P.op("dve", lambda e, pst=pst, g0=g0, j=j, sz=sz: e.tensor_copy(pT[0:sz, g0 + j, :], pst[0:sz, j, :]), reads=[bpst], writes=[bpT])
        pso, bpso = R["ps_o"].next()
        for ci, (v_ap, sz, koff) in enumerate(vch):
            P.op("pe", lambda e, ci=ci, v_ap=v_ap, sz=sz: e.matmul(pso[:, 0:dh], pT[0:sz, ci, :], v_ap, start=(ci == 0), stop=(ci == nch - 1)),
                 reads=[bpT] + deps, writes=[bpso])
        P.op("dve", lambda e: e.tensor_scalar_mul(out_ap, pso[:, 0:dh], sm[:, 3:4]), reads=[bpso, bsm], writes=[b_out])

    def tm_to_fm(self, R, src_ap, b_src, dst, i):
        P = self.P
        st, bst = R["st"].next()
        for g in range(2):
            pst, bpst = R["ps_t"].next()
            for j in range(4):
                cc = 4 * g + j
                P.op("pe", lambda e, pst=pst, j=j, cc=cc: e.transpose(pst[:, j, :], src_ap[:, cc * 128:(cc + 1) * 128], self.ident_b[:]), reads=[b_src], writes=[bpst])
            if g == 0:
                P.op("dve", lambda e, pst=pst, g=g: e.tensor_copy(st[:, 4 * g:4 * g + 4, :], pst[:]), reads=[bpst], writes=[bst])
            else:
                P.op("act", lambda e, pst=pst, g=g: e.copy(st[:, 4 * g:4 * g + 4, :], pst[:]), reads=[bpst], writes=[bst])
        P.dma("sp", lambda e: e.dma_start(out=dst[:, :, i * 128:(i + 1) * 128].rearrange("c p t -> p c t"), in_=st[:]), reads=[bst])

    def stage_na(self, l, with_ctx):
        P = self.P
        with self.stage("na%d" % l) as S:
            z, bz = S.sb([128, 13824], F32, "z")
            P.op("pool", lambda e: e.memset(z[:], 0.0), writes=[bz])
            b_F = P.buf()
            P.dma("sp", lambda e: e.dma_start(out=self.FB.rearrange("(p n) -> p n", p=128), in_=z[:]), reads=[bz], writes=[b_F])
            for h in range(16):
                src = bass.AP(tensor=self.rpb[l].tensor, offset=h * 465, ap=[[31, 15], [0, 64], [1, 31]])
                dst = bass.AP(tensor=self.FB.tensor, offset=h * 18 * 6144 + 6144, ap=[[6144, 15], [96, 64], [1, 31]])
                P.dma("sp", lambda e, src=src, dst=dst: e.dma_start(out=dst, in_=src), writes=[b_F])
            mk, bmk = S.sb([128, 5, 576], F32, "mk")
            P.dma("sp", lambda e: e.dma_start(out=mk[:], in_=self.mask_in.rearrange("t p n -> p t n")), writes=[bmk])
            R = self.attn_res(S, 832, 7, 64)
            q_r = S.ring_sb(2, [128, T], BF16, "q")
            k_r = S.ring_sb(2, [128, T], BF16, "k")
            v_r = S.ring_sb(2, [128, TT, 128], BF16, "v")
            bias_r = S.ring_sb(2, [128, 5, 576], F32, "bias")
            a_all, b_a = S.sb([128, TT, 1024], BF16, "a_all")
            types = [(7, 8), (5, 8), (3, 9), (3, 8), (1, 8)]
            tiles = list(range(16)) + ([16, 17] if with_ctx else [])
            for hp in range(8):
                qT, bq = q_r.next()
                kT, bk = k_r.next()
                v, bv = v_r.next()
                P.dma("sp", lambda e, qT=qT, hp=hp: e.dma_start(out=qT[:], in_=self.FM[hp]), writes=[bq])
                P.dma("sp", lambda e, kT=kT, hp=hp: e.dma_start(out=kT[:], in_=self.FM[8 + hp]), writes=[bk])
                P.dma("sp", lambda e, v=v, hp=hp: e.dma_start(out=v[:], in_=self.TM[:, hp * 128:(hp + 1) * 128].rearrange("(t p) c -> p t c", p=128)), writes=[bv])
                for sub in range(2):
                    h = 2 * hp + sub
                    bt, bbt = bias_r.next()
                    for ty, (joff, nr) in enumerate(types):
                        for a in range(2):
                            src = bass.AP(tensor=self.FB.tensor, offset=h * 18 * 6144 + (joff - a + 1) * 6144 + 15, ap=[[95, 64], [6144, nr], [1, 64]])
                            dst = bt[a * 64:(a + 1) * 64, ty, 0:nr * 64].rearrange("p (r k) -> p r k", k=64)
                            P.dma("sp", lambda e, src=src, dst=dst: e.dma_start(out=dst, in_=src), reads=[b_F], writes=[bbt])
                    for ty, (joff, nr) in enumerate(types):
                        P.op("pool", lambda e, bt=bt, ty=ty, nr=nr: e.tensor_tensor(out=bt[:, ty, 0:nr * 64], in0=bt[:, ty, 0:nr * 64], in1=mk[:, ty, 0:nr * 64], op=ALU.add),
                             reads=[bbt, bmk], writes=[bbt])
                    ps0 = sub * 64
                    deps = [bq, bk, bv, bbt]
                    for i in tiles:
                        q_ap = qT[ps0:ps0 + 64, i * 128:(i + 1) * 128]
                        segs = []
                        vch = []
                        if i < 16:
                            if i == 0:
                                ty, base, nr = 0, 0, 8
                            elif i == 1:
                                ty, base, nr = 1, 0, 8
                            elif i == 14:
                                ty, base, nr = 3, 24, 8
                            elif i == 15:
                                ty, base, nr = 4, 24, 8
                            else:
                                ty, base, nr = 2, 2 * i - 4, 9
                            t0 = base * 64
                            segs.append((kT[ps0:ps0 + 64, t0:t0 + 512], 512, bt[:, ty, 0:512]))
                            if nr == 9:
                                segs.append((kT[ps0:ps0 + 64, t0 + 512:t0 + 576], 64, bt[:, ty, 512:576]))
                            for m in range(4):
                                vch.append((v[:, base // 2 + m, ps0:ps0 + 64], 128, m * 128))
                            if nr == 9:
                                vch.append((v[0:64, base // 2 + 4, ps0:ps0 + 64], 64, 512))
                        koff = nr * 64 if i < 16 else 0
                        segs.append((kT[ps0:ps0 + 64, S_LAT:T], 256, None))
                        vch.append((v[:, 16, ps0:ps0 + 64], 128, koff))
                        vch.append((v[:, 17, ps0:ps0 + 64], 128, koff + 128))
                        self.attend(R, q_ap, segs, vch, 0.125, a_all[:, i, h * 64:(h + 1) * 64], b_a, deps, 64)
            for i in tiles:
                self.tm_to_fm(R, a_all[:, i, :], b_a, self.AT, i)

    def stage_gqa(self, l, with_ctx):
        P = self.P
        with self.stage("gqa%d" % l) as S:
            R = self.attn_res(S, T, TT, 128)
            qT, bq = S.sb([128, 8, T], BF16, "qT")
            kT, bk = S.sb([128, 2, T], BF16, "kT")
            v, bv = S.sb([128, TT, 256], BF16, "v")
            c_all, b_c = S.sb([128, TT, 1024], BF16, "c_all")
            tq_r = S.ring_sb(2, [128, 1280], BF16, "tq")
            P.dma("sp", lambda e: e.dma_start(out=v[:], in_=self.TM[:, 4864:5120].rearrange("(t p) c -> p t c", p=128)), writes=[bv])
            kk = 0
            for tt in range(TT):
                tq, btq = tq_r.next()
                P.dma("sp", lambda e, tq=tq, tt=tt: e.dma_start(out=tq[:], in_=self.TM[tt * 128:(tt + 1) * 128, 3584:4864]), writes=[btq])
                for (g0, ng) in [(0, 4), (4, 4), (8, 2)]:
                    pst, bpst = R["ps_t"].next()
                    for j in range(ng):
                        cc = g0 + j
                        P.op("pe", lambda e, pst=pst, j=j, cc=cc, tq=tq: e.transpose(pst[:, j, :], tq[:, cc * 128:(cc + 1) * 128], self.ident_b[:]), reads=[btq], writes=[bpst])
                    if g0 < 8:
                        dstv, bd = qT[:, g0:g0 + 4, tt * 128:(tt + 1) * 128], bq
                    else:
                        dstv, bd = kT[:, 0:2, tt * 128:(tt + 1) * 128], bk
                    kk += 1
                    if kk % 2 == 0:
                        P.op("dve", lambda e, pst=pst, dstv=dstv, ng=ng: e.tensor_copy(dstv, pst[:, 0:ng, :]), reads=[bpst], writes=[bd])
                    else:
                        P.op("act", lambda e, pst=pst, dstv=dstv, ng=ng: e.copy(dstv, pst[:, 0:ng, :]), reads=[bpst], writes=[bd])
            tiles = list(range(16)) + ([16, 17] if with_ctx else [])
            deps = [bq, bk, bv]
            sc = 128.0 ** -0.5
            for i in tiles:
                for h in range(8):
                    g = h // 4
                    q_ap = qT[:, h, i * 128:(i + 1) * 128]
                    segs = []
                    vch = []
                    if i < 16:
                        for j in range(4):
                            segs.append((kT[:, g, j * 512:(j + 1) * 512], 512, None))
                        for t in range(16):
                            vch.append((v[:, t, g * 128:(g + 1) * 128], 128, t * 128))
                        koff = S_LAT
                    else:
                        koff = 0
                    segs.append((kT[:, g, S_LAT:T], 256, None))
                    vch.append((v[:, 16, g * 128:(g + 1) * 128], 128, koff))
                    vch.append((v[:, 17, g * 128:(g + 1) * 128], 128, koff + 128))
                    self.attend(R, q_ap, segs, vch, sc, c_all[:, i, h * 128:(h + 1) * 128], b_c, deps, 128)
                self.tm_to_fm(R, c_all[:, i, :], b_c, self.CT, i)

    def stage_gla(self, l, d, with_ctx):
        P = self.P
        with self.stage("gla%d%d" % (l, d)) as S:
            R = {"st": S.ring_sb(2, [128, 8, 128], BF16, "fmst"), "ps_t": S.ring_ps(2, [128, 4, 128], BF16, "pst")}
            lowa, blow = S.sb([17, T], F32, "lowa")
            P.op("pool", lambda e: e.memset(lowa[:], 1.0), writes=[blow])
            P.dma("sp", lambda e: e.dma_start(out=lowa[0:16, :], in_=self.LOW[d * 16:(d + 1) * 16, :]), writes=[blow])
            w2a, bw2 = S.sb([17, 512], F32, "w2a")
            P.dma("sp", lambda e: e.dma_start(out=w2a[0:16, :], in_=self.w_a2[l][d]), writes=[bw2])
            P.dma("sp", lambda e: e.dma_start(out=w2a[16:17, :], in_=self.b_a[l][d:d + 1, :]), writes=[bw2])
            tri, btri = S.sb([128, 4, 128], F32, "tri")
            P.dma("sp", lambda e: e.dma_start(out=tri[:], in_=self.tri_in.rearrange("f p n -> p f n")), writes=[btri])
            qTa, bqa = S.sb([128, 4, T], BF16, "qTa")
            kTa, bka = S.sb([128, 4, T], BF16, "kTa")
            P.dma("sp", lambda e: e.dma_start(out=qTa[:], in_=self.FM[16:20].rearrange("c p t -> p c t")), writes=[bqa])
            P.dma("sp", lambda e: e.dma_start(out=kTa[:], in_=self.FM[20:24].rearrange("c p t -> p c t")), writes=[bka])
            st32, bs32 = S.sb([128, 4, 256], F32, "st32")
            stb, bsb = S.sb([128, 4, 256], BF16, "stb")
            P.op("pool", lambda e: e.memset(st32[:], 0.0), writes=[bs32])
            P.op("pool", lambda e: e.memset(stb[:], 0.0), writes=[bsb])
            bs, ks = (0, 2) if d == 0 else (1, 3)
            lastcol = 127 if d == 0 else 0
            k_r = S.ring_sb(2, [128, 512], BF16, "ktm")
            v_r = S.ring_sb(2, [128, 1024], BF16, "vtm")
            pz_r = S.ring_ps(2, [128, 512], F32, "pz")
            pbT, bpbT = S.ps([128, 4, 128], F32, "pbT")
            pat, bpat = S.ps([128, 128], F32, "pat")
            po, bpo = S.ps([128, 256], F32, "po")
            pst_, bpst_ = S.ps([128, 256], F32, "pstt")
            f5 = {n: S.ring_sb(2, [128, 512], F32, n) for n in ("az", "e1", "l1", "mn", "la", "ek")}
            eT_r = S.ring_sb(2, [128, 4, 128], F32, "eT")
            enT_r = S.ring_sb(2, [128, 4, 128], F32, "enT")
            qt_r = S.ring_sb(2, [128, 4, 128], BF16, "qt")
            kt_r = S.ring_sb(2, [128, 4, 128], BF16, "kt")
            kh_r = S.ring_sb(2, [128, 512], BF16, "kh")
            at_r = S.ring_sb(2, [128, 128], BF16, "at")
            of_r = S.ring_sb(2, [128, 1024], F32, "of")
            if d == 1:
                os_r = S.ring_sb(2, [128, 1024], F32, "osum")
                sq_r = S.ring_sb(1, [128, 1024], F32, "sqo")
                og_r = S.ring_sb(2, [128, 1024], BF16, "og")
                sg_r = S.ring_sb(2, [128, 1024], F32, "sg")
                tn_r = S.ring_sb(1, [128, 1024], F32, "tn")
                bt_r = S.ring_sb(2, [128, 1024], BF16, "btile")
                ssq_r = S.ring_sb(2, [128, 4], F32, "ssq")
            order = [16, 17] + list(range(16)) if d == 0 else [17, 16] + list(range(15, -1, -1))
            for tt in order:
                need_o = with_ctx or tt < 16
                tc = slice(tt * 128, (tt + 1) * 128)
                ktm, bktm = k_r.next()
                vtm, bvtm = v_r.next()
                P.dma("sp", lambda e, ktm=ktm, tc=tc: e.dma_start(out=ktm[:], in_=self.TM[tc, 1024:1536]), writes=[bktm])
                P.dma("sp", lambda e, vtm=vtm, tc=tc: e.dma_start(out=vtm[:], in_=self.TM[tc, 1536:2560]), writes=[bvtm])
                pz, bpz = pz_r.next()
                P.op("pe", lambda e, pz=pz, tc=tc: e.matmul(pz[:], lowa[0:17, tc], w2a[0:17, :], start=True, stop=True), reads=[blow, bw2], writes=[bpz])
                az, baz = f5["az"].next(); e1, be1 = f5["e1"].next(); l1, bl1 = f5["l1"].next()
                mn, bmn = f5["mn"].next(); la, bla = f5["la"].next(); ek, bek = f5["ek"].next()
                P.op("dve", lambda e, mn=mn, pz=pz: e.tensor_scalar_min(mn[:], pz[:], 0.0), reads=[bpz], writes=[bmn])
                P.op("dve", lambda e, az=az, mn=mn, pz=pz: e.scalar_tensor_tensor(out=az[:], in0=mn[:], scalar=2.0, in1=pz[:], op0=ALU.mult, op1=ALU.subtract), reads=[bpz, bmn], writes=[baz])
                P.op("act", lambda e, e1=e1, az=az: e.activation(e1[:], az[:], AF.Exp), reads=[baz], writes=[be1])
                P.op("act", lambda e, l1=l1, e1=e1: e.activation(l1[:], e1[:], AF.Ln, bias=1.0), reads=[be1], writes=[bl1])
                P.op("dve", lambda e, la=la, mn=mn, l1=l1: e.tensor_tensor(out=la[:], in0=mn[:], in1=l1[:], op=ALU.subtract), reads=[bmn, bl1], writes=[bla])
                pk, bpk = pz_r.next()
                P.op("pe", lambda e, pk=pk, la=la: e.matmul(pk[:], tri[:, ks, :], la[:], start=True, stop=True), reads=[btri, bla], writes=[bpk])
                for h in range(4):
                    P.op("pe", lambda e, la=la, h=h: e.matmul(pbT[:, h, :], la[:, h * 128:(h + 1) * 128], tri[:, bs, :], start=True, stop=True), reads=[btri, bla], writes=[bpbT])
                eT, beT = eT_r.next(); enT, benT = enT_r.next()
                P.op("act", lambda e, eT=eT: e.activation(eT[:], pbT[:], AF.Exp, scale=1.0 / 16), reads=[bpbT], writes=[beT])
                P.op("act", lambda e, enT=enT: e.activation(enT[:], pbT[:], AF.Exp, scale=-1.0 / 16), reads=[bpbT], writes=[benT])
                P.op("act", lambda e, ek=ek, pk=pk: e.activation(ek[:], pk[:], AF.Exp, scale=1.0 / 16), reads=[bpk], writes=[bek])
                qt, bqt = qt_r.next(); kt, bkt = kt_r.next(); kh, bkh = kh_r.next()
                P.op("dve", lambda e, qt=qt, eT=eT, tc=tc: e.scalar_tensor_tensor(out=qt[:], in0=qTa[:, :, tc], scalar=128.0 ** -0.5, in1=eT[:], op0=ALU.mult, op1=ALU.mult),
                     reads=[bqa, beT], writes=[bqt])
                P.op("pool", lambda e, kt=kt, enT=enT, tc=tc: e.tensor_tensor(out=kt[:], in0=kTa[:, :, tc], in1=enT[:], op=ALU.mult), reads=[bka, benT], writes=[bkt])
                P.op("pool", lambda e, kh=kh, ktm=ktm, ek=ek: e.tensor_tensor(out=kh[:], in0=ktm[:], in1=ek[:], op=ALU.mult), reads=[bktm, bek], writes=[bkh])
                of, bof = of_r.next()
                if d == 1 and need_o:
                    P.dma("sp", lambda e, of=of, tc=tc: e.dma_start(out=of[:], in_=self.OF[tc, :]), writes=[bof])
                    osum, bos = os_r.next()
                for h in range(4):
                    hv = slice(h * 256, (h + 1) * 256)
                    if need_o:
                        at, bat = at_r.next()
                        P.op("pe", lambda e, kt=kt, qt=qt, h=h: e.matmul(pat[:], kt[:, h, :], qt[:, h, :], start=True, stop=True), reads=[bkt, bqt], writes=[bpat])
                        P.op("dve", lambda e, at=at: e.tensor_tensor(out=at[:], in0=pat[:], in1=tri[:, bs, :], op=ALU.mult), reads=[bpat, btri], writes=[bat])
                        P.op("pe", lambda e, at=at, vtm=vtm, hv=hv: e.matmul(po[:], at[:], vtm[:, hv], start=True, stop=False), reads=[bat, bvtm], writes=[bpo])
                        P.op("pe", lambda e, qt=qt, h=h: e.matmul(po[:], qt[:, h, :], stb[:, h, :], start=False, stop=True), reads=[bqt, bsb], writes=[bpo])
                        if d == 0:
                            P.op("act", lambda e, of=of, hv=hv: e.copy(of[:, hv], po[:]), reads=[bpo], writes=[bof])
                        else:
                            P.op("dve", lambda e, osum=osum, of=of, hv=hv: e.tensor_tensor(out=osum[:, hv], in0=po[:], in1=of[:, hv], op=ALU.add), reads=[bpo, bof], writes=[bos])
                    P.op("pe", lambda e, kh=kh, vtm=vtm, h=h, hv=hv: e.matmul(pst_[:], kh[:, h * 128:(h + 1) * 128], vtm[:, hv], start=True, stop=True), reads=[bkh, bvtm], writes=[bpst_])
                    P.op("dve", lambda e, eT=eT, h=h: e.scalar_tensor_tensor(out=st32[:, h, :], in0=st32[:, h, :], scalar=eT[:, h, lastcol:lastcol + 1], in1=pst_[:],
                                                                             op0=ALU.mult, op1=ALU.add), reads=[bs32, beT, bpst_], writes=[bs32])
                    P.op("act", lambda e, h=h: e.copy(stb[:, h, :], st32[:, h, :]), reads=[bs32], writes=[bsb])
                if not need_o:
                    continue
                if d == 0:
                    P.dma("sp", lambda e, of=of, tc=tc: e.dma_start(out=self.OF[tc, :], in_=of[:]), reads=[bof])
                else:
                    sq, bsq = sq_r.next(); ssq, bssq = ssq_r.next(); og, bog = og_r.next(); sg, bsg = sg_r.next()
                    tn, btn = tn_r.next(); btile, bbt = bt_r.next()
                    P.dma("sp", lambda e, og=og, tc=tc: e.dma_start(out=og[:], in_=self.TM[tc, 2560:3584]), writes=[bog])
                    P.op("act", lambda e, sq=sq, osum=osum: e.activation(sq[:], osum[:], AF.Square), reads=[bos], writes=[bsq])
                    P.op("dve", lambda e, ssq=ssq, sq=sq: e.tensor_reduce(out=ssq[:, 0:4], in_=sq[:].rearrange("p (h d) -> p h d", h=4), axis=AX.X, op=ALU.add), reads=[bsq], writes=[bssq])
                    P.op("dve", lambda e, ssq=ssq: e.tensor_scalar(ssq[:], ssq[:], 1.0 / 256, EPS, ALU.mult, ALU.add), reads=[bssq], writes=[bssq])
                    P.op("act", lambda e, ssq=ssq: e.sqrt(ssq[:], ssq[:]), reads=[bssq], writes=[bssq])
                    P.op("dve", lambda e, ssq=ssq: e.reciprocal(ssq[:], ssq[:]), reads=[bssq], writes=[bssq])
                    P.op("act", lambda e, sg=sg, og=og: e.activation(sg[:], og[:], AF.Silu), reads=[bog], writes=[bsg])
                    gng = self.gbc[:, 512 + 256 * l:768 + 256 * l]
                    for h in range(4):
                        hv = slice(h * 256, (h + 1) * 256)
                        P.op("dve", lambda e, tn=tn, osum=osum, ssq=ssq, h=h, hv=hv: e.scalar_tensor_tensor(out=tn[:, hv], in0=osum[:, hv], scalar=ssq[:, h:h + 1], in1=gng,
                                                                                                          op0=ALU.mult, op1=ALU.mult), reads=[bos, bssq], writes=[btn])
                    P.op("pool", lambda e, btile=btile, tn=tn, sg=sg: e.tensor_tensor(out=btile[:], in0=tn[:], in1=sg[:], op=ALU.mult), reads=[btn, bsg], writes=[bbt])
                    self.tm_to_fm(R, btile[:], bbt, self.BT, tt)

    def stage_merge_a(self, l, with_ctx):
        P = self.P
        with self.stage("mga%d" % l) as S:
            br = []
            for nm, src in (("a", self.AT), ("b", self.BT), ("c", self.CT)):
                t, b = S.sb([128, 8, T], BF16, nm + "T")
                P.dma("sp", lambda e, t=t, src=src: e.dma_start(out=t[:], in_=src.rearrange("c p t -> p c t")), writes=[b])
                br.append((t, b))
            wsrc = [self.w_pa[l], self.w_pb[l], self.w_pc[l]]
            w_r = [S.ring_sb(2, [128, 8, 128], BF16, "wp%d" % i) for i in range(3)]
            g_r = [S.ring_sb(2, [128, T], BF16, "g%d" % i) for i in range(3)]
            ps_r = [S.ring_ps(2, [128, 512], F32, "psm%d" % i) for i in range(3)]
            t_r = [S.ring_sb(2, [128, 512], F32, "tm%d" % i) for i in range(3)]
            m_r = S.ring_sb(2, [128, T], BF16, "mst")
            blks = BLK5 if with_ctx else BLK5[:4]
            for mc in range(DC):
                ws = []
                gs = []
                for i in range(3):
                    wt, bw = w_r[i].next()
                    P.dma("pool", lambda e, wt=wt, i=i, mc=mc: e.dma_start(out=wt[:], in_=wsrc[i].rearrange("(k p) n -> p k n", p=128)[:, :, mc * 128:(mc + 1) * 128]), writes=[bw])
                    ws.append((wt, bw))
                    gt, bg = g_r[i].next()
                    P.dma("sp", lambda e, gt=gt, i=i, mc=mc: e.dma_start(out=gt[:], in_=self.FM[24 + 16 * i + mc]), writes=[bg])
                    gs.append((gt, bg))
                mst, bm = m_r.next()
                for (t0, nb, w) in blks:
                    tts = []
                    for i in range(3):
                        ps, bps = ps_r[i].next()
                        for kk in range(8):
                            P.op("pe", lambda e, ps=ps, i=i, kk=kk, t0=t0, nb=nb, wt=ws[i][0]: e.matmul(ps[:, 0:nb], wt[:, kk, :], br[i][0][:, kk, t0:t0 + nb], start=(kk == 0), stop=(kk == 7)),
                                 reads=[ws[i][1], br[i][1]], writes=[bps])
                        tt_, btt = t_r[i].next()
                        P.op("dve", lambda e, tt_=tt_, ps=ps, nb=nb, t0=t0, gt=gs[i][0]: e.tensor_tensor(out=tt_[:, 0:nb], in0=ps[:, 0:nb], in1=gt[:, t0:t0 + nb], op=ALU.mult),
                             reads=[bps, gs[i][1]], writes=[btt])
                        tts.append((tt_, btt))
                    P.op("pool", lambda e, nb=nb, a=tts[0][0], b=tts[1][0]: e.tensor_tensor(out=a[:, 0:nb], in0=a[:, 0:nb], in1=b[:, 0:nb], op=ALU.add),
                         reads=[tts[0][1], tts[1][1]], writes=[tts[0][1]])
                    P.op("pool", lambda e, nb=nb, t0=t0, mst=mst, a=tts[0][0], c=tts[2][0]: e.tensor_tensor(out=mst[:, t0:t0 + nb], in0=a[:, 0:nb], in1=c[:, 0:nb], op=ALU.add),
                         reads=[tts[0][1], tts[2][1]], writes=[bm])
                ncol = T if with_ctx else S_LAT
                P.dma("sp", lambda e, mst=mst, mc=mc, ncol=ncol: e.dma_start(out=self.MT[mc][:, 0:ncol], in_=mst[:, 0:ncol]), reads=[bm])

    def stage_merge_b(self, l, with_ctx):
        P = self.P
        with self.stage("mgb%d" % l) as S:
            ncol = T if with_ctx else S_LAT
            wo, bwo = S.sb([128, DC, D], BF16, "wo")
            P.dma("pool", lambda e: e.dma_start(out=wo[:], in_=self.w_out[l].rearrange("(k p) n -> p k n", p=128)), writes=[bwo])
            mT, bmT = S.sb([128, DC, T], BF16, "mT")
            P.dma("sp", lambda e: e.dma_start(out=mT[:, :, 0:ncol], in_=self.MT[:, :, 0:ncol].rearrange("c p t -> p c t")), writes=[bmT])
            x_r = S.ring_sb(2, [128, T], F32, "xr")
            ps_r = S.ring_ps(4, [128, 512], F32, "pso")
            blks = BLK5 if with_ctx else BLK5[:4]
            G1 = self.modT[l][:, 32:48, :]
            for mc in range(DC):
                xr, bx = x_r.next()
                P.dma("sp", lambda e, xr=xr, mc=mc: e.dma_start(out=xr[:, 0:ncol], in_=self.XT[mc][:, 0:ncol]), writes=[bx])
                for (t0, nb, w) in blks:
                    ps, bps = ps_r.next()
                    for kk in range(DC):
                        P.op("pe", lambda e, ps=ps, kk=kk, mc=mc, t0=t0, nb=nb: e.matmul(ps[:, 0:nb], wo[:, kk, mc * 128:(mc + 1) * 128], mT[:, kk, t0:t0 + nb], start=(kk == 0), stop=(kk == DC - 1)),
                             reads=[bwo, bmT], writes=[bps])
                    P.op("dve", lambda e, ps=ps, xr=xr, mc=mc, t0=t0, nb=nb, w=w: e.scalar_tensor_tensor(out=xr[:, t0:t0 + nb], in0=ps[:, 0:nb], scalar=G1[:, mc, w:w + 1], in1=xr[:, t0:t0 + nb],
                                                                                                       op0=ALU.mult, op1=ALU.add), reads=[bps, bx], writes=[bx])
                P.dma("sp", lambda e, xr=xr, mc=mc: e.dma_start(out=self.XT[mc][:, 0:ncol], in_=xr[:, 0:ncol]), reads=[bx])

    def stage_ffn_block(self, l, t0, nb, w, moe):
        P = self.P
        with self.stage("ffn%d_%d" % (l, t0)) as S:
            hT, b_hT = S.sb([128, DC, nb], BF16, "hT")
            XS = self.XH if moe else self.XT
            self.norm_blocks(S, hT, b_hT, self.A2[l], self.modT[l][:, 48:64, :], tok0=t0, ntok=nb, nring=1, src=XS)
            actT, bact = S.sb([128, FC, nb], BF16, "actT")
            G2 = self.modT[l][:, 80:96, :]
            wg_r = S.ring_sb(2, [128, DC, 256], BF16, "wg")
            wu_r = S.ring_sb(2, [128, DC, 256], BF16, "wu")
            wd_r = S.ring_sb(2, [128, FC, 128], BF16, "wd")
            psg_r = S.ring_ps(2, [128, 512], F32, "psg")
            psu_r = S.ring_ps(2, [128, 512], F32, "psu")
            psd_r = S.ring_ps(2, [128, 512], F32, "psd")
            sg_r = S.ring_sb(2, [128, 512], F32, "sg")
            x_r = S.ring_sb(2, [128, 512], F32, "xr")
            if moe:
                self.wait_conv()
                yacc, byacc = S.sb([128, DC, nb], F32, "yacc")
                g8, bg8 = S.sb([8, nb], F32, "g8")
                P.dma("sp", lambda e: e.dma_start(out=g8[:], in_=self.GATE[:, t0:t0 + nb]), writes=[bg8])
                sel, bsel = S.sb([8, 1024], F32, "sel")
                P.dma("sp", lambda e: e.dma_start(out=sel[:], in_=self.sel_in), writes=[bsel])
                gb_r = S.ring_sb(2, [128, 512], F32, "gb")
                tmp_r = S.ring_sb(2, [128, 512], F32, "tmpa")
                experts = list(range(NE))
            else:
                experts = [None]
            for ei, ex in enumerate(experts):
                if moe:
                    wgs = self.mw_gate[ex].rearrange("(k p) n -> p k n", p=128)
                    wus = self.mw_up[ex].rearrange("(k p) n -> p k n", p=128)
                    wds = self.mw_down[ex].rearrange("(f p) n -> p f n", p=128)
                    gb, bgb = gb_r.next()
                    psb, bpsb = psd_r.next()
                    P.op("pe", lambda e, psb=psb, ex=ex: e.matmul(psb[:, 0:nb], sel[0:8, ex * 128:(ex + 1) * 128], g8[0:8, :], start=True, stop=True), reads=[bsel, bg8], writes=[bpsb])
                    P.op("act", lambda e, psb=psb, gb=gb: e.copy(gb[:, 0:nb], psb[:, 0:nb]), reads=[bpsb], writes=[bgb])
                else:
                    wgs = self.dw_gate.rearrange("(k p) n -> p k n", p=128)
                    wus = self.dw_up.rearrange("(k p) n -> p k n", p=128)
                    wds = self.dw_down.rearrange("(f p) n -> p f n", p=128)
                for fg in range(FC // 2):
                    wg, bwg = wg_r.next()
                    wu, bwu = wu_r.next()
                    if moe:
                        P.dma("sp", lambda e, wg=wg, fg=fg, ex=ex: e.dma_start(out=wg[:], in_=self.WGB[ex, fg].rearrange("p (k n) -> p k n", k=DC)), writes=[bwg])
                        P.dma("sp", lambda e, wu=wu, fg=fg, ex=ex: e.dma_start(out=wu[:], in_=self.WUB[ex, fg].rearrange("p (k n) -> p k n", k=DC)), writes=[bwu])
                    else:
                        P.dma("pool", lambda e, wg=wg, fg=fg, wgs=wgs: e.dma_start(out=wg[:], in_=wgs[:, :, fg * 256:(fg + 1) * 256]), writes=[bwg])
                        P.dma("pool", lambda e, wu=wu, fg=fg, wus=wus: e.dma_start(out=wu[:], in_=wus[:, :, fg * 256:(fg + 1) * 256]), writes=[bwu])
                    for j in range(2):
                        fc = 2 * fg + j
                        psg, bpsg = psg_r.next()
                        psu, bpsu = psu_r.next()
                        for kk in range(DC):
                            P.op("pe", lambda e, psg=psg, wg=wg, kk=kk, j=j: e.matmul(psg[:, 0:nb], wg[:, kk, j * 128:(j + 1) * 128], hT[:, kk, :], start=(kk == 0), stop=(kk == DC - 1)),
                                 reads=[bwg, b_hT], writes=[bpsg])
                        for kk in range(DC):
                            P.op("pe", lambda e, psu=psu, wu=wu, kk=kk, j=j: e.matmul(psu[:, 0:nb], wu[:, kk, j * 128:(j + 1) * 128], hT[:, kk, :], start=(kk == 0), stop=(kk == DC - 1)),
                                 reads=[bwu, b_hT], writes=[bpsu])
                        sg, bsg = sg_r.next()
                        P.op("act", lambda e, sg=sg, psg=psg: e.activation(sg[:, 0:nb], psg[:, 0:nb], AF.Silu), reads=[bpsg], writes=[bsg])
                        if moe:
                            tmp, btmp = tmp_r.next()
                            P.op("dve", lambda e, tmp=tmp, sg=sg, psu=psu: e.tensor_tensor(out=tmp[:, 0:nb], in0=sg[:, 0:nb], in1=psu[:, 0:nb], op=ALU.mult), reads=[bsg, bpsu], writes=[btmp])
                            P.op("pool", lambda e, tmp=tmp, gb=gb, fc=fc: e.tensor_tensor(out=actT[:, fc, :], in0=tmp[:, 0:nb], in1=gb[:, 0:nb], op=ALU.mult), reads=[btmp, bgb], writes=[bact])
                        else:
                            P.op("dve", lambda e, sg=sg, psu=psu, fc=fc: e.tensor_tensor(out=actT[:, fc, :], in0=sg[:, 0:nb], in1=psu[:, 0:nb], op=ALU.mult), reads=[bsg, bpsu], writes=[bact])
                for mc in range(DC):
                    wd, bwd = wd_r.next()
                    if moe:
                        P.dma("sp", lambda e, wd=wd, mc=mc, ex=ex: e.dma_start(out=wd[:], in_=self.WDB[ex, mc].rearrange("p (f n) -> p f n", f=FC)), writes=[bwd])
                    else:
                        P.dma("pool", lambda e, wd=wd, mc=mc, wds=wds: e.dma_start(out=wd[:], in_=wds[:, :, mc * 128:(mc + 1) * 128]), writes=[bwd])
                    psd, bpsd = psd_r.next()
                    for fc in range(FC):
                        P.op("pe", lambda e, psd=psd, wd=wd, fc=fc: e.matmul(psd[:, 0:nb], wd[:, fc, :], actT[:, fc, :], start=(fc == 0), stop=(fc == FC - 1)),
                             reads=[bwd, bact], writes=[bpsd])
                    if moe:
                        if ei == 0:
                            P.op("act", lambda e, psd=psd, mc=mc: e.copy(yacc[:, mc, :], psd[:, 0:nb]), reads=[bpsd], writes=[byacc])
                        else:
                            P.op("dve", lambda e, psd=psd, mc=mc: e.tensor_tensor(out=yacc[:, mc, :], in0=yacc[:, mc, :], in1=psd[:, 0:nb], op=ALU.add), reads=[bpsd, byacc], writes=[byacc])
                    else:
                        xr, bx = x_r.next()
                        P.dma("sp", lambda e, xr=xr, mc=mc: e.dma_start(out=xr[:, 0:nb], in_=XS[mc][:, t0:t0 + nb]), writes=[bx])
                        P.op("dve", lambda e, psd=psd, xr=xr, mc=mc: e.scalar_tensor_tensor(out=xr[:, 0:nb], in0=psd[:, 0:nb], scalar=G2[:, mc, w:w + 1], in1=xr[:, 0:nb],
                                                                                             op0=ALU.mult, op1=ALU.add), reads=[bpsd, bx], writes=[bx])
                        P.dma("sp", lambda e, xr=xr, mc=mc: e.dma_start(out=XS[mc][:, t0:t0 + nb], in_=xr[:, 0:nb]), reads=[bx])
            if moe:
                for mc in range(DC):
                    xr, bx = x_r.next()
                    P.dma("sp", lambda e, xr=xr, mc=mc: e.dma_start(out=xr[:, 0:nb], in_=XS[mc][:, t0:t0 + nb]), writes=[bx])
                    P.op("dve", lambda e, xr=xr, mc=mc: e.scalar_tensor_tensor(out=xr[:, 0:nb], in0=yacc[:, mc, :], scalar=G2[:, mc, w:w + 1], in1=xr[:, 0:nb],
                                                                                op0=ALU.mult, op1=ALU.add), reads=[byacc, bx], writes=[bx])
                    P.dma("sp", lambda e, xr=xr, mc=mc: e.dma_start(out=XS[mc][:, t0:t0 + nb], in_=xr[:, 0:nb]), reads=[bx])

    def stage_router(self, l):
        P = self.P
        with self.stage("router") as S:
            rw, brw = S.sb([128, DC, NE], F32, "rw")
            P.dma("sp", lambda e: e.dma_start(out=rw[:], in_=self.router_w.rearrange("(k p) n -> p k n", p=128)), writes=[brw])
            rb, brb = S.sb([1, NE], F32, "rb")
            P.dma("sp", lambda e: e.dma_start(out=rb[:], in_=self.router_b), writes=[brb])
            gsb, bgsb = S.sb([8, HALF], F32, "gsb")
            pl_r = S.ring_ps(2, [128, NE], F32, "pl")
            pt_r = S.ring_ps(2, [8, 128], F32, "ptg")
            sm_r = S.ring_sb(2, [128, 8, 8], F32, "rsm")

            def cb(bi, t0, h32, b_h32):
                for s_ in range(2):
                    pl, bpl = pl_r.next()
                    for c in range(DC):
                        P.op("pe", lambda e, pl=pl, c=c, s_=s_, h32=h32: e.matmul(pl[:], h32[:, c, s_ * 128:(s_ + 1) * 128], rw[:, c, :], start=(c == 0), stop=False),
                             reads=[b_h32, brw], writes=[bpl])
                    P.op("pe", lambda e, pl=pl: e.matmul(pl[:], self.ones_f[0:1, :], rb[0:1, :], start=False, stop=True), reads=[brb], writes=[bpl])
                    sm, bsm = sm_r.next()
                    lg, eq1, lg2, eq2, gate, sc_ = sm[:, 0, :], sm[:, 1, :], sm[:, 2, :], sm[:, 3, :], sm[:, 4, :], sm[:, 5, :]
                    P.op("dve", lambda e, lg=lg, pl=pl: e.tensor_copy(lg, pl[:]), reads=[bpl], writes=[bsm])
                    P.op("dve", lambda e, lg=lg, sc_=sc_: e.reduce_max(out=sc_[:, 0:1], in_=lg, axis=AX.X), reads=[bsm], writes=[bsm])
                    P.op("dve", lambda e, sc_=sc_: e.tensor_scalar_mul(sc_[:, 1:2], sc_[:, 0:1], -1.0), reads=[bsm], writes=[bsm])
                    P.op("act", lambda e, lg=lg, eq1=eq1, sc_=sc_: e.sign(eq1, lg, bias=sc_[:, 1:2]), reads=[bsm], writes=[bsm])
                    P.op("dve", lambda e, eq1=eq1: e.tensor_scalar_add(eq1, eq1, 1.0), reads=[bsm], writes=[bsm])
                    P.op("dve", lambda e, lg=lg, eq1=eq1, lg2=lg2: e.scalar_tensor_tensor(out=lg2, in0=eq1, scalar=-1.0e30, in1=lg, op0=ALU.mult, op1=ALU.add), reads=[bsm], writes=[bsm])
                    P.op("dve", lambda e, lg2=lg2, sc_=sc_: e.reduce_max(out=sc_[:, 2:3], in_=lg2, axis=AX.X), reads=[bsm], writes=[bsm])
                    P.op("dve", lambda e, sc_=sc_: e.tensor_scalar_mul(sc_[:, 3:4], sc_[:, 2:3], -1.0), reads=[bsm], writes=[bsm])
                    P.op("act", lambda e, lg2=lg2, eq2=eq2, sc_=sc_: e.sign(eq2, lg2, bias=sc_[:, 3:4]), reads=[bsm], writes=[bsm])
                    P.op("dve", lambda e, eq2=eq2: e.tensor_scalar_add(eq2, eq2, 1.0), reads=[bsm], writes=[bsm])
                    P.op("dve", lambda e, sc_=sc_: e.tensor_tensor(out=sc_[:, 4:5], in0=sc_[:, 2:3], in1=sc_[:, 0:1], op=ALU.subtract), reads=[bsm], writes=[bsm])
                    P.op("act", lambda e, sc_=sc_: e.activation(sc_[:, 4:5], sc_[:, 4:5], AF.Exp), reads=[bsm], writes=[bsm])
                    P.op("dve", lambda e, sc_=sc_: e.tensor_scalar_add(sc_[:, 5:6], sc_[:, 4:5], 1.0), reads=[bsm], writes=[bsm])
                    P.op("dve", lambda e, sc_=sc_: e.reciprocal(sc_[:, 5:6], sc_[:, 5:6]), reads=[bsm], writes=[bsm])
                    P.op("dve", lambda e, sc_=sc_: e.tensor_tensor(out=sc_[:, 6:7], in0=sc_[:, 4:5], in1=sc_[:, 5:6], op=ALU.mult), reads=[bsm], writes=[bsm])
                    P.op("dve", lambda e, gate=gate, eq1=eq1, sc_=sc_: e.tensor_scalar_mul(gate, eq1, sc_[:, 5:6]), reads=[bsm], writes=[bsm])
                    P.op("dve", lambda e, gate=gate, eq2=eq2, sc_=sc_: e.scalar_tensor_tensor(out=gate, in0=eq2, scalar=sc_[:, 6:7], in1=gate, op0=ALU.mult, op1=ALU.add), reads=[bsm], writes=[bsm])
                    ptg, bptg = pt_r.next()
                    P.op("pe", lambda e, ptg=ptg, gate=gate: e.transpose(ptg[:], gate, self.ident_f[:]), reads=[bsm], writes=[bptg])
                    tcol = t0 + s_ * 128
                    P.op("act", lambda e, ptg=ptg, tcol=tcol: e.copy(gsb[:, tcol:tcol + 128], ptg[:]), reads=[bptg], writes=[bgsb])
            self.norm_blocks(S, None, None, self.A2[l], self.modT[l][:, 48:64, :], tok0=0, ntok=HALF, nring=2, cb=cb, src=self.XH)
            P.dma("sp", lambda e: e.dma_start(out=self.GATE, in_=gsb[:]), reads=[bgsb])

    def stage_final(self):
        P = self.P
        with self.stage("final") as S:
            pt_r = S.ring_ps(2, [128, 4, 128], F32, "ptf")
            ot_r = S.ring_sb(2, [128, D], F32, "ot")
            cnt = [0]

            def cb(bi, t0, h32, b_h32):
                for s_ in range(2):
                    ot, bot = ot_r.next()
                    for g in range(4):
                        pt, bpt = pt_r.next()
                        for j in range(4):
                            cc = 4 * g + j
                            P.op("pe", lambda e, pt=pt, j=j, cc=cc, s_=s_, h32=h32: e.transpose(pt[:, j, :], h32[:, cc, s_ * 128:(s_ + 1) * 128], self.ident_f[:]), reads=[b_h32], writes=[bpt])
                        cnt[0] += 1
                        if cnt[0] % 2 == 0:
                            P.op("dve", lambda e, pt=pt, ot=ot, g=g: e.tensor_copy(ot[:, g * 512:(g + 1) * 512], pt[:].rearrange("p a b -> p (a b)")), reads=[bpt], writes=[bot])
                        else:
                            P.op("act", lambda e, pt=pt, ot=ot, g=g: e.copy(ot[:, g * 512:(g + 1) * 512], pt[:].rearrange("p a b -> p (a b)")), reads=[bpt], writes=[bot])
                    r0 = t0 + s_ * 128
                    P.dma("sp", lambda e, ot=ot, r0=r0: e.dma_start(out=self.OUT[r0:r0 + 128, :], in_=ot[:]), reads=[bot])
            self.norm_blocks(S, None, None, None, None, tok0=0, ntok=HALF, nring=2, gain_only=self.gT[:, 64:80], cb=cb, src=self.XH)

    def stage_split(self):
        P = self.P
        with self.stage("split") as S:
            hs, bhs = S.sb([128, 2], F32, "hs")
            P.dma("sp", lambda e: e.dma_start(out=hs[:], in_=self.half_in), writes=[bhs])
            xa_r = S.ring_sb(2, [128, HALF], F32, "xa")
            xb_r = S.ring_sb(2, [128, HALF], F32, "xb")
            for c in range(DC):
                xa, bxa = xa_r.next()
                xb, bxb = xb_r.next()
                P.dma("sp", lambda e, xa=xa, c=c: e.dma_start(out=xa[:], in_=self.XT[c][:, 0:HALF]), writes=[bxa])
                P.dma("sp", lambda e, xb=xb, c=c: e.dma_start(out=xb[:], in_=self.XT[c][:, HALF:S_LAT]), writes=[bxb])
                P.op("dve", lambda e, xa=xa: e.tensor_scalar_mul(xa[:], xa[:], hs[:, 0:1]), reads=[bxa, bhs], writes=[bxa])
                P.op("dve", lambda e, xa=xa, xb=xb: e.scalar_tensor_tensor(out=xa[:], in0=xb[:], scalar=hs[:, 1:2], in1=xa[:], op0=ALU.mult, op1=ALU.add), reads=[bxa, bxb, bhs], writes=[bxa])
                P.dma("sp", lambda e, xa=xa, c=c: e.dma_start(out=self.XH[c], in_=xa[:]), reads=[bxa])

    def build_all(self):
        self.stage_prep()
        for l in range(2):
            wc = (l == 0)
            self.stage_inproj(l)
            self.stage_na(l, wc)
            self.stage_gqa(l, wc)
            self.stage_gla(l, 0, wc)
            self.stage_gla(l, 1, wc)
            self.stage_merge_a(l, wc)
            self.stage_merge_b(l, wc)
            if l == 0:
                for (t0, nb, w) in BLK5:
                    self.stage_ffn_block(0, t0, nb, w, False)
            else:
                self.stage_split()
                self.stage_router(1)
                for (t0, nb, w) in BLK5[:2]:
                    self.stage_ffn_block(1, t0, nb, w, True)
        self.stage_final()

    def finish(self):
        self.gst.__exit__(None, None, None)
        return self.nc


def make_consts():
    k = {}
    k["k_ident"] = np.eye(128, dtype=np.float32)
    j = np.arange(128)[:, None]
    i = np.arange(128)[None, :]
    tri = np.zeros((4, 128, 128), np.float32)
    tri[0] = (j <= i)
    tri[1] = (j >= i)
    tri[2] = (j > i)
    tri[3] = (j < i)
    k["k_tri"] = tri
    half = 64
    freqs = (10000.0 ** (-np.arange(0, half, 2, dtype=np.float32) / half)).astype(np.float32)
    t = np.arange(S_LAT)
    row = (t // 64).astype(np.float32)
    col = (t % 64).astype(np.float32)
    ang = np.concatenate([row[:, None] * freqs, col[:, None] * freqs], axis=-1).astype(np.float32)
    cos = np.ones((T, 64), np.float32)
    sin = np.zeros((T, 64), np.float32)
    cos[:S_LAT] = np.cos(ang)
    sin[:S_LAT] = np.sin(ang)
    cs = np.concatenate([np.tile(cos[:, None, :], (1, 4, 1)).reshape(T, 256), np.tile(sin[:, None, :], (1, 4, 1)).reshape(T, 256)], axis=1)
    k["k_cs"] = np.ascontiguousarray(cs, dtype=np.float32)
    mask = np.full((5, 128, 576), NEG, np.float32)
    qc = np.arange(64)
    cstart = np.clip(qc - 8, 0, 48)
    kc = np.arange(64)
    inwin = (kc[None, :] >= cstart[:, None]) & (kc[None, :] < cstart[:, None] + 16)
    for ty, i0 in enumerate([0, 1, 2, 14, 15]):
        rs0 = int(np.clip(2 * i0 - 4, 0, 24))
        rs1 = int(np.clip(2 * i0 + 1 - 4, 0, 24))
        base = rs0
        nr = rs1 + 8 - rs0
        for a in range(2):
            rs = rs0 if a == 0 else rs1
            for rho in range(nr):
                kr = base + rho
                if rs <= kr <= rs + 7:
                    blk = np.where(inwin, 0.0, NEG).astype(np.float32)
                    mask[ty, a * 64:(a + 1) * 64, rho * 64:(rho + 1) * 64] = blk
    k["k_namask"] = mask
    sel = np.zeros((8, 8, 128), np.float32)
    for e in range(8):
        sel[e, e, :] = 1.0
    k["k_sel"] = sel.reshape(8, 1024)
    return k


def make_inmap(inp, b, cfg, consts):
    m = dict(consts)
    f = lambda a: np.ascontiguousarray(a, dtype=np.float32)
    hsel = np.zeros((128, 2), np.float32)
    hsel[:, cfg.get("half", 0)] = 1.0
    m["k_half"] = hsel
    m["x"] = f(inp["x"][b])
    m["ctx"] = f(inp["ctx"][b])
    m["c"] = f(inp["c"][b:b + 1])
    m["c_ctx"] = f(inp["c_ctx"][None, :])
    m["w_ada"] = f(inp["w_ada"])
    m["b_ada"] = f(inp["b_ada"])
    m["norm1_g"] = f(inp["norm1_g"])
    m["norm2_g"] = f(inp["norm2_g"])
    m["final_norm_g"] = f(inp["final_norm_g"][None, :])
    m["gqa_qn_g"] = f(inp["gqa_qn_g"])
    m["gqa_kn_g"] = f(inp["gqa_kn_g"])
    m["gla_norm_g"] = f(inp["gla_norm_g"])
    for l in (cfg["layers"] if cfg.get("mixer", True) else []):
        m["w_in%d" % l] = f(inp["w_in"][l])
        m["na_rpb%d" % l] = f(inp["na_rpb"][l].reshape(16, 15 * 31))
        m["gla_w_a2_%d" % l] = f(inp["gla_w_a2"][l])
        m["gla_b_a%d" % l] = f(inp["gla_b_a"][l])
        m["w_pa%d" % l] = f(inp["w_pa"][l])
        m["w_pb%d" % l] = f(inp["w_pb"][l])
        m["w_pc%d" % l] = f(inp["w_pc"][l])
        m["w_out%d" % l] = f(inp["w_out"][l])
    if cfg.get("ffn", True):
        if 0 in cfg["layers"]:
            m["dense_w_gate"] = f(inp["dense_w_gate"][0])
            m["dense_w_up"] = f(inp["dense_w_up"][0])
            m["dense_w_down"] = f(inp["dense_w_down"][0])
        if 1 in cfg["layers"]:
            m["router_w"] = f(inp["router_w"][0])
            m["router_b"] = f(inp["router_b"])
            m["moe_w_gate"] = f(inp["moe_w_gate"][0])
            m["moe_w_up"] = f(inp["moe_w_up"][0])
            m["moe_w_down"] = f(inp["moe_w_down"][0])
    return m


FULL_CFG = {"layers": [0, 1], "ffn": True, "mixer": True, "debug": []}
N_CORES = 8


def kernel(**inputs):
    cfg = FULL_CFG
    B = Builder(cfg)
    B.declare()
    B.build_all()
    nc = B.finish()
    consts = make_consts()
    in_maps = []
    for c in range(N_CORES):
        b, half = c % 4, c // 4
        m = make_inmap(inputs, b, dict(cfg, half=half), consts)
        in_maps.append({k: v for k, v in m.items() if k in B.inputs})
    res = run_bass_kernel_spmd(nc, in_maps, core_ids=list(range(N_CORES)))
    out = np.empty((4, S_LAT, D), np.float32)
    for c in range(N_CORES):
        b, half = c % 4, c // 4
        out[b, half * HALF:(half + 1) * HALF] = np.asarray(res.results[c]["out"], dtype=np.float32)
    return out
```

```python
import numpy as np
import ml_dtypes
from contextlib import ExitStack
import concourse.bass as bass
import concourse.mybir as mybir
from concourse.bass_utils import run_bass_kernel_spmd

F32 = mybir.dt.float32
BF16 = mybir.dt.bfloat16
AF = mybir.ActivationFunctionType
ALU = mybir.AluOpType
AX = mybir.AxisListType

D = 2048
DC = 16
S_LAT = 2048
L_CTX = 256
T = S_LAT + L_CTX
TT = T // 128
D_IN = 13856
D_FF = 5632
FC = D_FF // 128
NE = 8
EPS = 1e-6
NEG = -30000.0
HALF = S_LAT // 2
BLK5 = [(0, 512, 0), (512, 512, 0), (1024, 512, 0), (1536, 512, 0), (2048, 256, 1)]


class Sem:
    def __init__(self, h):
        self.h = h
        self.n = 0


class Buf:
    __slots__ = ("name", "w", "r", "dsem")

    def __init__(self, name=""):
        self.name = name
        self.w = None
        self.r = []
        self.dsem = None


class Prog:
    ENG = ("pe", "act", "dve", "pool", "sp")
    ENGN = {"pe": "tensor", "act": "scalar", "dve": "vector", "pool": "gpsimd", "sp": "sync"}

    def __init__(self, nc, stack, n_dma_sems=90):
        self.nc = nc
        self.ops = {e: [] for e in self.ENG}
        self.esem = {e: Sem(stack.enter_context(nc.semaphore("es_" + e))) for e in self.ENG}
        self.seen = {e: {} for e in self.ENG}
        self.free_dsems = [Sem(stack.enter_context(nc.semaphore("ds%d" % i))) for i in range(n_dma_sems)]
        self.stage_bufs = []
        self.bar = Sem(stack.enter_context(nc.semaphore("bar")))
        self.n_ops = 0

    def buf(self, name=""):
        b = Buf(name)
        self.stage_bufs.append(b)
        return b

    def bufs(self, n, name=""):
        return [self.buf("%s%d" % (name, i)) for i in range(n)]

    def _dsem(self, b):
        if b.dsem is None:
            b.dsem = self.free_dsems.pop()
        return b.dsem

    def _waits_for(self, eng, reads, writes):
        need = {}

        def add(m):
            if m is None:
                return
            s, v = m
            if need.get(id(s), (s, 0))[1] < v:
                need[id(s)] = (s, v)
        for b in reads:
            add(b.w)
        for b in writes:
            add(b.w)
            for m in b.r:
                add(m)
        out = []
        seen = self.seen[eng]
        for s, v in need.values():
            if s is self.esem[eng] and eng == "pe":
                continue
            if seen.get(id(s), 0) >= v:
                continue
            seen[id(s)] = v
            out.append((s.h, v))
        return out

    def op(self, eng, emit, reads=(), writes=()):
        waits = self._waits_for(eng, reads, writes)
        s = self.esem[eng]
        s.n += 1
        mark = (s, s.n)
        self.ops[eng].append((waits, emit, [(s.h, 1)]))
        for b in reads:
            b.r.append(mark)
            if len(b.r) > 24:
                b.r = b.r[-24:] if False else b.r
        for b in writes:
            b.w = mark
            b.r = []
        self.n_ops += 1

    def dma(self, q, emit, reads=(), writes=(), n=1):
        waits = self._waits_for(q, reads, writes)
        prim = writes[0] if len(writes) else reads[0]
        s = self._dsem(prim)
        s.n += 16 * n
        mark = (s, s.n)
        self.ops[q].append((waits, emit, [(s.h, 16)]))
        for b in reads:
            b.r.append(mark)
        for b in writes:
            b.w = mark
            b.r = []
        self.n_ops += 1

    def raw_dma(self, q, emit, sem):
        sem.n += 16
        self.ops[q].append(([], emit, [(sem.h, 16)]))
        self.n_ops += 1

    def barrier(self):
        waits = []
        for e in self.ENG:
            if e != "sp" and self.esem[e].n > 0:
                waits.append((self.esem[e].h, self.esem[e].n))
        for b in self.stage_bufs:
            if b.dsem is not None:
                waits.append((b.dsem.h, b.dsem.n))
        self.bar.n += 1
        v = self.bar.n
        self.ops["sp"].append((waits, lambda e: e.nop(), [(self.bar.h, 1)]))
        for e in self.ENG:
            if e != "sp":
                self.ops[e].append(([(self.bar.h, v)], None, []))
        for b in self.stage_bufs:
            if b.dsem is not None:
                self.free_dsems.append(b.dsem)
                b.dsem = None
            b.w = None
            b.r = []
        self.stage_bufs = []
        for e in self.ENG:
            self.seen[e] = {}

    def flush(self):
        nc = self.nc
        with nc.Block() as block:
            for e in self.ENG:
                ops = self.ops[e]

                def body(eng, ops=ops):
                    for waits, emit, incs in ops:
                        for (sh, v) in waits:
                            eng.wait_ge(sh, v)
                        if emit is None:
                            continue
                        r = emit(eng)
                        rs = r if isinstance(r, (list, tuple)) else [r]
                        for ins in rs:
                            for (sh, amt) in incs:
                                ins.then_inc(sh, amt)
                getattr(block, self.ENGN[e])(body)
        self.ops = {e: [] for e in self.ENG}


class Ring:
    def __init__(self, tiles, bufs):
        self.t = tiles
        self.b = bufs
        self.i = 0

    def next(self):
        k = self.i % len(self.t)
        self.i += 1
        return self.t[k], self.b[k]


class Stage:
    def __init__(self, B, name):
        self.B = B
        self.name = name
        self.st = ExitStack()

    def __enter__(self):
        self.st.__enter__()
        self.B.issue_conv(self.name)
        return self

    def __exit__(self, *a):
        self.B.P.barrier()
        self.B.P.flush()
        return self.st.__exit__(*a)

    def sb(self, shape, dt, name=None):
        B = self.B
        B.uid += 1
        t = self.st.enter_context(B.nc.sbuf_tensor("%s_%s%d" % (self.name, name or "t", B.uid), list(shape), dt))
        return t, B.P.buf()

    def ps(self, shape, dt=F32, name=None):
        B = self.B
        B.uid += 1
        t = self.st.enter_context(B.nc.psum_tensor("%s_%s%d" % (self.name, name or "p", B.uid), list(shape), dt))
        return t, B.P.buf()

    def ring_sb(self, n, shape, dt, name=None):
        ts = [self.sb(shape, dt, name) for _ in range(n)]
        return Ring([t for t, _ in ts], [b for _, b in ts])

    def ring_ps(self, n, shape, dt=F32, name=None):
        ts = [self.ps(shape, dt, name) for _ in range(n)]
        return Ring([t for t, _ in ts], [b for _, b in ts])


class Builder:
    def __init__(self, cfg):
        self.cfg = cfg
        self.dbg = set(cfg.get("debug", ()))
        self.nc = bass.Bass("TRN2", target_bir_lowering=False)
        self.gst = ExitStack()
        self.gst.__enter__()
        self.P = Prog(self.nc, self.gst)
        self.uid = 0
        self.inputs = {}
        self.outputs = []

    def din(self, name, shape, dt=F32):
        ap = self.nc.dram_tensor(name, list(shape), dt, kind="ExternalInput").ap()
        self.inputs[name] = ap
        return ap

    def dscr(self, name, shape, dt):
        if name in self.dbg:
            self.outputs.append(name)
            return self.nc.dram_tensor(name, list(shape), dt, kind="ExternalOutput").ap()
        return self.nc.dram_tensor(name, list(shape), dt).ap()

    def gsb(self, name, shape, dt):
        return self.gst.enter_context(self.nc.sbuf_tensor(name, list(shape), dt))

    def stage(self, name):
        return Stage(self, name)

    def setup_conv(self):
        self.conv_sem = self.P.free_dsems.pop()
        self.conv_jobs = []
        if not hasattr(self, "mw_gate"):
            return
        for ex in range(NE):
            wgs = self.mw_gate[ex].rearrange("(k p) n -> p k n", p=128)
            wus = self.mw_up[ex].rearrange("(k p) n -> p k n", p=128)
            wds = self.mw_down[ex].rearrange("(f p) n -> p f n", p=128)
            for fg in range(FC // 2):
                self.conv_jobs.append(lambda e, ex=ex, fg=fg, wgs=wgs: e.dma_start(out=self.WGB[ex, fg].rearrange("p (k n) -> p k n", k=DC), in_=wgs[:, :, fg * 256:(fg + 1) * 256]))
                self.conv_jobs.append(lambda e, ex=ex, fg=fg, wus=wus: e.dma_start(out=self.WUB[ex, fg].rearrange("p (k n) -> p k n", k=DC), in_=wus[:, :, fg * 256:(fg + 1) * 256]))
            for mc in range(DC):
                self.conv_jobs.append(lambda e, ex=ex, mc=mc, wds=wds: e.dma_start(out=self.WDB[ex, mc].rearrange("p (f n) -> p f n", f=FC), in_=wds[:, :, mc * 128:(mc + 1) * 128]))
        self.conv_per_stage = 24

    def issue_conv(self, stage_name):
        if not getattr(self, "conv_jobs", None):
            return
        if stage_name == "prep":
            return
        n = len(self.conv_jobs) if (stage_name.startswith("ffn1") or stage_name == "router") else self.conv_per_stage
        for _ in range(min(n, len(self.conv_jobs))):
            job = self.conv_jobs.pop(0)
            self.P.raw_dma("pool", job, self.conv_sem)

    def wait_conv(self):
        for e in ("sp",):
            self.P.ops[e].append(([(self.conv_sem.h, self.conv_sem.n)], None, []))

    def declare(self):
        c = self.cfg
        L = c["layers"]
        self.x_in = self.din("x", [S_LAT, D])
        self.ctx_in = self.din("ctx", [L_CTX, D])
        self.c_in = self.din("c", [1, D])
        self.cctx_in = self.din("c_ctx", [1, D])
        self.ident_in = self.din("k_ident", [128, 128])
        self.tri_in = self.din("k_tri", [4, 128, 128])
        self.cs_in = self.din("k_cs", [T, 512])
        self.mask_in = self.din("k_namask", [5, 128, 576])
        self.sel_in = self.din("k_sel", [8, 8 * 128])
        self.half_in = self.din("k_half", [128, 2])
        self.w_ada = self.din("w_ada", [2, D, 6 * D])
        self.b_ada = self.din("b_ada", [2, 6 * D])
        self.norm1_g = self.din("norm1_g", [2, D])
        self.norm2_g = self.din("norm2_g", [2, D])
        self.final_g = self.din("final_norm_g", [1, D])
        self.gqa_qn = self.din("gqa_qn_g", [2, 128])
        self.gqa_kn = self.din("gqa_kn_g", [2, 128])
        self.gla_ng = self.din("gla_norm_g", [2, 256])
        self.w_in = {}; self.rpb = {}; self.w_a2 = {}; self.b_a = {}
        self.w_pa = {}; self.w_pb = {}; self.w_pc = {}; self.w_out = {}
        for l in (L if c.get("mixer", True) else []):
            self.w_in[l] = self.din("w_in%d" % l, [D, D_IN])
            self.rpb[l] = self.din("na_rpb%d" % l, [16, 15 * 31])
            self.w_a2[l] = self.din("gla_w_a2_%d" % l, [2, 16, 512])
            self.b_a[l] = self.din("gla_b_a%d" % l, [2, 512])
            self.w_pa[l] = self.din("w_pa%d" % l, [1024, D])
            self.w_pb[l] = self.din("w_pb%d" % l, [1024, D])
            self.w_pc[l] = self.din("w_pc%d" % l, [1024, D])
            self.w_out[l] = self.din("w_out%d" % l, [D, D])
        if 0 in L and c.get("ffn", True):
            self.dw_gate = self.din("dense_w_gate", [D, D_FF])
            self.dw_up = self.din("dense_w_up", [D, D_FF])
            self.dw_down = self.din("dense_w_down", [D_FF, D])
        if 1 in L and c.get("ffn", True):
            self.router_w = self.din("router_w", [D, NE])
            self.router_b = self.din("router_b", [1, NE])
            self.mw_gate = self.din("moe_w_gate", [NE, D, D_FF])
            self.mw_up = self.din("moe_w_up", [NE, D, D_FF])
            self.mw_down = self.din("moe_w_down", [NE, D_FF, D])
        self.XT = self.dscr("XT", [DC, 128, T], F32)
        self.FM = self.dscr("FM", [72, 128, T], BF16)
        self.LOW = self.dscr("LOW", [32, T], F32)
        self.TM = self.dscr("TM", [T, 5120], BF16)
        self.AT = self.dscr("AT", [8, 128, T], BF16)
        self.BT = self.dscr("BT", [8, 128, T], BF16)
        self.CT = self.dscr("CT", [8, 128, T], BF16)
        self.MT = self.dscr("MT", [DC, 128, T], BF16)
        self.OF = self.dscr("OF", [T, 1024], F32)
        self.FB = self.dscr("FB", [16 * 18 * 64 * 96], F32)
        self.MODT = self.dscr("MODT", [2, 128, 96 * 2], F32)
        self.GATE = self.dscr("GATE", [8, HALF], F32)
        self.XH = self.dscr("XH", [DC, 128, HALF], F32)
        if hasattr(self, "mw_gate"):
            self.WGB = self.dscr("WGB", [NE, FC // 2, 128, DC * 256], BF16)
            self.WUB = self.dscr("WUB", [NE, FC // 2, 128, DC * 256], BF16)
            self.WDB = self.dscr("WDB", [NE, DC, 128, FC * 128], BF16)
        self.setup_conv()
        self.OUT = self.nc.dram_tensor("out", [HALF, D], F32, kind="ExternalOutput").ap()
        self.outputs.append("out")
        self.ident_f = self.gsb("ident_f", [128, 128], F32)
        self.ident_b = self.gsb("ident_b", [128, 128], BF16)
        self.ones_f = self.gsb("ones_f", [128, 128], F32)
        self.cactT = self.gsb("cactT", [128, 32], F32)
        self.modT = [self.gsb("modT%d" % l, [128, 96, 2], F32) for l in range(2)]
        self.A1 = [self.gsb("A1_%d" % l, [128, 16, 2], F32) for l in range(2)]
        self.A2 = [self.gsb("A2_%d" % l, [128, 16, 2], F32) for l in range(2)]
        self.gT = self.gsb("gT", [128, 80], F32)
        self.gbc = self.gsb("gbc", [128, 1024], F32)

    def stage_prep(self):
        P = self.P
        nc = self.nc
        with self.stage("prep") as S:
            b_id = P.buf()
            P.dma("sp", lambda e: e.dma_start(out=self.ident_f[:], in_=self.ident_in), writes=[b_id])
            P.op("dve", lambda e: e.tensor_copy(self.ident_b[:], self.ident_f[:]), reads=[b_id], writes=[P.buf()])
            b_ones = P.buf()
            P.op("pool", lambda e: e.memset(self.ones_f[:], 1.0), writes=[b_ones])
            xs_r = S.ring_sb(2, [128, D], F32, "xs")
            xo_r = S.ring_sb(2, [128, DC, 128], F32, "xo")
            pt_r = S.ring_ps(2, [128, 4, 128], F32, "pt")
            k = 0
            for tt in range(TT):
                src = self.x_in[tt * 128:(tt + 1) * 128, :] if tt < 16 else self.ctx_in[(tt - 16) * 128:(tt - 15) * 128, :]
                xs, bxs = xs_r.next()
                xo, bxo = xo_r.next()
                P.dma("sp", lambda e, xs=xs, src=src: e.dma_start(out=xs[:], in_=src), writes=[bxs])
                for g in range(4):
                    pt, bpt = pt_r.next()
                    for j in range(4):
                        cc = 4 * g + j
                        P.op("pe", lambda e, pt=pt, xs=xs, j=j, cc=cc: e.transpose(pt[:, j, :], xs[:, cc * 128:(cc + 1) * 128], self.ident_f[:]),
                             reads=[bxs, b_id], writes=[bpt])
                    if k % 2 == 0:
                        P.op("dve", lambda e, pt=pt, xo=xo, g=g: e.tensor_copy(xo[:, 4 * g:4 * g + 4, :], pt[:]), reads=[bpt], writes=[bxo])
                    else:
                        P.op("act", lambda e, pt=pt, xo=xo, g=g: e.copy(xo[:, 4 * g:4 * g + 4, :], pt[:]), reads=[bpt], writes=[bxo])
                    k += 1
                dst = self.XT[:, :, tt * 128:(tt + 1) * 128].rearrange("c p t -> p c t")
                P.dma("sp", lambda e, xo=xo, dst=dst: e.dma_start(out=dst, in_=xo[:]), reads=[bxo])
            cc_t, bcc = S.sb([32, 128], F32, "cc")
            P.dma("sp", lambda e: e.dma_start(out=cc_t[0:16, :], in_=self.c_in.rearrange("o (c p) -> (o c) p", p=128)), writes=[bcc])
            P.dma("sp", lambda e: e.dma_start(out=cc_t[16:32, :], in_=self.cctx_in.rearrange("o (c p) -> (o c) p", p=128)), writes=[bcc])
            cs_t, bcs = S.sb([32, 128], F32, "cs")
            P.op("act", lambda e: e.activation(cs_t[:], cc_t[:], AF.Silu), reads=[bcc], writes=[bcs])
            pm, bpm = S.ps([128, 128], F32, "pm")
            P.op("pe", lambda e: e.transpose(pm[:, 0:32], cs_t[:], self.ident_f[0:32, 0:32]), reads=[bcs, b_id], writes=[bpm])
            b_cact = P.buf()
            P.op("dve", lambda e: e.tensor_copy(self.cactT[:], pm[:, 0:32]), reads=[bpm], writes=[b_cact])
            gr, bgr = S.sb([80, 128], F32, "gr")
            srcs = [self.norm1_g[0:1, :], self.norm2_g[0:1, :], self.norm1_g[1:2, :], self.norm2_g[1:2, :], self.final_g[0:1, :]]
            for v, sap in enumerate(srcs):
                P.dma("sp", lambda e, v=v, sap=sap: e.dma_start(out=gr[v * 16:(v + 1) * 16, :], in_=sap.rearrange("o (c p) -> (o c) p", p=128)), writes=[bgr])
            P.op("pe", lambda e: e.transpose(pm[:, 0:80], gr[:], self.ident_f[0:80, 0:80]), reads=[bgr, b_id], writes=[bpm])
            b_gT = P.buf()
            P.op("dve", lambda e: e.tensor_copy(self.gT[:], pm[:, 0:80]), reads=[bpm], writes=[b_gT])
            grow, bgrow = S.sb([1, 1024], F32, "grow")
            rs = [(self.gqa_qn[0:1, :], 0, 128), (self.gqa_kn[0:1, :], 128, 128), (self.gqa_qn[1:2, :], 256, 128),
                  (self.gqa_kn[1:2, :], 384, 128), (self.gla_ng[0:1, :], 512, 256), (self.gla_ng[1:2, :], 768, 256)]
            for sap, o, n in rs:
                P.dma("sp", lambda e, sap=sap, o=o, n=n: e.dma_start(out=grow[0:1, o:o + n], in_=sap), writes=[bgrow])
            pb, bpb = S.ps([128, 512], F32, "pb")
            b_gbc = P.buf()
            for hh in range(2):
                P.op("pe", lambda e, hh=hh: e.matmul(pb[:], self.ones_f[0:1, :], grow[0:1, hh * 512:(hh + 1) * 512], start=True, stop=True),
                     reads=[bgrow, b_ones], writes=[bpb])
                P.op("dve", lambda e, hh=hh: e.tensor_copy(self.gbc[:, hh * 512:(hh + 1) * 512], pb[:]), reads=[bpb], writes=[b_gbc])
            wt_r = S.ring_sb(2, [128, DC, 512], F32, "wada")
            pmod_r = S.ring_ps(2, [128, 4, 2], F32, "pmod")
            bad, bbad = S.sb([96, 128], F32, "bad")
            badT, bbadT = S.sb([128, 96], F32, "badT")
            for l in range(2):
                P.dma("sp", lambda e, l=l: e.dma_start(out=bad[:], in_=self.b_ada[l:l + 1, :].rearrange("o (c p) -> (o c) p", p=128)), writes=[bbad])
                P.op("pe", lambda e: e.transpose(pm[:, 0:96], bad[:], self.ident_f[0:96, 0:96]), reads=[bbad, b_id], writes=[bpm])
                P.op("dve", lambda e: e.tensor_copy(badT[:], pm[:, 0:96]), reads=[bpm], writes=[bbadT])
                b_mod = P.buf()
                wv = self.w_ada[l].rearrange("(k p) n -> p k n", p=128)
                for g in range(24):
                    wt, bwt = wt_r.next()
                    P.dma("sp", lambda e, wt=wt, g=g, wv=wv: e.dma_start(out=wt[:], in_=wv[:, :, g * 512:(g + 1) * 512]), writes=[bwt])
                    pmod, bpmod = pmod_r.next()
                    for j in range(4):
                        for kk in range(DC):
                            P.op("pe", lambda e, pmod=pmod, wt=wt, j=j, kk=kk: e.matmul(pmod[:, j, :], wt[:, kk, j * 128:(j + 1) * 128], self.cactT[:, kk:32:16],
                                                                                       start=(kk == 0), stop=(kk == DC - 1)),
                                 reads=[bwt, b_cact], writes=[bpmod])
                    for w in range(2):
                        P.op("dve", lambda e, pmod=pmod, g=g, w=w, l=l: e.tensor_tensor(out=self.modT[l][:, 4 * g:4 * g + 4, w], in0=pmod[:, :, w],
                                                                                          in1=badT[:, 4 * g:4 * g + 4], op=ALU.add),
                             reads=[bpmod, bbadT], writes=[b_mod])
                for w in range(2):
                    P.op("dve", lambda e, l=l, w=w: e.scalar_tensor_tensor(out=self.A1[l][:, :, w], in0=self.modT[l][:, 16:32, w], scalar=1.0,
                                                                           in1=self.gT[:, (2 * l) * 16:(2 * l + 1) * 16], op0=ALU.add, op1=ALU.mult),
                         reads=[b_mod, b_gT], writes=[P.buf()])
                    P.op("dve", lambda e, l=l, w=w: e.scalar_tensor_tensor(out=self.A2[l][:, :, w], in0=self.modT[l][:, 64:80, w], scalar=1.0,
                                                                           in1=self.gT[:, (2 * l + 1) * 16:(2 * l + 2) * 16], op0=ALU.add, op1=ALU.mult),
                         reads=[b_mod, b_gT], writes=[P.buf()])
                if "MODT" in self.dbg:
                    P.dma("sp", lambda e, l=l: e.dma_start(out=self.MODT[l], in_=self.modT[l][:].rearrange("p a b -> p (a b)")), reads=[b_mod])

    def norm_blocks(self, S, hT, b_hT, Acol, shcol, tok0=0, ntok=T, nring=2, gain_only=None, cb=None, want32=False, src=None):
        P = self.P
        XS = self.XT if src is None else src
        xb_r = S.ring_sb(nring, [128, DC, 256], F32, "xb")
        sq_r = S.ring_sb(2, [128, 256], F32, "sq")
        tmp_r = S.ring_sb(2, [128, 256], F32, "tmp")
        rstd_r = S.ring_sb(2, [128, 256], F32, "rstd")
        ss_r = S.ring_ps(1, [128, 256], F32, "ss")
        h32_r = S.ring_sb(2, [128, DC, 256], F32, "h32") if cb is not None else None
        for bi in range(ntok // 256):
            t0 = tok0 + bi * 256
            o0 = bi * 256
            w = 0 if t0 < S_LAT else 1
            xb, bxb = xb_r.next()
            P.dma("sp", lambda e, xb=xb, t0=t0: e.dma_start(out=xb[:], in_=XS[:, :, t0:t0 + 256].rearrange("c p t -> p c t")), writes=[bxb])
            ss, bss = ss_r.next()
            for c in range(DC):
                sq, bsq = sq_r.next()
                P.op("act", lambda e, sq=sq, xb=xb, c=c: e.activation(sq[:], xb[:, c, :], AF.Square), reads=[bxb], writes=[bsq])
                P.op("pe", lambda e, ss=ss, sq=sq, c=c: e.matmul(ss[:], self.ones_f[:], sq[:], start=(c == 0), stop=(c == DC - 1)), reads=[bsq], writes=[bss])
            rstd, brs = rstd_r.next()
            P.op("dve", lambda e, rstd=rstd, ss=ss: e.tensor_scalar(rstd[:], ss[:], 1.0 / D, EPS, ALU.mult, ALU.add), reads=[bss], writes=[brs])
            P.op("act", lambda e, rstd=rstd: e.sqrt(rstd[:], rstd[:]), reads=[brs], writes=[brs])
            P.op("dve", lambda e, rstd=rstd: e.reciprocal(rstd[:], rstd[:]), reads=[brs], writes=[brs])
            if cb is not None:
                h32, b_h32 = h32_r.next()
            for c in range(DC):
                tmp, btmp = tmp_r.next()
                P.op("pool", lambda e, tmp=tmp, xb=xb, c=c, rstd=rstd: e.tensor_tensor(out=tmp[:], in0=xb[:, c, :], in1=rstd[:], op=ALU.mult),
                     reads=[bxb, brs], writes=[btmp])
                if gain_only is not None:
                    P.op("dve", lambda e, tmp=tmp, c=c, h32=h32: e.tensor_scalar_mul(h32[:, c, :], tmp[:], gain_only[:, c:c + 1]), reads=[btmp], writes=[b_h32])
                    continue
                if hT is not None:
                    P.op("dve", lambda e, tmp=tmp, c=c, w=w, o0=o0: e.tensor_scalar(hT[:, c, o0:o0 + 256], tmp[:], Acol[:, c, w:w + 1], shcol[:, c, w:w + 1], ALU.mult, ALU.add),
                         reads=[btmp], writes=[b_hT])
                if cb is not None:
                    P.op("act", lambda e, tmp=tmp, c=c, w=w, h32=h32: e.activation(h32[:, c, :], tmp[:], AF.Identity, bias=shcol[:, c, w:w + 1], scale=Acol[:, c, w:w + 1]),
                         reads=[btmp], writes=[b_h32])
            if cb is not None:
                cb(bi, t0, h32, b_h32)

    def stage_inproj(self, l):
        P = self.P
        with self.stage("ip%d" % l) as S:
            hT, b_hT = S.sb([128, DC, T], BF16, "hT")
            self.norm_blocks(S, hT, b_hT, self.A1[l], self.modT[l][:, 0:16, :])
            wv = self.w_in[l].rearrange("(k p) n -> p k n", p=128)
            wt_r = S.ring_sb(2, [128, DC, 512], BF16, "wt")
            ps_r = S.ring_ps(4, [128, 512], F32, "ps")
            st_r = S.ring_sb(2, [128, T], BF16, "stf")
            fm_groups = [(0, 4, 0, False), (3072, 2, 16, False), (7712, 12, 24, True)]
            k = 0
            for (c0, ng, f0, sig) in fm_groups:
                for g in range(ng):
                    wt, bwt = wt_r.next()
                    P.dma("pool", lambda e, wt=wt, c0=c0, g=g: e.dma_start(out=wt[:], in_=wv[:, :, c0 + g * 512:c0 + (g + 1) * 512]), writes=[bwt])
                    for j in range(4):
                        st, bst = st_r.next()
                        for (t0, nb, w) in BLK5:
                            ps, bps = ps_r.next()
                            for kk in range(DC):
                                P.op("pe", lambda e, ps=ps, wt=wt, j=j, kk=kk, t0=t0, nb=nb: e.matmul(ps[:, 0:nb], wt[:, kk, j * 128:(j + 1) * 128], hT[:, kk, t0:t0 + nb],
                                                                                                    start=(kk == 0), stop=(kk == DC - 1)),
                                     reads=[bwt, b_hT], writes=[bps])
                            if sig:
                                P.op("act", lambda e, ps=ps, st=st, t0=t0, nb=nb: e.activation(st[:, t0:t0 + nb], ps[:, 0:nb], AF.Sigmoid), reads=[bps], writes=[bst])
                            elif k % 2 == 0:
                                P.op("dve", lambda e, ps=ps, st=st, t0=t0, nb=nb: e.tensor_copy(st[:, t0:t0 + nb], ps[:, 0:nb]), reads=[bps], writes=[bst])
                            else:
                                P.op("act", lambda e, ps=ps, st=st, t0=t0, nb=nb: e.copy(st[:, t0:t0 + nb], ps[:, 0:nb]), reads=[bps], writes=[bst])
                            k += 1
                        fi = f0 + g * 4 + j
                        P.dma("sp", lambda e, st=st, fi=fi: e.dma_start(out=self.FM[fi], in_=st[:]), reads=[bst])
            wl, bwl = S.sb([128, DC, 32], BF16, "wl")
            P.dma("pool", lambda e: e.dma_start(out=wl[:], in_=wv[:, :, 6144:6176]), writes=[bwl])
            lo, blo = S.sb([32, T], F32, "lo")
            for (t0, nb, w) in BLK5:
                ps, bps = ps_r.next()
                for kk in range(DC):
                    P.op("pe", lambda e, ps=ps, kk=kk, t0=t0, nb=nb: e.matmul(ps[0:32, 0:nb], wl[:, kk, :], hT[:, kk, t0:t0 + nb], start=(kk == 0), stop=(kk == DC - 1)),
                         reads=[bwl, b_hT], writes=[bps])
                P.op("dve", lambda e, ps=ps, t0=t0, nb=nb: e.tensor_copy(lo[:, t0:t0 + nb], ps[0:32, 0:nb]), reads=[bps], writes=[blo])
            P.dma("sp", lambda e: e.dma_start(out=self.LOW, in_=lo[:]), reads=[blo])
            tm_groups = [(2048, 0, "c"), (2560, 512, "c"), (3584, 1024, "c"), (4096, 1536, "c"), (4608, 2048, "c"),
                         (5120, 2560, "c"), (5632, 3072, "c"), (6176, 3584, "q"), (6688, 4096, "q"), (7200, 4608, "kv")]
            stt_r = S.ring_sb(3, [128, 512], BF16, "stt")
            cs_r = S.ring_sb(2, [128, 512], F32, "cs")
            sq_r = S.ring_sb(2, [128, 512], F32, "sq2")
            qn_r = S.ring_sb(2, [128, 512], F32, "qn")
            t1_r = S.ring_sb(2, [128, 256], F32, "t1")
            t2_r = S.ring_sb(2, [128, 256], F32, "t2")
            ss_r = S.ring_sb(2, [128, 4], F32, "ssq")
            qoff = 256 * l
            for (c0, off, kind) in tm_groups:
                wt, bwt = wt_r.next()
                P.dma("pool", lambda e, wt=wt, c0=c0: e.dma_start(out=wt[:], in_=wv[:, :, c0:c0 + 512]), writes=[bwt])
                for tt in range(TT):
                    ps, bps = ps_r.next()
                    for kk in range(DC):
                        P.op("pe", lambda e, ps=ps, wt=wt, kk=kk, tt=tt: e.matmul(ps[:], hT[:, kk, tt * 128:(tt + 1) * 128], wt[:, kk, :], start=(kk == 0), stop=(kk == DC - 1)),
                             reads=[bwt, b_hT], writes=[bps])
                    st, bst = stt_r.next()
                    if kind == "c":
                        if k % 2 == 0:
                            P.op("dve", lambda e, ps=ps, st=st: e.tensor_copy(st[:], ps[:]), reads=[bps], writes=[bst])
                        else:
                            P.op("act", lambda e, ps=ps, st=st: e.copy(st[:], ps[:]), reads=[bps], writes=[bst])
                        k += 1
                    else:
                        nh = 4 if kind == "q" else 2
                        gb = self.gbc[:, qoff:qoff + 128] if kind == "q" else self.gbc[:, qoff + 128:qoff + 256]
                        nw = nh * 128
                        cs, bcs = cs_r.next()
                        P.dma("sp", lambda e, cs=cs, tt=tt: e.dma_start(out=cs[:], in_=self.cs_in[tt * 128:(tt + 1) * 128, :]), writes=[bcs])
                        sq, bsq = sq_r.next()
                        P.op("act", lambda e, sq=sq, ps=ps, nw=nw: e.activation(sq[:, 0:nw], ps[:, 0:nw], AF.Square), reads=[bps], writes=[bsq])
                        ssq, bssq = ss_r.next()
                        P.op("dve", lambda e, ssq=ssq, sq=sq, nh=nh, nw=nw: e.tensor_reduce(out=ssq[:, 0:nh], in_=sq[:, 0:nw].rearrange("p (h d) -> p h d", h=nh), axis=AX.X, op=ALU.add),
                             reads=[bsq], writes=[bssq])
                        P.op("dve", lambda e, ssq=ssq, nh=nh: e.tensor_scalar(ssq[:, 0:nh], ssq[:, 0:nh], 1.0 / 128, EPS, ALU.mult, ALU.add), reads=[bssq], writes=[bssq])
                        P.op("act", lambda e, ssq=ssq, nh=nh: e.sqrt(ssq[:, 0:nh], ssq[:, 0:nh]), reads=[bssq], writes=[bssq])
                        P.op("dve", lambda e, ssq=ssq, nh=nh: e.reciprocal(ssq[:, 0:nh], ssq[:, 0:nh]), reads=[bssq], writes=[bssq])
                        qn, bqn = qn_r.next()
                        for h in range(nh):
                            P.op("dve", lambda e, qn=qn, ps=ps, ssq=ssq, h=h, gb=gb: e.scalar_tensor_tensor(out=qn[:, h * 128:(h + 1) * 128], in0=ps[:, h * 128:(h + 1) * 128],
                                                                                                           scalar=ssq[:, h:h + 1], in1=gb, op0=ALU.mult, op1=ALU.mult),
                                 reads=[bps, bssq], writes=[bqn])
                        qv = qn[:, 0:nw].rearrange("p (h i two) -> p h i two", h=nh, two=2)
                        ev, od = qv[:, :, :, 0], qv[:, :, :, 1]
                        cosv = cs[:, 0:nh * 64].rearrange("p (h i) -> p h i", h=nh)
                        sinv = cs[:, 256:256 + nh * 64].rearrange("p (h i) -> p h i", h=nh)
                        sv = st[:, 0:nw].rearrange("p (h i two) -> p h i two", h=nh, two=2)
                        t1, bt1 = t1_r.next()
                        t2, bt2 = t2_r.next()
                        t1v = t1[:, 0:nh * 64].rearrange("p (h i) -> p h i", h=nh)
                        t2v = t2[:, 0:nh * 64].rearrange("p (h i) -> p h i", h=nh)
                        P.op("pool", lambda e, t1v=t1v, ev=ev, cosv=cosv: e.tensor_tensor(out=t1v, in0=ev, in1=cosv, op=ALU.mult), reads=[bqn, bcs], writes=[bt1])
                        P.op("pool", lambda e, t2v=t2v, od=od, sinv=sinv: e.tensor_tensor(out=t2v, in0=od, in1=sinv, op=ALU.mult), reads=[bqn, bcs], writes=[bt2])
                        P.op("dve", lambda e, sv=sv, t1v=t1v, t2v=t2v: e.tensor_tensor(out=sv[:, :, :, 0], in0=t1v, in1=t2v, op=ALU.subtract), reads=[bt1, bt2], writes=[bst])
                        t1, bt1 = t1_r.next()
                        t2, bt2 = t2_r.next()
                        t1v = t1[:, 0:nh * 64].rearrange("p (h i) -> p h i", h=nh)
                        t2v = t2[:, 0:nh * 64].rearrange("p (h i) -> p h i", h=nh)
                        P.op("pool", lambda e, t1v=t1v, ev=ev, sinv=sinv: e.tensor_tensor(out=t1v, in0=ev, in1=sinv, op=ALU.mult), reads=[bqn, bcs], writes=[bt1])
                        P.op("pool", lambda e, t2v=t2v, od=od, cosv=cosv: e.tensor_tensor(out=t2v, in0=od, in1=cosv, op=ALU.mult), reads=[bqn, bcs], writes=[bt2])
                        P.op("dve", lambda e, sv=sv, t1v=t1v, t2v=t2v: e.tensor_tensor(out=sv[:, :, :, 1], in0=t1v, in1=t2v, op=ALU.add), reads=[bt1, bt2], writes=[bst])
                        if kind == "kv":
                            P.op("act", lambda e, ps=ps, st=st: e.copy(st[:, 256:512], ps[:, 256:512]), reads=[bps], writes=[bst])
                    P.dma("sp", lambda e, st=st, tt=tt, off=off: e.dma_start(out=self.TM[tt * 128:(tt + 1) * 128, off:off + 512], in_=st[:]), reads=[bst])

    def attn_res(self, S, nkmax, nch, dh):
        R = {}
        R["ps_s"] = S.ring_ps(3, [128, 512], F32, "pss")
        R["sc"] = S.ring_sb(2, [128, nkmax], F32, "sc")
        R["p"] = S.ring_sb(2, [128, nkmax], BF16, "p")
        R["ps_t"] = S.ring_ps(3, [128, 4, 128], BF16, "pst")
        R["pT"] = S.ring_sb(2, [128, nch, 128], BF16, "pT")
        R["ps_o"] = S.ring_ps(2, [128, dh], F32, "pso")
        R["sm"] = S.ring_sb(4, [128, 4], F32, "sm")
        R["st"] = S.ring_sb(2, [128, 8, 128], BF16, "fmst")
        R["k"] = 0
        return R

    def attend(self, R, q_ap, segs, vch, scale, out_ap, b_out, deps, dh):
        c = dict(vch=vch, out_ap=out_ap, b_out=b_out, deps=deps, dh=dh)
        prev = R.get("pending")
        R["pending"] = c
        self._att_scores(R, c, q_ap, segs, scale, deps)
        self._att_max(R, c)
        if prev is not None:
            self._att_transposes(R, prev)
        self._att_exp(R, c)
        if prev is not None:
            self._att_pv(R, prev)
        self._att_sum(R, c)
        if prev is not None:
            self._att_evac(R, prev)

    def attend_flush(self, R):
        prev = R.get("pending")
        R["pending"] = None
        if prev is not None:
            self._att_transposes(R, prev)
            self._att_pv(R, prev)
            self._att_evac(R, prev)

    def _att_scores(self, R, c, q_ap, segs, scale, deps):
        P = self.P
        sc, bsc = R["sc"].next()
        off = 0
        for (k_ap, n, bias_ap) in segs:
            ps, bps = R["ps_s"].next()
            P.op("pe", lambda e, ps=ps, k_ap=k_ap, n=n: e.matmul(ps[:, 0:n], q_ap, k_ap, start=True, stop=True), reads=deps, writes=[bps])
            if bias_ap is not None:
                P.op("dve", lambda e, ps=ps, n=n, off=off, bias_ap=bias_ap: e.scalar_tensor_tensor(out=sc[:, off:off + n], in0=ps[:, 0:n], scalar=scale, in1=bias_ap,
                                                                                                  op0=ALU.mult, op1=ALU.add), reads=[bps] + deps, writes=[bsc])
            else:
                P.op("act", lambda e, ps=ps, n=n, off=off: e.mul(sc[:, off:off + n], ps[:, 0:n], scale), reads=[bps], writes=[bsc])
            off += n
        c.update(sc=sc, bsc=bsc, NK=off)

    def _att_max(self, R, c):
        P = self.P
        sc, bsc, NK = c["sc"], c["bsc"], c["NK"]
        sm, bsm = R["sm"].next()
        P.op("dve", lambda e: e.reduce_max(out=sm[:, 0:1], in_=sc[:, 0:NK], axis=AX.X), reads=[bsc], writes=[bsm])
        P.op("dve", lambda e: e.tensor_scalar_mul(sm[:, 1:2], sm[:, 0:1], -1.0), reads=[bsm], writes=[bsm])
        c.update(sm=sm, bsm=bsm)

    def _att_exp(self, R, c):
        P = self.P
        sc, bsc, NK, sm, bsm = c["sc"], c["bsc"], c["NK"], c["sm"], c["bsm"]
        p, bp = R["p"].next()
        P.op("act", lambda e: e.activation(p[:, 0:NK], sc[:, 0:NK], AF.Exp, bias=sm[:, 1:2], scale=1.0), reads=[bsc, bsm], writes=[bp])
        c.update(p=p, bp=bp)

    def _att_sum(self, R, c):
        P = self.P
        p, bp, NK, sm, bsm = c["p"], c["bp"], c["NK"], c["sm"], c["bsm"]
        P.op("dve", lambda e: e.reduce_sum(out=sm[:, 2:3], in_=p[:, 0:NK], axis=AX.X), reads=[bp], writes=[bsm])
        P.op("dve", lambda e: e.reciprocal(sm[:, 3:4], sm[:, 2:3]), reads=[bsm], writes=[bsm])

    def _att_transposes(self, R, c):
        P = self.P
        p, bp, vch = c["p"], c["bp"], c["vch"]
        pT, bpT = R["pT"].next()
        c.update(pT=pT, bpT=bpT)
        nch = len(vch)
        for g0 in range(0, nch, 4):
            pst, bpst = R["ps_t"].next()
            grp = vch[g0:g0 + 4]
            for j, (v_ap, sz, koff) in enumerate(grp):
                P.op("pe", lambda e, pst=pst, j=j, sz=sz, koff=koff: e.transpose(pst[0:sz, j, :], p[:, koff:koff + sz], self.ident_b[:]), reads=[bp], writes=[bpst])
            ng = len(grp)
            full = all(sz == 128 for (_, sz, _) in grp)
            R["k"] += 1
            if full:
                if R["k"] % 2 == 0:
                    P.op("dve", lambda e, pst=pst, g0=g0, ng=ng: e.tensor_copy(pT[:, g0:g0 + ng, :], pst[:, 0:ng, :]), reads=[bpst], writes=[bpT])
                else:
                    P.op("act", lambda e, pst=pst, g0=g0, ng=ng: e.copy(pT[:, g0:g0 + ng, :], pst[:, 0:ng, :]), reads=[bpst], writes=[bpT])
            else:
                for j, (v_ap, sz, koff) in enumerate(grp):
                    P.op("dve", lambda e, pst=pst, g0=g0, j=j, sz=sz: e.tensor_copy(pT[0:sz, g0 + j, :], pst[0:sz, j, :]), reads=[bpst], writes=[bpT])

    def _att_pv(self, R, c):
        P = self.P
        vch, deps, dh, pT, bpT = c["vch"], c["deps"], c["dh"], c["pT"], c["bpT"]
        nch = len(vch)
        pso, bpso = R["ps_o"].next()
        for ci, (v_ap, sz, koff) in enumerate(vch):
            P.op("pe", lambda e, ci=ci, v_ap=v_ap, sz=sz: e.matmul(pso[:, 0:dh], pT[0:sz, ci, :], v_ap, start=(ci == 0), stop=(ci == nch - 1)),
                 reads=[bpT] + deps, writes=[bpso])
        c.update(pso=pso, bpso=bpso)

    def _att_evac(self, R, c):
        P = self.P
        pso, bpso, sm, bsm, out_ap, b_out, dh = c["pso"], c["bpso"], c["sm"], c["bsm"], c["out_ap"], c["b_out"], c["dh"]
        P.op("dve", lambda e: e.tensor_scalar_mul(out_ap, pso[:, 0:dh], sm[:, 3:4]), reads=[bpso, bsm], writes=[b_out])

    def tm_to_fm(self, R, src_ap, b_src, dst, i):
        P = self.P
        st, bst = R["st"].next()
        for g in range(2):
            pst, bpst = R["ps_t"].next()
            for j in range(4):
                cc = 4 * g + j
                P.op("pe", lambda e, pst=pst, j=j, cc=cc: e.transpose(pst[:, j, :], src_ap[:, cc * 128:(cc + 1) * 128], self.ident_b[:]), reads=[b_src], writes=[bpst])
            if g == 0:
                P.op("dve", lambda e, pst=pst, g=g: e.tensor_copy(st[:, 4 * g:4 * g + 4, :], pst[:]), reads=[bpst], writes=[bst])
            else:
                P.op("act", lambda e, pst=pst, g=g: e.copy(st[:, 4 * g:4 * g + 4, :], pst[:]), reads=[bpst], writes=[bst])
        P.dma("sp", lambda e: e.dma_start(out=dst[:, :, i * 128:(i + 1) * 128].rearrange("c p t -> p c t"), in_=st[:]), reads=[bst])

    def stage_na(self, l, with_ctx):
        P = self.P
        with self.stage("na%d" % l) as S:
            z, bz = S.sb([128, 13824], F32, "z")
            P.op("pool", lambda e: e.memset(z[:], 0.0), writes=[bz])
            b_F = P.buf()
            P.dma("sp", lambda e: e.dma_start(out=self.FB.rearrange("(p n) -> p n", p=128), in_=z[:]), reads=[bz], writes=[b_F])
            for h in range(16):
                src = bass.AP(tensor=self.rpb[l].tensor, offset=h * 465, ap=[[31, 15], [0, 64], [1, 31]])
                dst = bass.AP(tensor=self.FB.tensor, offset=h * 18 * 6144 + 6144, ap=[[6144, 15], [96, 64], [1, 31]])
                P.dma("sp", lambda e, src=src, dst=dst: e.dma_start(out=dst, in_=src), writes=[b_F])
            mk, bmk = S.sb([128, 5, 576], F32, "mk")
            P.dma("sp", lambda e: e.dma_start(out=mk[:], in_=self.mask_in.rearrange("t p n -> p t n")), writes=[bmk])
            R = self.attn_res(S, 832, 7, 64)
            q_r = S.ring_sb(2, [128, T], BF16, "q")
            k_r = S.ring_sb(2, [128, T], BF16, "k")
            v_r = S.ring_sb(2, [128, TT, 128], BF16, "v")
            bias_r = S.ring_sb(2, [128, 5, 576], F32, "bias")
            a_all, b_a = S.sb([128, TT, 1024], BF16, "a_all")
            types = [(7, 8), (5, 8), (3, 9), (3, 8), (1, 8)]
            tiles = list(range(16)) + ([16, 17] if with_ctx else [])
            for hp in range(8):
                qT, bq = q_r.next()
                kT, bk = k_r.next()
                v, bv = v_r.next()
                P.dma("sp", lambda e, qT=qT, hp=hp: e.dma_start(out=qT[:], in_=self.FM[hp]), writes=[bq])
                P.dma("sp", lambda e, kT=kT, hp=hp: e.dma_start(out=kT[:], in_=self.FM[8 + hp]), writes=[bk])
                P.dma("sp", lambda e, v=v, hp=hp: e.dma_start(out=v[:], in_=self.TM[:, hp * 128:(hp + 1) * 128].rearrange("(t p) c -> p t c", p=128)), writes=[bv])
                for sub in range(2):
                    h = 2 * hp + sub
                    bt, bbt = bias_r.next()
                    for ty, (joff, nr) in enumerate(types):
                        for a in range(2):
                            src = bass.AP(tensor=self.FB.tensor, offset=h * 18 * 6144 + (joff - a + 1) * 6144 + 15, ap=[[95, 64], [6144, nr], [1, 64]])
                            dst = bt[a * 64:(a + 1) * 64, ty, 0:nr * 64].rearrange("p (r k) -> p r k", k=64)
                            P.dma("sp", lambda e, src=src, dst=dst: e.dma_start(out=dst, in_=src), reads=[b_F], writes=[bbt])
                    for ty, (joff, nr) in enumerate(types):
                        P.op("pool", lambda e, bt=bt, ty=ty, nr=nr: e.tensor_tensor(out=bt[:, ty, 0:nr * 64], in0=bt[:, ty, 0:nr * 64], in1=mk[:, ty, 0:nr * 64], op=ALU.add),
                             reads=[bbt, bmk], writes=[bbt])
                    ps0 = sub * 64
                    deps = [bq, bk, bv, bbt]
                    for i in tiles:
                        q_ap = qT[ps0:ps0 + 64, i * 128:(i + 1) * 128]
                        segs = []
                        vch = []
                        if i < 16:
                            if i == 0:
                                ty, base, nr = 0, 0, 8
                            elif i == 1:
                                ty, base, nr = 1, 0, 8
                            elif i == 14:
                                ty, base, nr = 3, 24, 8
                            elif i == 15:
                                ty, base, nr = 4, 24, 8
                            else:
                                ty, base, nr = 2, 2 * i - 4, 9
                            t0 = base * 64
                            segs.append((kT[ps0:ps0 + 64, t0:t0 + 512], 512, bt[:, ty, 0:512]))
                            if nr == 9:
                                segs.append((kT[ps0:ps0 + 64, t0 + 512:t0 + 576], 64, bt[:, ty, 512:576]))
                            for m in range(4):
                                vch.append((v[:, base // 2 + m, ps0:ps0 + 64], 128, m * 128))
                            if nr == 9:
                                vch.append((v[0:64, base // 2 + 4, ps0:ps0 + 64], 64, 512))
                        koff = nr * 64 if i < 16 else 0
                        segs.append((kT[ps0:ps0 + 64, S_LAT:T], 256, None))
                        vch.append((v[:, 16, ps0:ps0 + 64], 128, koff))
                        vch.append((v[:, 17, ps0:ps0 + 64], 128, koff + 128))
                        self.attend(R, q_ap, segs, vch, 0.125, a_all[:, i, h * 64:(h + 1) * 64], b_a, deps, 64)
            self.attend_flush(R)
            for i in tiles:
                self.tm_to_fm(R, a_all[:, i, :], b_a, self.AT, i)

    def stage_gqa(self, l, with_ctx):
        P = self.P
        with self.stage("gqa%d" % l) as S:
            R = self.attn_res(S, T, TT, 128)
            qT, bq = S.sb([128, 8, T], BF16, "qT")
            kT, bk = S.sb([128, 2, T], BF16, "kT")
            v, bv = S.sb([128, TT, 256], BF16, "v")
            c_all, b_c = S.sb([128, TT, 1024], BF16, "c_all")
            tq_r = S.ring_sb(2, [128, 1280], BF16, "tq")
            P.dma("sp", lambda e: e.dma_start(out=v[:], in_=self.TM[:, 4864:5120].rearrange("(t p) c -> p t c", p=128)), writes=[bv])
            kk = 0
            for tt in range(TT):
                tq, btq = tq_r.next()
                P.dma("sp", lambda e, tq=tq, tt=tt: e.dma_start(out=tq[:], in_=self.TM[tt * 128:(tt + 1) * 128, 3584:4864]), writes=[btq])
                for (g0, ng) in [(0, 4), (4, 4), (8, 2)]:
                    pst, bpst = R["ps_t"].next()
                    for j in range(ng):
                        cc = g0 + j
                        P.op("pe", lambda e, pst=pst, j=j, cc=cc, tq=tq: e.transpose(pst[:, j, :], tq[:, cc * 128:(cc + 1) * 128], self.ident_b[:]), reads=[btq], writes=[bpst])
                    if g0 < 8:
                        dstv, bd = qT[:, g0:g0 + 4, tt * 128:(tt + 1) * 128], bq
                    else:
                        dstv, bd = kT[:, 0:2, tt * 128:(tt + 1) * 128], bk
                    kk += 1
                    if kk % 2 == 0:
                        P.op("dve", lambda e, pst=pst, dstv=dstv, ng=ng: e.tensor_copy(dstv, pst[:, 0:ng, :]), reads=[bpst], writes=[bd])
                    else:
                        P.op("act", lambda e, pst=pst, dstv=dstv, ng=ng: e.copy(dstv, pst[:, 0:ng, :]), reads=[bpst], writes=[bd])
            tiles = list(range(16)) + ([16, 17] if with_ctx else [])
            deps = [bq, bk, bv]
            sc = 128.0 ** -0.5
            for i in tiles:
                for h in range(8):
                    g = h // 4
                    q_ap = qT[:, h, i * 128:(i + 1) * 128]
                    segs = []
                    vch = []
                    if i < 16:
                        for j in range(4):
                            segs.append((kT[:, g, j * 512:(j + 1) * 512], 512, None))
                        for t in range(16):
                            vch.append((v[:, t, g * 128:(g + 1) * 128], 128, t * 128))
                        koff = S_LAT
                    else:
                        koff = 0
                    segs.append((kT[:, g, S_LAT:T], 256, None))
                    vch.append((v[:, 16, g * 128:(g + 1) * 128], 128, koff))
                    vch.append((v[:, 17, g * 128:(g + 1) * 128], 128, koff + 128))
                    self.attend(R, q_ap, segs, vch, sc, c_all[:, i, h * 128:(h + 1) * 128], b_c, deps, 128)
            self.attend_flush(R)
            for i in tiles:
                self.tm_to_fm(R, c_all[:, i, :], b_c, self.CT, i)

    def stage_gla(self, l, d, with_ctx):
        P = self.P
        with self.stage("gla%d%d" % (l, d)) as S:
            R = {"st": S.ring_sb(2, [128, 8, 128], BF16, "fmst"), "ps_t": S.ring_ps(2, [128, 4, 128], BF16, "pst")}
            lowa, blow = S.sb([17, T], F32, "lowa")
            P.op("pool", lambda e: e.memset(lowa[:], 1.0), writes=[blow])
            P.dma("sp", lambda e: e.dma_start(out=lowa[0:16, :], in_=self.LOW[d * 16:(d + 1) * 16, :]), writes=[blow])
            w2a, bw2 = S.sb([17, 512], F32, "w2a")
            P.dma("sp", lambda e: e.dma_start(out=w2a[0:16, :], in_=self.w_a2[l][d]), writes=[bw2])
            P.dma("sp", lambda e: e.dma_start(out=w2a[16:17, :], in_=self.b_a[l][d:d + 1, :]), writes=[bw2])
            tri, btri = S.sb([128, 4, 128], F32, "tri")
            P.dma("sp", lambda e: e.dma_start(out=tri[:], in_=self.tri_in.rearrange("f p n -> p f n")), writes=[btri])
            qTa, bqa = S.sb([128, 4, T], BF16, "qTa")
            kTa, bka = S.sb([128, 4, T], BF16, "kTa")
            P.dma("sp", lambda e: e.dma_start(out=qTa[:], in_=self.FM[16:20].rearrange("c p t -> p c t")), writes=[bqa])
            P.dma("sp", lambda e: e.dma_start(out=kTa[:], in_=self.FM[20:24].rearrange("c p t -> p c t")), writes=[bka])
            st32, bs32 = S.sb([128, 4, 256], F32, "st32")
            stb, bsb = S.sb([128, 4, 256], BF16, "stb")
            P.op("pool", lambda e: e.memset(st32[:], 0.0), writes=[bs32])
            P.op("pool", lambda e: e.memset(stb[:], 0.0), writes=[bsb])
            bs, ks = (0, 2) if d == 0 else (1, 3)
            lastcol = 127 if d == 0 else 0
            k_r = S.ring_sb(2, [128, 512], BF16, "ktm")
            v_r = S.ring_sb(2, [128, 1024], BF16, "vtm")
            pz_r = S.ring_ps(2, [128, 512], F32, "pz")
            pbT, bpbT = S.ps([128, 4, 128], F32, "pbT")
            pat, bpat = S.ps([128, 128], F32, "pat")
            po, bpo = S.ps([128, 256], F32, "po")
            pst_, bpst_ = S.ps([128, 256], F32, "pstt")
            f5 = {n: S.ring_sb(2, [128, 512], F32, n) for n in ("az", "e1", "l1", "mn", "la", "ek")}
            eT_r = S.ring_sb(2, [128, 4, 128], F32, "eT")
            enT_r = S.ring_sb(2, [128, 4, 128], F32, "enT")
            qt_r = S.ring_sb(2, [128, 4, 128], BF16, "qt")
            kt_r = S.ring_sb(2, [128, 4, 128], BF16, "kt")
            kh_r = S.ring_sb(2, [128, 512], BF16, "kh")
            at_r = S.ring_sb(2, [128, 128], BF16, "at")
            of_r = S.ring_sb(2, [128, 1024], F32, "of")
            if d == 1:
                os_r = S.ring_sb(2, [128, 1024], F32, "osum")
                sq_r = S.ring_sb(1, [128, 1024], F32, "sqo")
                og_r = S.ring_sb(2, [128, 1024], BF16, "og")
                sg_r = S.ring_sb(2, [128, 1024], F32, "sg")
                tn_r = S.ring_sb(1, [128, 1024], F32, "tn")
                bt_r = S.ring_sb(2, [128, 1024], BF16, "btile")
                ssq_r = S.ring_sb(2, [128, 4], F32, "ssq")
            order = [16, 17] + list(range(16)) if d == 0 else [17, 16] + list(range(15, -1, -1))
            for tt in order:
                need_o = with_ctx or tt < 16
                tc = slice(tt * 128, (tt + 1) * 128)
                ktm, bktm = k_r.next()
                vtm, bvtm = v_r.next()
                P.dma("sp", lambda e, ktm=ktm, tc=tc: e.dma_start(out=ktm[:], in_=self.TM[tc, 1024:1536]), writes=[bktm])
                P.dma("sp", lambda e, vtm=vtm, tc=tc: e.dma_start(out=vtm[:], in_=self.TM[tc, 1536:2560]), writes=[bvtm])
                pz, bpz = pz_r.next()
                P.op("pe", lambda e, pz=pz, tc=tc: e.matmul(pz[:], lowa[0:17, tc], w2a[0:17, :], start=True, stop=True), reads=[blow, bw2], writes=[bpz])
                az, baz = f5["az"].next(); e1, be1 = f5["e1"].next(); l1, bl1 = f5["l1"].next()
                mn, bmn = f5["mn"].next(); la, bla = f5["la"].next(); ek, bek = f5["ek"].next()
                P.op("dve", lambda e, mn=mn, pz=pz: e.tensor_scalar_min(mn[:], pz[:], 0.0), reads=[bpz], writes=[bmn])
                P.op("dve", lambda e, az=az, mn=mn, pz=pz: e.scalar_tensor_tensor(out=az[:], in0=mn[:], scalar=2.0, in1=pz[:], op0=ALU.mult, op1=ALU.subtract), reads=[bpz, bmn], writes=[baz])
                P.op("act", lambda e, e1=e1, az=az: e.activation(e1[:], az[:], AF.Exp), reads=[baz], writes=[be1])
                P.op("act", lambda e, l1=l1, e1=e1: e.activation(l1[:], e1[:], AF.Ln, bias=1.0), reads=[be1], writes=[bl1])
                P.op("dve", lambda e, la=la, mn=mn, l1=l1: e.tensor_tensor(out=la[:], in0=mn[:], in1=l1[:], op=ALU.subtract), reads=[bmn, bl1], writes=[bla])
                pk, bpk = pz_r.next()
                P.op("pe", lambda e, pk=pk, la=la: e.matmul(pk[:], tri[:, ks, :], la[:], start=True, stop=True), reads=[btri, bla], writes=[bpk])
                for h in range(4):
                    P.op("pe", lambda e, la=la, h=h: e.matmul(pbT[:, h, :], la[:, h * 128:(h + 1) * 128], tri[:, bs, :], start=True, stop=True), reads=[btri, bla], writes=[bpbT])
                eT, beT = eT_r.next(); enT, benT = enT_r.next()
                P.op("act", lambda e, eT=eT: e.activation(eT[:], pbT[:], AF.Exp, scale=1.0 / 16), reads=[bpbT], writes=[beT])
                P.op("act", lambda e, enT=enT: e.activation(enT[:], pbT[:], AF.Exp, scale=-1.0 / 16), reads=[bpbT], writes=[benT])
                P.op("act", lambda e, ek=ek, pk=pk: e.activation(ek[:], pk[:], AF.Exp, scale=1.0 / 16), reads=[bpk], writes=[bek])
                qt, bqt = qt_r.next(); kt, bkt = kt_r.next(); kh, bkh = kh_r.next()
                P.op("dve", lambda e, qt=qt, eT=eT, tc=tc: e.scalar_tensor_tensor(out=qt[:], in0=qTa[:, :, tc], scalar=128.0 ** -0.5, in1=eT[:], op0=ALU.mult, op1=ALU.mult),
                     reads=[bqa, beT], writes=[bqt])
                P.op("pool", lambda e, kt=kt, enT=enT, tc=tc: e.tensor_tensor(out=kt[:], in0=kTa[:, :, tc], in1=enT[:], op=ALU.mult), reads=[bka, benT], writes=[bkt])
                P.op("pool", lambda e, kh=kh, ktm=ktm, ek=ek: e.tensor_tensor(out=kh[:], in0=ktm[:], in1=ek[:], op=ALU.mult), reads=[bktm, bek], writes=[bkh])
                of, bof = of_r.next()
                if d == 1 and need_o:
                    P.dma("sp", lambda e, of=of, tc=tc: e.dma_start(out=of[:], in_=self.OF[tc, :]), writes=[bof])
                    osum, bos = os_r.next()
                for h in range(4):
                    hv = slice(h * 256, (h + 1) * 256)
                    if need_o:
                        at, bat = at_r.next()
                        P.op("pe", lambda e, kt=kt, qt=qt, h=h: e.matmul(pat[:], kt[:, h, :], qt[:, h, :], start=True, stop=True), reads=[bkt, bqt], writes=[bpat])
                        P.op("dve", lambda e, at=at: e.tensor_tensor(out=at[:], in0=pat[:], in1=tri[:, bs, :], op=ALU.mult), reads=[bpat, btri], writes=[bat])
                        P.op("pe", lambda e, at=at, vtm=vtm, hv=hv: e.matmul(po[:], at[:], vtm[:, hv], start=True, stop=False), reads=[bat, bvtm], writes=[bpo])
                        P.op("pe", lambda e, qt=qt, h=h: e.matmul(po[:], qt[:, h, :], stb[:, h, :], start=False, stop=True), reads=[bqt, bsb], writes=[bpo])
                        if d == 0:
                            P.op("act", lambda e, of=of, hv=hv: e.copy(of[:, hv], po[:]), reads=[bpo], writes=[bof])
                        else:
                            P.op("dve", lambda e, osum=osum, of=of, hv=hv: e.tensor_tensor(out=osum[:, hv], in0=po[:], in1=of[:, hv], op=ALU.add), reads=[bpo, bof], writes=[bos])
                    P.op("pe", lambda e, kh=kh, vtm=vtm, h=h, hv=hv: e.matmul(pst_[:], kh[:, h * 128:(h + 1) * 128], vtm[:, hv], start=True, stop=True), reads=[bkh, bvtm], writes=[bpst_])
                    P.op("dve", lambda e, eT=eT, h=h: e.scalar_tensor_tensor(out=st32[:, h, :], in0=st32[:, h, :], scalar=eT[:, h, lastcol:lastcol + 1], in1=pst_[:],
                                                                             op0=ALU.mult, op1=ALU.add), reads=[bs32, beT, bpst_], writes=[bs32])
                    P.op("act", lambda e, h=h: e.copy(stb[:, h, :], st32[:, h, :]), reads=[bs32], writes=[bsb])
                if not need_o:
                    continue
                if d == 0:
                    P.dma("sp", lambda e, of=of, tc=tc: e.dma_start(out=self.OF[tc, :], in_=of[:]), reads=[bof])
                else:
                    sq, bsq = sq_r.next(); ssq, bssq = ssq_r.next(); og, bog = og_r.next(); sg, bsg = sg_r.next()
                    tn, btn = tn_r.next(); btile, bbt = bt_r.next()
                    P.dma("sp", lambda e, og=og, tc=tc: e.dma_start(out=og[:], in_=self.TM[tc, 2560:3584]), writes=[bog])
                    P.op("act", lambda e, sq=sq, osum=osum: e.activation(sq[:], osum[:], AF.Square), reads=[bos], writes=[bsq])
                    P.op("dve", lambda e, ssq=ssq, sq=sq: e.tensor_reduce(out=ssq[:, 0:4], in_=sq[:].rearrange("p (h d) -> p h d", h=4), axis=AX.X, op=ALU.add), reads=[bsq], writes=[bssq])
                    P.op("dve", lambda e, ssq=ssq: e.tensor_scalar(ssq[:], ssq[:], 1.0 / 256, EPS, ALU.mult, ALU.add), reads=[bssq], writes=[bssq])
                    P.op("act", lambda e, ssq=ssq: e.sqrt(ssq[:], ssq[:]), reads=[bssq], writes=[bssq])
                    P.op("dve", lambda e, ssq=ssq: e.reciprocal(ssq[:], ssq[:]), reads=[bssq], writes=[bssq])
                    P.op("act", lambda e, sg=sg, og=og: e.activation(sg[:], og[:], AF.Silu), reads=[bog], writes=[bsg])
                    gng = self.gbc[:, 512 + 256 * l:768 + 256 * l]
                    for h in range(4):
                        hv = slice(h * 256, (h + 1) * 256)
                        P.op("dve", lambda e, tn=tn, osum=osum, ssq=ssq, h=h, hv=hv: e.scalar_tensor_tensor(out=tn[:, hv], in0=osum[:, hv], scalar=ssq[:, h:h + 1], in1=gng,
                                                                                                          op0=ALU.mult, op1=ALU.mult), reads=[bos, bssq], writes=[btn])
                    P.op("pool", lambda e, btile=btile, tn=tn, sg=sg: e.tensor_tensor(out=btile[:], in0=tn[:], in1=sg[:], op=ALU.mult), reads=[btn, bsg], writes=[bbt])
                    self.tm_to_fm(R, btile[:], bbt, self.BT, tt)

    def stage_merge_a(self, l, with_ctx):
        P = self.P
        with self.stage("mga%d" % l) as S:
            br = []
            for nm, src in (("a", self.AT), ("b", self.BT), ("c", self.CT)):
                t, b = S.sb([128, 8, T], BF16, nm + "T")
                P.dma("sp", lambda e, t=t, src=src: e.dma_start(out=t[:], in_=src.rearrange("c p t -> p c t")), writes=[b])
                br.append((t, b))
            wsrc = [self.w_pa[l], self.w_pb[l], self.w_pc[l]]
            w_r = [S.ring_sb(2, [128, 8, 128], BF16, "wp%d" % i) for i in range(3)]
            g_r = [S.ring_sb(2, [128, T], BF16, "g%d" % i) for i in range(3)]
            ps_r = [S.ring_ps(2, [128, 512], F32, "psm%d" % i) for i in range(3)]
            t_r = [S.ring_sb(2, [128, 512], F32, "tm%d" % i) for i in range(3)]
            m_r = S.ring_sb(2, [128, T], BF16, "mst")
            blks = BLK5 if with_ctx else BLK5[:4]
            for mc in range(DC):
                ws = []
                gs = []
                for i in range(3):
                    wt, bw = w_r[i].next()
                    P.dma("pool", lambda e, wt=wt, i=i, mc=mc: e.dma_start(out=wt[:], in_=wsrc[i].rearrange("(k p) n -> p k n", p=128)[:, :, mc * 128:(mc + 1) * 128]), writes=[bw])
                    ws.append((wt, bw))
                    gt, bg = g_r[i].next()
                    P.dma("sp", lambda e, gt=gt, i=i, mc=mc: e.dma_start(out=gt[:], in_=self.FM[24 + 16 * i + mc]), writes=[bg])
                    gs.append((gt, bg))
                mst, bm = m_r.next()
                for (t0, nb, w) in blks:
                    tts = []
                    for i in range(3):
                        ps, bps = ps_r[i].next()
                        for kk in range(8):
                            P.op("pe", lambda e, ps=ps, i=i, kk=kk, t0=t0, nb=nb, wt=ws[i][0]: e.matmul(ps[:, 0:nb], wt[:, kk, :], br[i][0][:, kk, t0:t0 + nb], start=(kk == 0), stop=(kk == 7)),
                                 reads=[ws[i][1], br[i][1]], writes=[bps])
                        tt_, btt = t_r[i].next()
                        P.op("dve", lambda e, tt_=tt_, ps=ps, nb=nb, t0=t0, gt=gs[i][0]: e.tensor_tensor(out=tt_[:, 0:nb], in0=ps[:, 0:nb], in1=gt[:, t0:t0 + nb], op=ALU.mult),
                             reads=[bps, gs[i][1]], writes=[btt])
                        tts.append((tt_, btt))
                    P.op("pool", lambda e, nb=nb, a=tts[0][0], b=tts[1][0]: e.tensor_tensor(out=a[:, 0:nb], in0=a[:, 0:nb], in1=b[:, 0:nb], op=ALU.add),
                         reads=[tts[0][1], tts[1][1]], writes=[tts[0][1]])
                    P.op("pool", lambda e, nb=nb, t0=t0, mst=mst, a=tts[0][0], c=tts[2][0]: e.tensor_tensor(out=mst[:, t0:t0 + nb], in0=a[:, 0:nb], in1=c[:, 0:nb], op=ALU.add),
                         reads=[tts[0][1], tts[2][1]], writes=[bm])
                ncol = T if with_ctx else S_LAT
                P.dma("sp", lambda e, mst=mst, mc=mc, ncol=ncol: e.dma_start(out=self.MT[mc][:, 0:ncol], in_=mst[:, 0:ncol]), reads=[bm])

    def stage_merge_b(self, l, with_ctx):
        P = self.P
        with self.stage("mgb%d" % l) as S:
            ncol = T if with_ctx else S_LAT
            wo, bwo = S.sb([128, DC, D], BF16, "wo")
            P.dma("pool", lambda e: e.dma_start(out=wo[:], in_=self.w_out[l].rearrange("(k p) n -> p k n", p=128)), writes=[bwo])
            mT, bmT = S.sb([128, DC, T], BF16, "mT")
            P.dma("sp", lambda e: e.dma_start(out=mT[:, :, 0:ncol], in_=self.MT[:, :, 0:ncol].rearrange("c p t -> p c t")), writes=[bmT])
            x_r = S.ring_sb(2, [128, T], F32, "xr")
            ps_r = S.ring_ps(4, [128, 512], F32, "pso")
            blks = BLK5 if with_ctx else BLK5[:4]
            G1 = self.modT[l][:, 32:48, :]
            for mc in range(DC):
                xr, bx = x_r.next()
                P.dma("sp", lambda e, xr=xr, mc=mc: e.dma_start(out=xr[:, 0:ncol], in_=self.XT[mc][:, 0:ncol]), writes=[bx])
                for (t0, nb, w) in blks:
                    ps, bps = ps_r.next()
                    for kk in range(DC):
                        P.op("pe", lambda e, ps=ps, kk=kk, mc=mc, t0=t0, nb=nb: e.matmul(ps[:, 0:nb], wo[:, kk, mc * 128:(mc + 1) * 128], mT[:, kk, t0:t0 + nb], start=(kk == 0), stop=(kk == DC - 1)),
                             reads=[bwo, bmT], writes=[bps])
                    P.op("dve", lambda e, ps=ps, xr=xr, mc=mc, t0=t0, nb=nb, w=w: e.scalar_tensor_tensor(out=xr[:, t0:t0 + nb], in0=ps[:, 0:nb], scalar=G1[:, mc, w:w + 1], in1=xr[:, t0:t0 + nb],
                                                                                                       op0=ALU.mult, op1=ALU.add), reads=[bps, bx], writes=[bx])
                P.dma("sp", lambda e, xr=xr, mc=mc: e.dma_start(out=self.XT[mc][:, 0:ncol], in_=xr[:, 0:ncol]), reads=[bx])

    def stage_ffn_block(self, l, t0, nb, w, moe):
        P = self.P
        with self.stage("ffn%d_%d" % (l, t0)) as S:
            hT, b_hT = S.sb([128, DC, nb], BF16, "hT")
            XS = self.XH if moe else self.XT
            self.norm_blocks(S, hT, b_hT, self.A2[l], self.modT[l][:, 48:64, :], tok0=t0, ntok=nb, nring=1, src=XS)
            actT, bact = S.sb([128, FC, nb], BF16, "actT")
            G2 = self.modT[l][:, 80:96, :]
            wg_r = S.ring_sb(2, [128, DC, 256], BF16, "wg")
            wu_r = S.ring_sb(2, [128, DC, 256], BF16, "wu")
            wd_r = S.ring_sb(2, [128, FC, 128], BF16, "wd")
            psg_r = S.ring_ps(2, [128, 512], F32, "psg")
            psu_r = S.ring_ps(2, [128, 512], F32, "psu")
            psd_r = S.ring_ps(2, [128, 512], F32, "psd")
            sg_r = S.ring_sb(2, [128, 512], F32, "sg")
            x_r = S.ring_sb(2, [128, 512], F32, "xr")
            if moe:
                self.wait_conv()
                yacc, byacc = S.sb([128, DC, nb], F32, "yacc")
                g8, bg8 = S.sb([8, nb], F32, "g8")
                P.dma("sp", lambda e: e.dma_start(out=g8[:], in_=self.GATE[:, t0:t0 + nb]), writes=[bg8])
                sel, bsel = S.sb([8, 1024], F32, "sel")
                P.dma("sp", lambda e: e.dma_start(out=sel[:], in_=self.sel_in), writes=[bsel])
                gb_r = S.ring_sb(2, [128, 512], F32, "gb")
                tmp_r = S.ring_sb(2, [128, 512], F32, "tmpa")
                experts = list(range(NE))
            else:
                experts = [None]
            for ei, ex in enumerate(experts):
                if moe:
                    wgs = self.mw_gate[ex].rearrange("(k p) n -> p k n", p=128)
                    wus = self.mw_up[ex].rearrange("(k p) n -> p k n", p=128)
                    wds = self.mw_down[ex].rearrange("(f p) n -> p f n", p=128)
                    gb, bgb = gb_r.next()
                    psb, bpsb = psd_r.next()
                    P.op("pe", lambda e, psb=psb, ex=ex: e.matmul(psb[:, 0:nb], sel[0:8, ex * 128:(ex + 1) * 128], g8[0:8, :], start=True, stop=True), reads=[bsel, bg8], writes=[bpsb])
                    P.op("act", lambda e, psb=psb, gb=gb: e.copy(gb[:, 0:nb], psb[:, 0:nb]), reads=[bpsb], writes=[bgb])
                else:
                    wgs = self.dw_gate.rearrange("(k p) n -> p k n", p=128)
                    wus = self.dw_up.rearrange("(k p) n -> p k n", p=128)
                    wds = self.dw_down.rearrange("(f p) n -> p f n", p=128)
                for fg in range(FC // 2):
                    wg, bwg = wg_r.next()
                    wu, bwu = wu_r.next()
                    if moe:
                        P.dma("sp", lambda e, wg=wg, fg=fg, ex=ex: e.dma_start(out=wg[:], in_=self.WGB[ex, fg].rearrange("p (k n) -> p k n", k=DC)), writes=[bwg])
                        P.dma("sp", lambda e, wu=wu, fg=fg, ex=ex: e.dma_start(out=wu[:], in_=self.WUB[ex, fg].rearrange("p (k n) -> p k n", k=DC)), writes=[bwu])
                    else:
                        P.dma("pool", lambda e, wg=wg, fg=fg, wgs=wgs: e.dma_start(out=wg[:], in_=wgs[:, :, fg * 256:(fg + 1) * 256]), writes=[bwg])
                        P.dma("pool", lambda e, wu=wu, fg=fg, wus=wus: e.dma_start(out=wu[:], in_=wus[:, :, fg * 256:(fg + 1) * 256]), writes=[bwu])
                    for j in range(2):
                        fc = 2 * fg + j
                        psg, bpsg = psg_r.next()
                        psu, bpsu = psu_r.next()
                        for kk in range(DC):
                            P.op("pe", lambda e, psg=psg, wg=wg, kk=kk, j=j: e.matmul(psg[:, 0:nb], wg[:, kk, j * 128:(j + 1) * 128], hT[:, kk, :], start=(kk == 0), stop=(kk == DC - 1)),
                                 reads=[bwg, b_hT], writes=[bpsg])
                        for kk in range(DC):
                            P.op("pe", lambda e, psu=psu, wu=wu, kk=kk, j=j: e.matmul(psu[:, 0:nb], wu[:, kk, j * 128:(j + 1) * 128], hT[:, kk, :], start=(kk == 0), stop=(kk == DC - 1)),
                                 reads=[bwu, b_hT], writes=[bpsu])
                        sg, bsg = sg_r.next()
                        P.op("act", lambda e, sg=sg, psg=psg: e.activation(sg[:, 0:nb], psg[:, 0:nb], AF.Silu), reads=[bpsg], writes=[bsg])
                        if moe:
                            tmp, btmp = tmp_r.next()
                            P.op("dve", lambda e, tmp=tmp, sg=sg, psu=psu: e.tensor_tensor(out=tmp[:, 0:nb], in0=sg[:, 0:nb], in1=psu[:, 0:nb], op=ALU.mult), reads=[bsg, bpsu], writes=[btmp])
                            P.op("pool", lambda e, tmp=tmp, gb=gb, fc=fc: e.tensor_tensor(out=actT[:, fc, :], in0=tmp[:, 0:nb], in1=gb[:, 0:nb], op=ALU.mult), reads=[btmp, bgb], writes=[bact])
                        else:
                            P.op("dve", lambda e, sg=sg, psu=psu, fc=fc: e.tensor_tensor(out=actT[:, fc, :], in0=sg[:, 0:nb], in1=psu[:, 0:nb], op=ALU.mult), reads=[bsg, bpsu], writes=[bact])
                for mc in range(DC):
                    wd, bwd = wd_r.next()
                    if moe:
                        P.dma("sp", lambda e, wd=wd, mc=mc, ex=ex: e.dma_start(out=wd[:], in_=self.WDB[ex, mc].rearrange("p (f n) -> p f n", f=FC)), writes=[bwd])
                    else:
                        P.dma("pool", lambda e, wd=wd, mc=mc, wds=wds: e.dma_start(out=wd[:], in_=wds[:, :, mc * 128:(mc + 1) * 128]), writes=[bwd])
                    psd, bpsd = psd_r.next()
                    for fc in range(FC):
                        P.op("pe", lambda e, psd=psd, wd=wd, fc=fc: e.matmul(psd[:, 0:nb], wd[:, fc, :], actT[:, fc, :], start=(fc == 0), stop=(fc == FC - 1)),
                             reads=[bwd, bact], writes=[bpsd])
                    if moe:
                        if ei == 0:
                            P.op("act", lambda e, psd=psd, mc=mc: e.copy(yacc[:, mc, :], psd[:, 0:nb]), reads=[bpsd], writes=[byacc])
                        else:
                            P.op("dve", lambda e, psd=psd, mc=mc: e.tensor_tensor(out=yacc[:, mc, :], in0=yacc[:, mc, :], in1=psd[:, 0:nb], op=ALU.add), reads=[bpsd, byacc], writes=[byacc])
                    else:
                        xr, bx = x_r.next()
                        P.dma("sp", lambda e, xr=xr, mc=mc: e.dma_start(out=xr[:, 0:nb], in_=XS[mc][:, t0:t0 + nb]), writes=[bx])
                        P.op("dve", lambda e, psd=psd, xr=xr, mc=mc: e.scalar_tensor_tensor(out=xr[:, 0:nb], in0=psd[:, 0:nb], scalar=G2[:, mc, w:w + 1], in1=xr[:, 0:nb],
                                                                                             op0=ALU.mult, op1=ALU.add), reads=[bpsd, bx], writes=[bx])
                        P.dma("sp", lambda e, xr=xr, mc=mc: e.dma_start(out=XS[mc][:, t0:t0 + nb], in_=xr[:, 0:nb]), reads=[bx])
            if moe:
                for mc in range(DC):
                    xr, bx = x_r.next()
                    P.dma("sp", lambda e, xr=xr, mc=mc: e.dma_start(out=xr[:, 0:nb], in_=XS[mc][:, t0:t0 + nb]), writes=[bx])
                    P.op("dve", lambda e, xr=xr, mc=mc: e.scalar_tensor_tensor(out=xr[:, 0:nb], in0=yacc[:, mc, :], scalar=G2[:, mc, w:w + 1], in1=xr[:, 0:nb],
                                                                                op0=ALU.mult, op1=ALU.add), reads=[byacc, bx], writes=[bx])
                    P.dma("sp", lambda e, xr=xr, mc=mc: e.dma_start(out=XS[mc][:, t0:t0 + nb], in_=xr[:, 0:nb]), reads=[bx])

    def stage_router(self, l):
        P = self.P
        with self.stage("router") as S:
            rw, brw = S.sb([128, DC, NE], F32, "rw")
            P.dma("sp", lambda e: e.dma_start(out=rw[:], in_=self.router_w.rearrange("(k p) n -> p k n", p=128)), writes=[brw])
            rb, brb = S.sb([1, NE], F32, "rb")
            P.dma("sp", lambda e: e.dma_start(out=rb[:], in_=self.router_b), writes=[brb])
            gsb, bgsb = S.sb([8, HALF], F32, "gsb")
            pl_r = S.ring_ps(2, [128, NE], F32, "pl")
            pt_r = S.ring_ps(2, [8, 128], F32, "ptg")
            sm_r = S.ring_sb(2, [128, 8, 8], F32, "rsm")

            def cb(bi, t0, h32, b_h32):
                for s_ in range(2):
                    pl, bpl = pl_r.next()
                    for c in range(DC):
                        P.op("pe", lambda e, pl=pl, c=c, s_=s_, h32=h32: e.matmul(pl[:], h32[:, c, s_ * 128:(s_ + 1) * 128], rw[:, c, :], start=(c == 0), stop=False),
                             reads=[b_h32, brw], writes=[bpl])
                    P.op("pe", lambda e, pl=pl: e.matmul(pl[:], self.ones_f[0:1, :], rb[0:1, :], start=False, stop=True), reads=[brb], writes=[bpl])
                    sm, bsm = sm_r.next()
                    lg, eq1, lg2, eq2, gate, sc_ = sm[:, 0, :], sm[:, 1, :], sm[:, 2, :], sm[:, 3, :], sm[:, 4, :], sm[:, 5, :]
                    P.op("dve", lambda e, lg=lg, pl=pl: e.tensor_copy(lg, pl[:]), reads=[bpl], writes=[bsm])
                    P.op("dve", lambda e, lg=lg, sc_=sc_: e.reduce_max(out=sc_[:, 0:1], in_=lg, axis=AX.X), reads=[bsm], writes=[bsm])
                    P.op("dve", lambda e, sc_=sc_: e.tensor_scalar_mul(sc_[:, 1:2], sc_[:, 0:1], -1.0), reads=[bsm], writes=[bsm])
                    P.op("act", lambda e, lg=lg, eq1=eq1, sc_=sc_: e.sign(eq1, lg, bias=sc_[:, 1:2]), reads=[bsm], writes=[bsm])
                    P.op("dve", lambda e, eq1=eq1: e.tensor_scalar_add(eq1, eq1, 1.0), reads=[bsm], writes=[bsm])
                    P.op("dve", lambda e, lg=lg, eq1=eq1, lg2=lg2: e.scalar_tensor_tensor(out=lg2, in0=eq1, scalar=-1.0e30, in1=lg, op0=ALU.mult, op1=ALU.add), reads=[bsm], writes=[bsm])
                    P.op("dve", lambda e, lg2=lg2, sc_=sc_: e.reduce_max(out=sc_[:, 2:3], in_=lg2, axis=AX.X), reads=[bsm], writes=[bsm])
                    P.op("dve", lambda e, sc_=sc_: e.tensor_scalar_mul(sc_[:, 3:4], sc_[:, 2:3], -1.0), reads=[bsm], writes=[bsm])
                    P.op("act", lambda e, lg2=lg2, eq2=eq2, sc_=sc_: e.sign(eq2, lg2, bias=sc_[:, 3:4]), reads=[bsm], writes=[bsm])
                    P.op("dve", lambda e, eq2=eq2: e.tensor_scalar_add(eq2, eq2, 1.0), reads=[bsm], writes=[bsm])
                    P.op("dve", lambda e, sc_=sc_: e.tensor_tensor(out=sc_[:, 4:5], in0=sc_[:, 2:3], in1=sc_[:, 0:1], op=ALU.subtract), reads=[bsm], writes=[bsm])
                    P.op("act", lambda e, sc_=sc_: e.activation(sc_[:, 4:5], sc_[:, 4:5], AF.Exp), reads=[bsm], writes=[bsm])
                    P.op("dve", lambda e, sc_=sc_: e.tensor_scalar_add(sc_[:, 5:6], sc_[:, 4:5], 1.0), reads=[bsm], writes=[bsm])
                    P.op("dve", lambda e, sc_=sc_: e.reciprocal(sc_[:, 5:6], sc_[:, 5:6]), reads=[bsm], writes=[bsm])
                    P.op("dve", lambda e, sc_=sc_: e.tensor_tensor(out=sc_[:, 6:7], in0=sc_[:, 4:5], in1=sc_[:, 5:6], op=ALU.mult), reads=[bsm], writes=[bsm])
                    P.op("dve", lambda e, gate=gate, eq1=eq1, sc_=sc_: e.tensor_scalar_mul(gate, eq1, sc_[:, 5:6]), reads=[bsm], writes=[bsm])
                    P.op("dve", lambda e, gate=gate, eq2=eq2, sc_=sc_: e.scalar_tensor_tensor(out=gate, in0=eq2, scalar=sc_[:, 6:7], in1=gate, op0=ALU.mult, op1=ALU.add), reads=[bsm], writes=[bsm])
                    ptg, bptg = pt_r.next()
                    P.op("pe", lambda e, ptg=ptg, gate=gate: e.transpose(ptg[:], gate, self.ident_f[:]), reads=[bsm], writes=[bptg])
                    tcol = t0 + s_ * 128
                    P.op("act", lambda e, ptg=ptg, tcol=tcol: e.copy(gsb[:, tcol:tcol + 128], ptg[:]), reads=[bptg], writes=[bgsb])
            self.norm_blocks(S, None, None, self.A2[l], self.modT[l][:, 48:64, :], tok0=0, ntok=HALF, nring=2, cb=cb, src=self.XH)
            P.dma("sp", lambda e: e.dma_start(out=self.GATE, in_=gsb[:]), reads=[bgsb])

    def stage_final(self):
        P = self.P
        with self.stage("final") as S:
            pt_r = S.ring_ps(2, [128, 4, 128], F32, "ptf")
            ot_r = S.ring_sb(2, [128, D], F32, "ot")
            cnt = [0]

            def cb(bi, t0, h32, b_h32):
                for s_ in range(2):
                    ot, bot = ot_r.next()
                    for g in range(4):
                        pt, bpt = pt_r.next()
                        for j in range(4):
                            cc = 4 * g + j
                            P.op("pe", lambda e, pt=pt, j=j, cc=cc, s_=s_, h32=h32: e.transpose(pt[:, j, :], h32[:, cc, s_ * 128:(s_ + 1) * 128], self.ident_f[:]), reads=[b_h32], writes=[bpt])
                        cnt[0] += 1
                        if cnt[0] % 2 == 0:
                            P.op("dve", lambda e, pt=pt, ot=ot, g=g: e.tensor_copy(ot[:, g * 512:(g + 1) * 512], pt[:].rearrange("p a b -> p (a b)")), reads=[bpt], writes=[bot])
                        else:
                            P.op("act", lambda e, pt=pt, ot=ot, g=g: e.copy(ot[:, g * 512:(g + 1) * 512], pt[:].rearrange("p a b -> p (a b)")), reads=[bpt], writes=[bot])
                    r0 = t0 + s_ * 128
                    P.dma("sp", lambda e, ot=ot, r0=r0: e.dma_start(out=self.OUT[r0:r0 + 128, :], in_=ot[:]), reads=[bot])
            self.norm_blocks(S, None, None, None, None, tok0=0, ntok=HALF, nring=2, gain_only=self.gT[:, 64:80], cb=cb, src=self.XH)

    def stage_split(self):
        P = self.P
        with self.stage("split") as S:
            hs, bhs = S.sb([128, 2], F32, "hs")
            P.dma("sp", lambda e: e.dma_start(out=hs[:], in_=self.half_in), writes=[bhs])
            xa_r = S.ring_sb(2, [128, HALF], F32, "xa")
            xb_r = S.ring_sb(2, [128, HALF], F32, "xb")
            for c in range(DC):
                xa, bxa = xa_r.next()
                xb, bxb = xb_r.next()
                P.dma("sp", lambda e, xa=xa, c=c: e.dma_start(out=xa[:], in_=self.XT[c][:, 0:HALF]), writes=[bxa])
                P.dma("sp", lambda e, xb=xb, c=c: e.dma_start(out=xb[:], in_=self.XT[c][:, HALF:S_LAT]), writes=[bxb])
                P.op("dve", lambda e, xa=xa: e.tensor_scalar_mul(xa[:], xa[:], hs[:, 0:1]), reads=[bxa, bhs], writes=[bxa])
                P.op("dve", lambda e, xa=xa, xb=xb: e.scalar_tensor_tensor(out=xa[:], in0=xb[:], scalar=hs[:, 1:2], in1=xa[:], op0=ALU.mult, op1=ALU.add), reads=[bxa, bxb, bhs], writes=[bxa])
                P.dma("sp", lambda e, xa=xa, c=c: e.dma_start(out=self.XH[c], in_=xa[:]), reads=[bxa])

    def build_all(self):
        self.stage_prep()
        for l in range(2):
            wc = (l == 0)
            self.stage_inproj(l)
            self.stage_na(l, wc)
            self.stage_gqa(l, wc)
            self.stage_gla(l, 0, wc)
            self.stage_gla(l, 1, wc)
            self.stage_merge_a(l, wc)
            self.stage_merge_b(l, wc)
            if l == 0:
                for (t0, nb, w) in BLK5:
                    self.stage_ffn_block(0, t0, nb, w, False)
            else:
                self.stage_split()
                self.stage_router(1)
                for (t0, nb, w) in BLK5[:2]:
                    self.stage_ffn_block(1, t0, nb, w, True)
        self.stage_final()

    def finish(self):
        self.gst.__exit__(None, None, None)
        return self.nc


def make_consts():
    k = {}
    k["k_ident"] = np.eye(128, dtype=np.float32)
    j = np.arange(128)[:, None]
    i = np.arange(128)[None, :]
    tri = np.zeros((4, 128, 128), np.float32)
    tri[0] = (j <= i)
    tri[1] = (j >= i)
    tri[2] = (j > i)
    tri[3] = (j < i)
    k["k_tri"] = tri
    half = 64
    freqs = (10000.0 ** (-np.arange(0, half, 2, dtype=np.float32) / half)).astype(np.float32)
    t = np.arange(S_LAT)
    row = (t // 64).astype(np.float32)
    col = (t % 64).astype(np.float32)
    ang = np.concatenate([row[:, None] * freqs, col[:, None] * freqs], axis=-1).astype(np.float32)
    cos = np.ones((T, 64), np.float32)
    sin = np.zeros((T, 64), np.float32)
    cos[:S_LAT] = np.cos(ang)
    sin[:S_LAT] = np.sin(ang)
    cs = np.concatenate([np.tile(cos[:, None, :], (1, 4, 1)).reshape(T, 256), np.tile(sin[:, None, :], (1, 4, 1)).reshape(T, 256)], axis=1)
    k["k_cs"] = np.ascontiguousarray(cs, dtype=np.float32)
    mask = np.full((5, 128, 576), NEG, np.float32)
    qc = np.arange(64)
    cstart = np.clip(qc - 8, 0, 48)
    kc = np.arange(64)
    inwin = (kc[None, :] >= cstart[:, None]) & (kc[None, :] < cstart[:, None] + 16)
    for ty, i0 in enumerate([0, 1, 2, 14, 15]):
        rs0 = int(np.clip(2 * i0 - 4, 0, 24))
        rs1 = int(np.clip(2 * i0 + 1 - 4, 0, 24))
        base = rs0
        nr = rs1 + 8 - rs0
        for a in range(2):
            rs = rs0 if a == 0 else rs1
            for rho in range(nr):
                kr = base + rho
                if rs <= kr <= rs + 7:
                    blk = np.where(inwin, 0.0, NEG).astype(np.float32)
                    mask[ty, a * 64:(a + 1) * 64, rho * 64:(rho + 1) * 64] = blk
    k["k_namask"] = mask
    sel = np.zeros((8, 8, 128), np.float32)
    for e in range(8):
        sel[e, e, :] = 1.0
    k["k_sel"] = sel.reshape(8, 1024)
    return k


def make_inmap(inp, b, cfg, consts):
    m = dict(consts)
    f = lambda a: np.ascontiguousarray(a, dtype=np.float32)
    hsel = np.zeros((128, 2), np.float32)
    hsel[:, cfg.get("half", 0)] = 1.0
    m["k_half"] = hsel
    m["x"] = f(inp["x"][b])
    m["ctx"] = f(inp["ctx"][b])
    m["c"] = f(inp["c"][b:b + 1])
    m["c_ctx"] = f(inp["c_ctx"][None, :])
    m["w_ada"] = f(inp["w_ada"])
    m["b_ada"] = f(inp["b_ada"])
    m["norm1_g"] = f(inp["norm1_g"])
    m["norm2_g"] = f(inp["norm2_g"])
    m["final_norm_g"] = f(inp["final_norm_g"][None, :])
    m["gqa_qn_g"] = f(inp["gqa_qn_g"])
    m["gqa_kn_g"] = f(inp["gqa_kn_g"])
    m["gla_norm_g"] = f(inp["gla_norm_g"])
    for l in (cfg["layers"] if cfg.get("mixer", True) else []):
        m["w_in%d" % l] = f(inp["w_in"][l])
        m["na_rpb%d" % l] = f(inp["na_rpb"][l].reshape(16, 15 * 31))
        m["gla_w_a2_%d" % l] = f(inp["gla_w_a2"][l])
        m["gla_b_a%d" % l] = f(inp["gla_b_a"][l])
        m["w_pa%d" % l] = f(inp["w_pa"][l])
        m["w_pb%d" % l] = f(inp["w_pb"][l])
        m["w_pc%d" % l] = f(inp["w_pc"][l])
        m["w_out%d" % l] = f(inp["w_out"][l])
    if cfg.get("ffn", True):
        if 0 in cfg["layers"]:
            m["dense_w_gate"] = f(inp["dense_w_gate"][0])
            m["dense_w_up"] = f(inp["dense_w_up"][0])
            m["dense_w_down"] = f(inp["dense_w_down"][0])
        if 1 in cfg["layers"]:
            m["router_w"] = f(inp["router_w"][0])
            m["router_b"] = f(inp["router_b"])
            m["moe_w_gate"] = f(inp["moe_w_gate"][0])
            m["moe_w_up"] = f(inp["moe_w_up"][0])
            m["moe_w_down"] = f(inp["moe_w_down"][0])
    return m


FULL_CFG = {"layers": [0, 1], "ffn": True, "mixer": True, "debug": []}
N_CORES = 8


def kernel(**inputs):
    cfg = FULL_CFG
    B = Builder(cfg)
    B.declare()
    B.build_all()
    nc = B.finish()
    consts = make_consts()
    in_maps = []
    for c in range(N_CORES):
        b, half = c % 4, c // 4
        m = make_inmap(inputs, b, dict(cfg, half=half), consts)
        in_maps.append({k: v for k, v in m.items() if k in B.inputs})
    res = run_bass_kernel_spmd(nc, in_maps, core_ids=list(range(N_CORES)))
    out = np.empty((4, S_LAT, D), np.float32)
    for c in range(N_CORES):
        b, half = c % 4, c // 4
        out[b, half * HALF:(half + 1) * HALF] = np.asarray(res.results[c]["out"], dtype=np.float32)
    return out
```
